# Optimizing a Trainium2 kernel written in Bass

```python
import math
import jax, jax.numpy as jnp
from jax import lax
import numpy as np

D_MODEL = 2048
BATCH = 4
SEQ = 4096
DEPTH = 2

N_EVEN = (DEPTH + 1) // 2
N_ODD = DEPTH // 2
EPS = 1e-6

SSD_HEADS = 32
SSD_HEAD_DIM = 64
SSD_INNER = SSD_HEADS * SSD_HEAD_DIM
SSD_GROUPS = 8
SSD_STATE = 128
SSD_CONV = 4
SSD_CHUNK = 128
SSD_CONV_DIM = SSD_INNER + 2 * SSD_GROUPS * SSD_STATE

GMLP_GROUPS = 16
GMLP_GROUP_DIM = 128
GMLP_INNER = GMLP_GROUPS * GMLP_GROUP_DIM
GMLP_CHUNK = 128

IN0_DIM = SSD_INNER + SSD_CONV_DIM + SSD_HEADS + 2 * GMLP_INNER
MIX0_DIM = SSD_INNER + GMLP_INNER

SB_HEADS = 16
SB_HEAD_DIM = D_MODEL // SB_HEADS
SB_BLOCK = 128

PEER_HEADS = 8
PEER_KEYS = 128
PEER_EXPERTS = PEER_KEYS * PEER_KEYS
PEER_KEY_DIM = 256
PEER_HALF = PEER_KEY_DIM // 2
PEER_TOPK = 16
PEER_TOKEN_BLOCK = 128

kernel_name = 'hybrid_ssd_gmlp_stickbreak_peer'


def rms_norm(x, g):
    xf = x.astype(jnp.float32)
    y = xf * lax.rsqrt(jnp.mean(xf * xf, axis=-1, keepdims=True) + EPS)
    return (y * g.astype(jnp.float32)).astype(x.dtype)


def layer_norm(x, g, b):
    xf = x.astype(jnp.float32)
    mu = jnp.mean(xf, axis=-1, keepdims=True)
    xc = xf - mu
    var = jnp.mean(xc * xc, axis=-1, keepdims=True)
    y = xc * lax.rsqrt(var + EPS) * g.astype(jnp.float32) + b.astype(jnp.float32)
    return y.astype(x.dtype)


def modulate(h, shift, scale):
    return h * (1 + scale[:, None, :]) + shift[:, None, :]


def causal_depthwise_conv(x, w, b):
    K, C = w.shape
    y = lax.conv_general_dilated(
        x, w[:, None, :].astype(x.dtype), window_strides=(1,), padding=[(K - 1, 0)],
        dimension_numbers=('NWC', 'WIO', 'NWC'), feature_group_count=C)
    return y + b


def ssd_chunked_scan(xs, dt, A, Bm, Cm):
    f32 = jnp.float32
    Bsz, S, H, P = xs.shape
    G, N = Bm.shape[2], Bm.shape[3]
    R = H // G
    L = SSD_CHUNK
    nc = S // L
    x = (xs.astype(f32) * dt[..., None]).reshape(Bsz, nc, L, G, R, P)
    a = (dt * A).reshape(Bsz, nc, L, G, R).transpose(0, 3, 4, 1, 2)
    a_cum = jnp.cumsum(a, axis=-1)
    Bc = Bm.astype(f32).reshape(Bsz, nc, L, G, N)
    Cc = Cm.astype(f32).reshape(Bsz, nc, L, G, N)
    causal = jnp.tril(jnp.ones((L, L), dtype=bool))
    seg = a_cum[..., :, None] - a_cum[..., None, :]
    decay = jnp.where(causal, jnp.exp(jnp.where(causal, seg, 0.0)), 0.0)
    cb = jnp.einsum('bclgn,bcsgn->bcgls', Cc, Bc)
    y_diag = jnp.einsum('bcgls,bgrcls,bcsgrp->bclgrp', cb, decay, x)
    decay_to_end = jnp.exp(a_cum[..., -1:] - a_cum)
    states = jnp.einsum('bclgn,bgrcl,bclgrp->bcgrpn', Bc, decay_to_end, x)
    chunk_decay = jnp.exp(a_cum[..., -1])

    def step(h, inp):
        st, dec = inp
        return h * dec[..., None, None] + st, h

    h0 = jnp.zeros((Bsz, G, R, P, N), f32)
    _, h_in = lax.scan(step, h0, (states.transpose(1, 0, 2, 3, 4, 5),
                                  chunk_decay.transpose(3, 0, 1, 2)))
    y_off = jnp.einsum('bclgn,cbgrpn,bgrcl->bclgrp', Cc, h_in, jnp.exp(a_cum))
    return (y_diag + y_off).reshape(Bsz, S, H, P)


def ssd_gmlp_mixer(h, in_w, conv_w, conv_b, dt_bias, a_log, d_skip, ssd_norm_g,
                   gmlp_ln_g, gmlp_ln_b, gmlp_ws, gmlp_bs, out_w):
    Bsz, S, _ = h.shape
    proj = h @ in_w
    cuts = np.cumsum([SSD_INNER, SSD_CONV_DIM, SSD_HEADS, GMLP_INNER]).tolist()
    z, xbc, dt_raw, u, v = jnp.split(proj, cuts, axis=-1)
    xbc = jax.nn.silu(causal_depthwise_conv(xbc, conv_w, conv_b))
    xs, Bm, Cm = jnp.split(xbc, [SSD_INNER, SSD_INNER + SSD_GROUPS * SSD_STATE], axis=-1)
    xs = xs.reshape(Bsz, S, SSD_HEADS, SSD_HEAD_DIM)
    Bm = Bm.reshape(Bsz, S, SSD_GROUPS, SSD_STATE)
    Cm = Cm.reshape(Bsz, S, SSD_GROUPS, SSD_STATE)
    dt = jax.nn.softplus(dt_raw.astype(jnp.float32) + dt_bias.astype(jnp.float32))
    A = -jnp.exp(a_log.astype(jnp.float32))
    y = ssd_chunked_scan(xs, dt, A, Bm, Cm) + d_skip.astype(jnp.float32)[:, None] * xs.astype(jnp.float32)
    y = y.reshape(Bsz, S, SSD_INNER).astype(h.dtype)
    y_a = rms_norm(y * jax.nn.silu(z), ssd_norm_g)
    u = jax.nn.gelu(u, approximate=False)
    v = layer_norm(jax.nn.gelu(v, approximate=False), gmlp_ln_g, gmlp_ln_b)
    v = v.reshape(Bsz, S // GMLP_CHUNK, GMLP_CHUNK, GMLP_GROUPS, GMLP_GROUP_DIM)
    ws = gmlp_ws * jnp.tril(jnp.ones((GMLP_CHUNK, GMLP_CHUNK), gmlp_ws.dtype))
    v = jnp.einsum('gts,bnsgc->bntgc', ws, v) + gmlp_bs.T[:, :, None]
    y_b = u * v.reshape(Bsz, S, GMLP_INNER)
    return jnp.concatenate([y_a, y_b], axis=-1) @ out_w


def stick_breaking_attention(h, qkv_w, out_w):
    Bsz, S, _ = h.shape
    qkv = (h @ qkv_w).reshape(Bsz, S, 3, SB_HEADS, SB_HEAD_DIM)
    q = qkv[:, :, 0].transpose(0, 2, 1, 3)
    k = qkv[:, :, 1].transpose(0, 2, 1, 3)
    v = qkv[:, :, 2].transpose(0, 2, 1, 3)
    nb = S // SB_BLOCK
    q_blocks = q.reshape(Bsz, SB_HEADS, nb, SB_BLOCK, SB_HEAD_DIM).transpose(2, 0, 1, 3, 4)
    key_pos = jnp.arange(S)
    scale = SB_HEAD_DIM ** -0.5

    def block(args):
        qb, start = args
        logits = jnp.einsum('bhqd,bhkd->bhqk', qb, k).astype(jnp.float32) * scale
        q_pos = start + jnp.arange(SB_BLOCK)
        mask = key_pos[None, :] < q_pos[:, None]
        log_beta = jax.nn.log_sigmoid(logits)
        log_1m_beta = jnp.where(mask, jax.nn.log_sigmoid(-logits), 0.0)
        tail = lax.cumsum(log_1m_beta, axis=3, reverse=True) - log_1m_beta
        weights = jnp.where(mask, jnp.exp(log_beta + tail), 0.0)
        return jnp.einsum('bhqk,bhkd->bhqd', weights.astype(v.dtype), v)

    starts = jnp.arange(nb, dtype=jnp.int32) * SB_BLOCK
    o = lax.map(block, (q_blocks, starts))
    o = o.transpose(1, 0, 3, 2, 4).reshape(Bsz, S, SB_HEADS * SB_HEAD_DIM)
    return o @ out_w


def peer_ffn(h, w_query, sub_keys, expert_u, expert_v):
    Bsz, S, D = h.shape
    T = Bsz * S
    hf = h.reshape(T, D)
    q = (hf @ w_query).reshape(T, PEER_HEADS, 2, PEER_HALF)
    scores = jnp.einsum('thic,hikc->thik', q, sub_keys).astype(jnp.float32)
    s, idx = lax.top_k(scores, PEER_TOPK)
    cand = s[:, :, 0, :, None] + s[:, :, 1, None, :]
    cand_idx = idx[:, :, 0, :, None] * PEER_KEYS + idx[:, :, 1, None, :]
    cand = cand.reshape(T, PEER_HEADS, PEER_TOPK * PEER_TOPK)
    cand_idx = cand_idx.reshape(T, PEER_HEADS, PEER_TOPK * PEER_TOPK)
    top_s, pos = lax.top_k(cand, PEER_TOPK)
    expert_idx = jnp.take_along_axis(cand_idx, pos, axis=-1)
    gates = jax.nn.softmax(top_s, axis=-1).astype(h.dtype)
    nblk = T // PEER_TOKEN_BLOCK
    n_sel = PEER_HEADS * PEER_TOPK

    def block(args):
        xb, eb, gb = args
        u = jnp.take(expert_u, eb, axis=0)
        act = jax.nn.gelu(jnp.einsum('tnd,td->tn', u, xb), approximate=False) * gb
        vv = jnp.take(expert_v, eb, axis=0)
        return jnp.einsum('tn,tnd->td', act, vv)

    out = lax.map(block, (hf.reshape(nblk, PEER_TOKEN_BLOCK, D),
                          expert_idx.reshape(nblk, PEER_TOKEN_BLOCK, n_sel),
                          gates.reshape(nblk, PEER_TOKEN_BLOCK, n_sel)))
    return out.reshape(Bsz, S, D)


def setup_inputs(seed: int = 0) -> dict:
    key = jax.random.key(seed)
    ks = jax.random.split(key, 26)
    f32 = jnp.float32
    D = D_MODEL

    def nrm(k, shape, scale):
        return jax.random.normal(k, shape, f32) * scale

    dt0 = jnp.exp(jax.random.uniform(ks[9], (N_EVEN, SSD_HEADS), f32,
                                     minval=math.log(1e-3), maxval=math.log(1e-1)))
    return {
        'x': nrm(ks[0], (BATCH, SEQ, D), 1.0),
        'c': nrm(ks[1], (BATCH, D), 1.0),
        'ada_w': nrm(ks[2], (DEPTH, D, 6 * D), 0.5 * D ** -0.5),
        'ada_b': nrm(ks[3], (DEPTH, 6 * D), 0.01),
        'norm_mix_g': 1.0 + nrm(ks[4], (DEPTH, D), 0.02),
        'norm_ffn_g': 1.0 + nrm(ks[5], (DEPTH, D), 0.02),
        'in0_w': nrm(ks[6], (N_EVEN, D, IN0_DIM), D ** -0.5),
        'conv_w': nrm(ks[7], (N_EVEN, SSD_CONV, SSD_CONV_DIM), SSD_CONV ** -0.5),
        'conv_b': nrm(ks[8], (N_EVEN, SSD_CONV_DIM), 0.01),
        'dt_bias': dt0 + jnp.log(-jnp.expm1(-dt0)),
        'a_log': jnp.log(jax.random.uniform(ks[10], (N_EVEN, SSD_HEADS), f32, minval=1.0, maxval=16.0)),
        'd_skip': 1.0 + nrm(ks[11], (N_EVEN, SSD_HEADS), 0.02),
        'ssd_norm_g': 1.0 + nrm(ks[12], (N_EVEN, SSD_INNER), 0.02),
        'gmlp_ln_g': 1.0 + nrm(ks[13], (N_EVEN, GMLP_INNER), 0.02),
        'gmlp_ln_b': nrm(ks[14], (N_EVEN, GMLP_INNER), 0.01),
        'gmlp_ws': nrm(ks[15], (N_EVEN, GMLP_GROUPS, GMLP_CHUNK, GMLP_CHUNK), GMLP_CHUNK ** -0.5),
        'gmlp_bs': 1.0 + nrm(ks[16], (N_EVEN, GMLP_GROUPS, GMLP_CHUNK), 0.02),
        'out0_w': nrm(ks[17], (N_EVEN, MIX0_DIM, D), MIX0_DIM ** -0.5),
        'sb_qkv_w': nrm(ks[18], (N_ODD, D, 3 * D), D ** -0.5),
        'sb_out_w': nrm(ks[19], (N_ODD, D, D), D ** -0.5),
        'peer_wq': nrm(ks[20], (DEPTH, D, PEER_HEADS * PEER_KEY_DIM), D ** -0.5),
        'peer_keys': nrm(ks[21], (DEPTH, PEER_HEADS, 2, PEER_KEYS, PEER_HALF), PEER_HALF ** -0.5),
        'peer_u': nrm(ks[22], (DEPTH, PEER_EXPERTS, D), D ** -0.5),
        'peer_v': nrm(ks[23], (DEPTH, PEER_EXPERTS, D), PEER_HEADS ** -0.5),
        'final_g': 1.0 + nrm(ks[24], (D,), 0.02),
    }


def reference(x, c, ada_w, ada_b, norm_mix_g, norm_ffn_g, in0_w, conv_w, conv_b,
              dt_bias, a_log, d_skip, ssd_norm_g, gmlp_ln_g, gmlp_ln_b, gmlp_ws,
              gmlp_bs, out0_w, sb_qkv_w, sb_out_w, peer_wq, peer_keys, peer_u,
              peer_v, final_g):
    cond = jax.nn.silu(c)
    for i in range(DEPTH):
        mod = cond @ ada_w[i] + ada_b[i]
        shift1, scale1, gate1, shift2, scale2, gate2 = jnp.split(mod, 6, axis=-1)
        h = modulate(rms_norm(x, norm_mix_g[i]), shift1, scale1)
        j = i // 2
        if i % 2 == 0:
            mix = ssd_gmlp_mixer(h, in0_w[j], conv_w[j], conv_b[j], dt_bias[j], a_log[j],
                                 d_skip[j], ssd_norm_g[j], gmlp_ln_g[j], gmlp_ln_b[j],
                                 gmlp_ws[j], gmlp_bs[j], out0_w[j])
        else:
            mix = stick_breaking_attention(h, sb_qkv_w[j], sb_out_w[j])
        x = x + gate1[:, None, :] * mix
        h = modulate(rms_norm(x, norm_ffn_g[i]), shift2, scale2)
        x = x + gate2[:, None, :] * peer_ffn(h, peer_wq[i], peer_keys[i], peer_u[i], peer_v[i])
    return rms_norm(x, final_g)
```

```python
import numpy as np
import concourse.bass as bass
import concourse.mybir as mybir
from concourse.bass_utils import run_bass_kernel_spmd

F32 = mybir.dt.float32
BF16 = mybir.dt.bfloat16
AF = mybir.ActivationFunctionType
ALU = mybir.AluOpType
AX = mybir.AxisListType

D = 2048
DC = 16
EPS = 1e-6
NEG = -1.0e30


class Res:
    __slots__ = ("w", "rs", "name")

    def __init__(self, name=""):
        self.w = None
        self.rs = {}
        self.name = name


class Prog:
    ENGS = ["pe", "act", "dve", "pool", "sp"]

    def __init__(self, nc, n_dma_sems=40):
        self.nc = nc
        self.q = {e: [] for e in self.ENGS}
        self.sem = {e: nc.alloc_semaphore(name=f"sem_{e}") for e in self.ENGS}
        self.cnt = {e: 0 for e in self.ENGS}
        self.seen = {e: {} for e in self.ENGS}
        self.dsem = [nc.alloc_semaphore(name=f"dsem{i}") for i in range(n_dma_sems)]
        self.dcnt = [0] * n_dma_sems
        self.dnext = 0
        self.ninstr = 0

    def _need(self, e, waits, tok):
        if tok is None:
            return
        sem, val = tok
        if sem.num == self.sem[e].num and e == "pe":
            return
        if self.seen[e].get(sem.num, 0) >= val:
            return
        self.seen[e][sem.num] = val
        waits.append((sem, val))

    def _deps(self, e, r, w):
        waits = []
        for res in r:
            self._need(e, waits, res.w)
        for res in w:
            self._need(e, waits, res.w)
            for t in res.rs.values():
                self._need(e, waits, t)
        return waits

    def _commit(self, tok, r, w):
        for res in r:
            res.rs[tok[0].num] = tok
        for res in w:
            res.w = tok
            res.rs = {}

    def op(self, e, fn, r=(), w=()):
        waits = self._deps(e, r, w)
        self.cnt[e] += 1
        tok = (self.sem[e], self.cnt[e])
        self.q[e].append((waits, fn, self.sem[e], 1))
        self._commit(tok, r, w)
        self.ninstr += 1 + len(waits)
        return tok

    def dma(self, e, out, in_, r=(), w=(), **kw):
        waits = self._deps(e, r, w)
        i = self.dnext
        self.dnext = (self.dnext + 1) % len(self.dsem)
        if self.dcnt[i] > 0:
            self._need(e, waits, (self.dsem[i], self.dcnt[i]))
        self.dcnt[i] += 16
        tok = (self.dsem[i], self.dcnt[i])
        self.q[e].append((waits, lambda eng: eng.dma_start(out=out, in_=in_, **kw), self.dsem[i], 16))
        self._commit(tok, r, w)
        self.ninstr += 1 + len(waits)
        return tok

    def wait(self, e, toks):
        waits = []
        for t in toks:
            self._need(e, waits, t)
        if waits:
            self.q[e].append((waits, None, None, 0))

    def barrier(self):
        toks = [(self.sem[e], self.cnt[e]) for e in self.ENGS if self.cnt[e] > 0]
        toks += [(self.dsem[i], self.dcnt[i]) for i in range(len(self.dsem)) if self.dcnt[i] > 0]
        for e in self.ENGS:
            self.wait(e, toks)

    def emit(self):
        nc = self.nc
        q = self.q

        def run(eng, items):
            for waits, fn, sem, inc in items:
                for s, v in waits:
                    eng.wait_ge(s, v)
                if fn is not None:
                    fn(eng).then_inc(sem, inc)

        with nc.Block() as block:
            @block.tensor
            def _(t):
                run(t, q["pe"])

            @block.scalar
            def _(t):
                run(t, q["act"])

            @block.vector
            def _(t):
                run(t, q["dve"])

            @block.gpsimd
            def _(t):
                run(t, q["pool"])

            @block.sync
            def _(t):
                run(t, q["sp"])


class T:
    _n = [0]

    def __init__(self, es, nc, name, shape, dtype, psum=False):
        T._n[0] += 1
        name = f"{name}_u{T._n[0]}"
        cm = nc.psum_tensor(name, shape, dtype) if psum else nc.sbuf_tensor(name, shape, dtype)
        self.t = es.enter_context(cm)
        self.r = Res(name)

    def __getitem__(self, k):
        return self.t[k]


class Ctx:
    def __init__(self, nc, es):
        self.nc = nc
        self.P = Prog(nc)
        self.h = H(self.P)
        self.ps = [T(es, nc, f"ps{i}", [128, 512], F32, psum=True) for i in range(8)]
        self.out_toks = []


def _io(nc, env, pfx):
    def din(name, shape):
        if env is not None and name in env:
            return env[name]
        return nc.dram_tensor(pfx + name, shape, F32, kind="ExternalInput").ap()

    def dout(name, shape):
        if env is not None and name in env:
            return env[name]
        return nc.dram_tensor(pfx + name, shape, F32, kind="ExternalOutput").ap()
    return din, dout


class H:
    def __init__(self, P):
        self.P = P

    def mm(self, out, lhsT, rhs, start, stop, r, w):
        return self.P.op("pe", lambda e: e.matmul(out, lhsT=lhsT, rhs=rhs, start=start, stop=stop), r, w)

    def tr(self, out, in_, ident, r, w):
        return self.P.op("pe", lambda e: e.transpose(out, in_, ident), r, w)

    def act(self, out, in_, func, r, w, bias=None, scale=None, accum=None):
        kw = {}
        if bias is not None:
            kw["bias"] = bias
        if scale is not None:
            kw["scale"] = scale
        if accum is not None:
            kw["accum_out"] = accum
        return self.P.op("act", lambda e: e.activation(out=out, in_=in_, func=func, **kw), r, w)

    def tt(self, eng, out, in0, in1, op, r, w):
        return self.P.op(eng, lambda e: e.tensor_tensor(out=out, in0=in0, in1=in1, op=op), r, w)

    def ts(self, eng, out, in0, s1, s2, op0, op1, r, w, accum=None):
        if accum is not None:
            return self.P.op(eng, lambda e: e.tensor_scalar(out=out, in0=in0, scalar1=s1, scalar2=s2, op0=op0, op1=op1, accum_out=accum), r, w)
        if op1 is None:
            return self.P.op(eng, lambda e: e.tensor_scalar(out=out, in0=in0, scalar1=s1, scalar2=None, op0=op0), r, w)
        return self.P.op(eng, lambda e: e.tensor_scalar(out=out, in0=in0, scalar1=s1, scalar2=s2, op0=op0, op1=op1), r, w)

    def stt(self, out, in0, scalar, in1, op0, op1, r, w):
        return self.P.op("dve", lambda e: e.scalar_tensor_tensor(out=out, in0=in0, scalar=scalar, in1=in1, op0=op0, op1=op1), r, w)

    def cp(self, eng, out, in_, r, w):
        if eng == "act":
            return self.P.op("act", lambda e: e.copy(out=out, in_=in_), r, w)
        return self.P.op(eng, lambda e: e.tensor_copy(out=out, in_=in_), r, w)

    def memset(self, eng, ap, val, w):
        return self.P.op(eng, lambda e: e.memset(ap, val), (), w)

    def max8(self, out, in_, r, w):
        return self.P.op("dve", lambda e: e.max(out=out, in_=in_), r, w)

    def mrep(self, out, rep, vals, r, w):
        return self.P.op("dve", lambda e: e.match_replace(out=out, in_to_replace=rep, in_values=vals, imm_value=NEG), r, w)

    def recip(self, out, in_, r, w):
        return self.P.op("dve", lambda e: e.reciprocal(out=out, in_=in_), r, w)

    def red(self, out, in_, op, r, w):
        return self.P.op("dve", lambda e: e.tensor_reduce(out=out, in_=in_, axis=AX.X, op=op), r, w)


TB = 512
NTT = TB // 128
NJ = TB // 16


def ada_compute(es0, nc, P, h, ps, cfm_d, adaw_d, adabfm_d, adab_d, fm_secs, bc_secs, tag):
    from contextlib import ExitStack
    outs = {}
    for s in fm_secs:
        outs[s] = T(es0, nc, f"ada_fm{tag}_{s}", [128, 16], F32)
    for s in bc_secs:
        outs[s] = T(es0, nc, f"ada_bc{tag}_{s}", [128, 2048], F32)
    with ExitStack() as es:
        condT = T(es, nc, f"condT{tag}", [128, 16], F32)
        condB = T(es, nc, f"condB{tag}", [128, 16, 128], F32)
        abfm = T(es, nc, f"abfm{tag}", [128, 96], F32)
        wblk = [T(es, nc, f"adaw{tag}_{i}", [128, 16, 512], F32) for i in range(2)]
        abb = [T(es, nc, f"adabb{tag}_{i}", [128, 512], F32) for i in range(2)]
        P.dma("sp", condT[:, :], cfm_d, w=[condT.r])
        P.dma("sp", abfm[:, :], adabfm_d, w=[abfm.r])
        h.act(condT[:, :], condT[:, :], AF.Silu, [condT.r], [condT.r])
        h.cp("dve", condB[:, :, :], condT[:, :].unsqueeze(2).to_broadcast([128, 16, 128]), [condT.r], [condB.r])
        it = 0
        for s in sorted(set(fm_secs) | set(bc_secs)):
            for cb in range(4):
                wb = wblk[it % 2]
                ab = abb[it % 2]
                it += 1
                c0 = s * 2048 + cb * 512
                P.dma("sp", wb[:, :, :], adaw_d[:, c0:c0 + 512].rearrange("(k p) n -> p k n", p=128), w=[wb.r])
                if s in bc_secs:
                    P.dma("sp", ab[:, :], adab_d[0:1, c0:c0 + 512].partition_broadcast(128), w=[ab.r])
                    pb = ps[it % 2]
                    for k in range(16):
                        h.mm(pb[:, :], condB[:, k, :], wb[:, k, :], k == 0, k == 15, [condB.r, wb.r], [pb.r])
                    h.tt("dve", outs[s][:, cb * 512:(cb + 1) * 512], pb[:, :], ab[:, :], ALU.add, [pb.r, ab.r], [outs[s].r])
                if s in fm_secs:
                    pf = ps[2 + (it % 2)]
                    for jj in range(4):
                        for k in range(16):
                            h.mm(pf[:, jj:jj + 1], wb[:, k, jj * 128:(jj + 1) * 128], condT[:, k:k + 1], k == 0, k == 15,
                                 [condT.r, wb.r], [pf.r])
                    j0 = cb * 4
                    h.tt("dve", outs[s][:, j0:j0 + 4], pf[:, 0:4], abfm[:, s * 16 + j0:s * 16 + j0 + 4], ALU.add,
                         [pf.r, abfm.r], [outs[s].r])
    P.barrier()
    return outs


def rms_stats(P, h, x_ap, sq_scr, ssq_ap, rstd_ap, r, scr_res, st_res, n):
    h.act(sq_scr, x_ap, AF.Square, r, [scr_res, st_res], accum=ssq_ap)
    h.ts("dve", rstd_ap, ssq_ap, 1.0 / n, EPS, ALU.mult, ALU.add, [st_res], [st_res])
    h.act(rstd_ap, rstd_ap, AF.Sqrt, [st_res], [st_res])
    h.recip(rstd_ap, rstd_ap, [st_res], [st_res])


def to_fm(P, h, ps, ident, src, src_res, ncol, dst, tt, evac):
    for q in range(ncol // 4):
        pb = ps[q % 2]
        for j in range(4):
            kc = q * 4 + j
            h.tr(pb[:, j * 128:(j + 1) * 128], src[:, kc * 128:(kc + 1) * 128], ident[:, :], [src_res, ident.r], [pb.r])
        evac(q, pb)


def build_tail(NT, KMIX, final, ya_norm, n_i1=128, ctx=None, env=None, pfx="", blend=False):
    from contextlib import ExitStack
    nc = ctx.nc if ctx else bass.Bass("TRN2", target_bir_lowering=False)
    NB = NT // TB
    KC = KMIX // 128
    GV = 4
    din, dout = _io(nc, env, pfx)
    NSRC = 2 * NT if blend else NT
    sel_d = din("sel", [128, 2]) if blend else None
    x_d = din("x", [NSRC, D])
    mix_d = din("mix", [NSRC, KMIX])
    wo_d = din("wo", [KMIX, D])
    cfm_d = din("c_fm", [128, 16])
    adaw_d = din("ada_w", [D, 6 * D])
    adabfm_d = din("ada_b_fm", [128, 96])
    adab_d = din("ada_b", [1, 6 * D])
    gffn_d = din("g_ffn_fm", [128, 16])
    wq_d = din("wq", [D, D])
    keys_d = din("keys", [16, 128, 128])
    U_d = din("U", [16384, D])
    V_d = din("V", [16384, D])
    fing_d = din("final_g", [1, D])
    gya_d = din("g_ya_fm", [128, 16])
    ident_d = din("ident", [128, 128])
    summ_d = din("summ", [128, 16])
    out_d = dout("out", [NT, D])
    sc_d = nc.dram_tensor(pfx + "sc_scr", [TB, 2048], F32, kind="Internal").ap()
    bc_d = nc.dram_tensor(pfx + "bc_scr", [2, 128, 2048], F32, kind="Internal").ap()
    scd_res = Res("sc_d")
    bcd_res = Res("bc_d")
    UT_d = nc.dram_tensor(pfx + "UT_scr", [n_i1, 128, 2048], BF16, kind="Internal").ap()
    Vb_d = nc.dram_tensor(pfx + "Vb_scr", [n_i1 * 128, D], BF16, kind="Internal").ap()

    with ExitStack() as es:
        if ctx is None:
            P = Prog(nc)
            h = H(P)
            ps = [T(es, nc, f"ps{i}", [128, 512], F32, psum=True) for i in range(8)]
        else:
            P, h, ps = ctx.P, ctx.h, ctx.ps
        ident = T(es, nc, "ident", [128, 128], F32)
        summ = T(es, nc, "summ", [128, 16], F32)
        sel = T(es, nc, "sel", [128, 2], F32)
        if blend:
            P.dma("sp", sel[:, :], sel_d, w=[sel.r])
        gffn = T(es, nc, "gffn", [128, 16], F32)
        gya = T(es, nc, "gya", [128, 16], F32)
        gs2 = T(es, nc, "gs2", [128, 16], F32)
        keysT = T(es, nc, "keysT", [128, 16, 128], F32)
        P.dma("sp", ident[:, :], ident_d, w=[ident.r])
        P.dma("sp", summ[:, :], summ_d, w=[summ.r])
        P.dma("sp", gffn[:, :], gffn_d, w=[gffn.r])
        P.dma("sp", gya[:, :], gya_d, w=[gya.r])

        sh2 = T(es, nc, "sh2", [128, 16], F32)
        with ExitStack() as es1:
            ada = ada_compute(es1, nc, P, h, ps, cfm_d, adaw_d, adabfm_d, adab_d, fm_secs=[3, 4], bc_secs=[2, 5], tag="t")
            h.cp("dve", sh2[:, :], ada[3][:, :], [ada[3].r], [sh2.r])
            h.stt(gs2[:, :], ada[4][:, :], 1.0, gffn[:, :], ALU.add, ALU.mult, [ada[4].r, gffn.r], [gs2.r])
            P.dma("sp", bc_d[0], ada[2][:, :], r=[ada[2].r], w=[bcd_res])
            P.dma("sp", bc_d[1], ada[5][:, :], r=[ada[5].r], w=[bcd_res])
            kraw = T(es1, nc, "kraw", [128, 16, 128], F32)
            P.dma("sp", kraw[:, :, :], keys_d.rearrange("a k c -> k a c"), w=[kraw.r])
            for q in range(4):
                pb = ps[4 + q % 2]
                for j in range(4):
                    h.tr(pb[:, j * 128:(j + 1) * 128], kraw[:, q * 4 + j, :], ident[:, :], [kraw.r, ident.r], [pb.r])
                h.cp("dve", keysT[:, q * 4:(q + 1) * 4, :], pb[:, :].rearrange("p (a b) -> p a b", a=4), [pb.r], [keysT.r])
            P.barrier()

        with ExitStack() as esp:
            Uraw = [T(esp, nc, f"Uraw{i}", [128, D], F32) for i in range(2)]
            utb = [T(esp, nc, f"utb{i}", [128, 16, 128], BF16) for i in range(2)]
            VR = 1024
            for r0 in range(0, n_i1 * 128, VR):
                P.dma("pool", Vb_d[r0:r0 + VR, :], V_d[r0:r0 + VR, :])
            for i1 in range(n_i1):
                u = Uraw[i1 % 2]
                ut = utb[i1 % 2]
                P.dma("sp", u[:, :], U_d[i1 * 128:(i1 + 1) * 128, :], w=[u.r])
                for q in range(4):
                    pb = ps[(i1 * 4 + q) % 4]
                    for j in range(4):
                        h.tr(pb[:, j * 128:(j + 1) * 128], u[:, (q * 4 + j) * 128:(q * 4 + j + 1) * 128], ident[:, :],
                             [u.r, ident.r], [pb.r])
                    h.cp("act" if q % 2 else "dve", ut[:, q * 4:(q + 1) * 4, :], pb[:, :].rearrange("p (a b) -> p a b", a=4),
                         [pb.r], [ut.r])
                P.dma("sp", UT_d[i1].rearrange("p (k e) -> p k e", k=16), ut[:, :, :], r=[ut.r])
            P.barrier()

        for b in range(NB):
            t0 = b * TB
            ores = Res(f"out_blk{b}")
            oblk = out_d[t0:t0 + TB, :].rearrange("(t p) d -> p t d", p=128)
            with ExitStack() as esb:
                hT = T(esb, nc, f"hT", [128, 16, TB], BF16)
                with ExitStack() as esa:
                    xb = T(esa, nc, "xb", [128, NTT, D], F32)
                    P.dma("sp", xb[:, :, :], x_d[t0:t0 + TB, :].rearrange("(t p) d -> p t d", p=128), w=[xb.r])
                    if blend:
                        with ExitStack() as esx:
                            xb2 = T(esx, nc, "xb2", [128, NTT, D], F32)
                            P.dma("sp", xb2[:, :, :], x_d[NT + t0:NT + t0 + TB, :].rearrange("(t p) d -> p t d", p=128), w=[xb2.r])
                            for tt in range(NTT):
                                h.ts("dve", xb[:, tt, :], xb[:, tt, :], sel[:, 0:1], None, ALU.mult, None, [xb.r, sel.r], [xb.r])
                                h.stt(xb[:, tt, :], xb2[:, tt, :], sel[:, 1:2], xb[:, tt, :], ALU.mult, ALU.add, [xb2.r, sel.r, xb.r], [xb.r])
                            P.barrier()
                    g1 = T(esa, nc, "g1", [128, D], F32)
                    P.dma("sp", g1[:, :], bc_d[0], r=[bcd_res], w=[g1.r])
                    with ExitStack() as esa2:
                        mixT = T(esa2, nc, "mixT", [128, KC, TB], BF16)
                        mraw = [T(esa2, nc, f"mraw{i}", [128, KMIX], F32) for i in range(2)]
                        sta = T(esa2, nc, "sta", [128, 8], F32)
                        sqa = T(esa2, nc, "sqa", [128, 2048], F32)
                        tmp = T(esa2, nc, "tmpa", [128, 512], F32)
                        wob = [T(esa2, nc, f"wob{i}", [128, KC, 512], BF16) for i in range(2)]
                        mrB = T(esa2, nc, "mrB", [128, KMIX], F32) if blend else None
                        for tt in range(NTT):
                            mr = mraw[tt % 2]
                            P.dma("sp", mr[:, :], mix_d[t0 + tt * 128:t0 + (tt + 1) * 128, :], w=[mr.r])
                            if blend:
                                P.dma("sp", mrB[:, :], mix_d[NT + t0 + tt * 128:NT + t0 + (tt + 1) * 128, :], w=[mrB.r])
                                h.ts("dve", mr[:, :], mr[:, :], sel[:, 0:1], None, ALU.mult, None, [mr.r, sel.r], [mr.r])
                                h.stt(mr[:, :], mrB[:, :], sel[:, 1:2], mr[:, :], ALU.mult, ALU.add, [mrB.r, sel.r, mr.r], [mr.r])
                            if ya_norm:
                                rms_stats(P, h, mr[:, 0:2048], sqa[:, :], sta[:, 0:1], sta[:, 1:2], [mr.r], sqa.r, sta.r, 2048)
                                h.ts("dve", mr[:, 0:2048], mr[:, 0:2048], sta[:, 1:2], None, ALU.mult, None, [mr.r, sta.r], [mr.r])

                            def evac(q, pb, tt=tt):
                                dst = mixT[:, q * 4:(q + 1) * 4, tt * 128:(tt + 1) * 128]
                                src = pb[:, :].rearrange("p (a b) -> p a b", a=4)
                                if ya_norm and q < 4:
                                    h.tt("dve", dst, src, gya[:, q * 4:(q + 1) * 4].unsqueeze(2).to_broadcast([128, 4, 128]),
                                         ALU.mult, [pb.r, gya.r], [mixT.r])
                                elif q % 2 == 0:
                                    h.cp("dve", dst, src, [pb.r], [mixT.r])
                                else:
                                    h.cp("act", dst, src, [pb.r], [mixT.r])
                            to_fm(P, h, ps, ident, mr, mr.r, KC, mixT, tt, evac)
                        for db in range(4):
                            wb = wob[db % 2]
                            P.dma("pool", wb[:, :, :], wo_d[:, db * 512:(db + 1) * 512].rearrange("(k p) n -> p k n", p=128), w=[wb.r])
                            for tt in range(NTT):
                                pb = ps[4 + (tt % 2)]
                                for kc in range(KC):
                                    h.mm(pb[:, :], mixT[:, kc, tt * 128:(tt + 1) * 128], wb[:, kc, :], kc == 0, kc == KC - 1,
                                         [mixT.r, wb.r], [pb.r])
                                h.tt("dve", tmp[:, :], pb[:, :], g1[:, db * 512:(db + 1) * 512], ALU.mult, [pb.r, g1.r], [tmp.r])
                                h.tt("dve", xb[:, tt, db * 512:(db + 1) * 512], xb[:, tt, db * 512:(db + 1) * 512], tmp[:, :], ALU.add,
                                     [tmp.r, xb.r], [xb.r])
                    P.barrier()
                    P.dma("sp", oblk, xb[:, :, :], r=[xb.r], w=[ores])
                    with ExitStack() as esn:
                        sq = T(esn, nc, "sq", [128, D], F32)
                        st = T(esn, nc, "st", [128, 8], F32)
                        for tt in range(NTT):
                            rms_stats(P, h, xb[:, tt, :], sq[:, :], st[:, 0:1], st[:, 1:2], [xb.r], sq.r, st.r, D)
                            h.ts("dve", sq[:, :], xb[:, tt, :], st[:, 1:2], None, ALU.mult, None, [xb.r, st.r], [sq.r])

                            def evac(q, pb, tt=tt):
                                for j in range(4):
                                    kc = q * 4 + j
                                    h.act(hT[:, kc, tt * 128:(tt + 1) * 128], pb[:, j * 128:(j + 1) * 128], AF.Identity,
                                          [pb.r, gs2.r, sh2.r], [hT.r], bias=sh2[:, kc:kc + 1], scale=gs2[:, kc:kc + 1])
                            to_fm(P, h, ps, ident, sq, sq.r, 16, hT, tt, evac)
                    P.barrier()
                s2 = T(esb, nc, "s2", [128, NJ, 128], F32)
                theta = T(esb, nc, "theta", [128, NJ, 128], F32)
                e1 = T(esb, nc, "e1", [128, NJ, 128], BF16)
                e2 = T(esb, nc, "e2", [128, NJ, 128], BF16)
                with ExitStack() as esc:
                    qT = T(esc, nc, "qT", [128, 16, TB], F32)
                    wqb = [T(esc, nc, f"wqb{i}", [128, 16, 128], BF16) for i in range(2)]
                    sc = [T(esc, nc, f"sc{i}", [128, 2048], F32) for i in range(2)]
                    for oc in range(16):
                        wb = wqb[oc % 2]
                        P.dma("pool", wb[:, :, :], wq_d[:, oc * 128:(oc + 1) * 128].rearrange("(k p) n -> p k n", p=128), w=[wb.r])
                        pb = ps[oc % 2]
                        for k in range(16):
                            h.mm(pb[:, :], wb[:, k, :], hT[:, k, :], k == 0, k == 15, [wb.r, hT.r], [pb.r])
                        h.cp("act" if oc % 2 else "dve", qT[:, oc, :], pb[:, :], [pb.r], [qT.r])
                    for tt in range(NTT):
                        s_ = sc[tt % 2]
                        for q4 in range(4):
                            pb = ps[2 + q4 % 2]
                            for j in range(4):
                                hi = q4 * 4 + j
                                h.mm(pb[:, j * 128:(j + 1) * 128], qT[:, hi, tt * 128:(tt + 1) * 128], keysT[:, hi, :], True, True,
                                     [qT.r, keysT.r], [pb.r])
                            h.cp("act" if q4 % 2 else "dve", s_[:, q4 * 512:(q4 + 1) * 512], pb[:, :], [pb.r], [s_.r])
                        P.dma("sp", sc_d[tt * 128:(tt + 1) * 128, :], s_[:, :], r=[s_.r], w=[scd_res])
                P.barrier()
                with ExitStack() as esk:
                    S = T(esk, nc, "S", [128, NJ, 256], F32)
                    P.dma("sp", S[:, :, :], sc_d.rearrange("t (h x) -> (t h) x", h=8).rearrange("(j p) x -> p j x", p=128),
                          r=[scd_res], w=[S.r])
                    A16 = T(esk, nc, "A16", [128, NJ, 16], F32)
                    B16 = T(esk, nc, "B16", [128, NJ, 16], F32)
                    C24 = T(esk, nc, "C24", [128, NJ, 24], F32)
                    wk = T(esk, nc, "wk", [128, 128], F32)
                    cand = T(esk, nc, "cand", [128, 256], F32)
                    cw = T(esk, nc, "cw", [128, 256], F32)
                    cw2 = T(esk, nc, "cw2", [128, 256], F32)
                    sm_ = T(esk, nc, "smalls", [128, 4, NJ], F32)
                    ce = T(esk, nc, "ce", [128, NJ, 16], F32)
                    tmpf = T(esk, nc, "tmpf", [128, NJ, 128], F32)
                    for j in range(NJ):
                        for half, dst in ((0, A16), (1, B16)):
                            src = S[:, j, half * 128:(half + 1) * 128]
                            h.max8(dst[:, j, 0:8], src, [S.r], [dst.r])
                            h.mrep(wk[:, :], dst[:, j, 0:8], src, [S.r, dst.r], [wk.r])
                            h.max8(dst[:, j, 8:16], wk[:, :], [wk.r], [dst.r])
                        h.tt("dve", cand[:, :].rearrange("p (a b) -> p a b", a=16),
                             A16[:, j, :].unsqueeze(2).to_broadcast([128, 16, 16]),
                             B16[:, j, :].unsqueeze(1).to_broadcast([128, 16, 16]), ALU.add, [A16.r, B16.r], [cand.r])
                        h.max8(C24[:, j, 0:8], cand[:, :], [cand.r], [C24.r])
                        h.mrep(cw[:, :], C24[:, j, 0:8], cand[:, :], [cand.r, C24.r], [cw.r])
                        h.max8(C24[:, j, 8:16], cw[:, :], [cw.r], [C24.r])
                        h.mrep(cw2[:, :], C24[:, j, 8:16], cw[:, :], [cw.r, C24.r], [cw2.r])
                        h.max8(C24[:, j, 16:24], cw2[:, :], [cw2.r], [C24.r])
                    mt = sm_[:, 0, :]
                    thr = sm_[:, 1, :]
                    zz = sm_[:, 2, :]
                    h.tt("dve", mt, A16[:, :, 0], B16[:, :, 0], ALU.add, [A16.r, B16.r], [sm_.r])
                    h.tt("dve", thr, C24[:, :, 15], C24[:, :, 16], ALU.add, [C24.r], [sm_.r])
                    h.ts("dve", thr, thr, 0.5, None, ALU.mult, None, [sm_.r], [sm_.r])
                    h.tt("dve", ce[:, :, :], C24[:, :, 0:16], mt.unsqueeze(2).to_broadcast([128, NJ, 16]), ALU.subtract,
                         [C24.r, sm_.r], [ce.r])
                    h.act(ce[:, :, :], ce[:, :, :], AF.Exp, [ce.r], [ce.r])
                    h.red(zz, ce[:, :, :], ALU.add, [ce.r], [sm_.r])
                    h.recip(zz, zz, [sm_.r], [sm_.r])
                    h.tt("dve", theta[:, :, :], thr.unsqueeze(2).to_broadcast([128, NJ, 128]), S[:, :, 0:128], ALU.subtract,
                         [sm_.r, S.r], [theta.r])
                    h.tt("dve", tmpf[:, :, :], S[:, :, 0:128], A16[:, :, 0:1].to_broadcast([128, NJ, 128]), ALU.subtract,
                         [S.r, A16.r], [tmpf.r])
                    h.act(e1[:, :, :], tmpf[:, :, :], AF.Exp, [tmpf.r], [e1.r])
                    h.tt("dve", tmpf[:, :, :], S[:, :, 128:256], B16[:, :, 0:1].to_broadcast([128, NJ, 128]), ALU.subtract,
                         [S.r, B16.r], [tmpf.r])
                    h.act(tmpf[:, :, :], tmpf[:, :, :], AF.Exp, [tmpf.r], [tmpf.r])
                    h.tt("dve", e2[:, :, :], tmpf[:, :, :], zz.unsqueeze(2).to_broadcast([128, NJ, 128]), ALU.mult,
                         [tmpf.r, sm_.r], [e2.r])
                    h.cp("dve", s2[:, :, :], S[:, :, 128:256], [S.r], [s2.r])
                P.barrier()
                acc = T(esb, nc, "acc", [128, NTT, D], F32)
                h.memset("pool", acc[:, :, :], 0.0, [acc.r])
                with ExitStack() as esl:
                    UT = [T(esl, nc, f"UT{i}", [128, 16, 128], BF16) for i in range(3)]
                    Vg = [T(esl, nc, f"Vg{i}", [128, GV, D], BF16) for i in range(2)]
                    GA = [T(esl, nc, f"GA{i}", [128, TB], BF16) for i in range(2)]
                    AG = [T(esl, nc, f"AG{i}", [128, GV, TB], BF16) for i in range(2)]
                    mk = T(esl, nc, "mk", [128, NJ, 128], BF16)
                    Gh = [T(esl, nc, f"Gh{i}", [128, NJ, 128], BF16) for i in range(2)]
                    sums = [T(esl, nc, f"sums{i}", [128, NJ, 16], BF16) for i in range(2)]
                    for g in range(n_i1 // GV):
                        vg = Vg[g % 2]
                        ag = AG[g % 2]
                        P.dma("sp", vg[:, :, :], Vb_d[g * GV * 128:(g + 1) * GV * 128, :].rearrange("(c p) d -> p c d", p=128), w=[vg.r])
                        for c in range(GV):
                            i1 = g * GV + c
                            ut = UT[i1 % 3]
                            P.dma("sp", ut[:, :, :], UT_d[i1].rearrange("p (k e) -> p k e", k=16), w=[ut.r])
                            pa = ps[2 + i1 % 2]
                            for k in range(16):
                                h.mm(pa[:, :], ut[:, k, :], hT[:, k, :], k == 0, k == 15, [ut.r, hT.r], [pa.r])
                            ga = GA[i1 % 2]
                            h.act(ga[:, :], pa[:, :], AF.Gelu, [pa.r], [ga.r])
                            gh = Gh[i1 % 2]
                            sm = sums[i1 % 2]
                            h.tt("dve", mk[:, :, :], s2[:, :, :], theta[:, :, i1:i1 + 1].to_broadcast([128, NJ, 128]), ALU.is_ge,
                                 [s2.r, theta.r], [mk.r])
                            h.tt("pool", gh[:, :, :], mk[:, :, :], e2[:, :, :], ALU.mult, [mk.r, e2.r], [gh.r])
                            h.tt("pool", sm[:, :, :], summ[:, :].unsqueeze(1).to_broadcast([128, NJ, 16]),
                                 e1[:, :, i1:i1 + 1].to_broadcast([128, NJ, 16]), ALU.mult, [summ.r, e1.r], [sm.r])
                            pg = ps[4 + i1 % 2]
                            for j in range(NJ):
                                h.mm(pg[:, j * 16:(j + 1) * 16], gh[:, j, :], sm[:, j, :], True, True, [gh.r, sm.r], [pg.r])
                            h.tt("dve", ag[:, c, :], ga[:, :], pg[:, :], ALU.mult, [ga.r, pg.r], [ag.r])
                        for tt in range(NTT):
                            for db in range(4):
                                po = [ps[6], ps[7], ps[0], ps[1]][(tt * 4 + db) % 4]
                                for c in range(GV):
                                    h.mm(po[:, :], ag[:, c, tt * 128:(tt + 1) * 128], vg[:, c, db * 512:(db + 1) * 512], c == 0, c == GV - 1,
                                         [ag.r, vg.r], [po.r])
                                h.tt("dve", acc[:, tt, db * 512:(db + 1) * 512], acc[:, tt, db * 512:(db + 1) * 512], po[:, :], ALU.add,
                                     [po.r, acc.r], [acc.r])
                P.barrier()
                with ExitStack() as esf:
                    xb = T(esf, nc, "xbf", [128, NTT, D], F32)
                    g2 = T(esf, nc, "g2", [128, D], F32)
                    fg = T(esf, nc, "fg", [128, D], F32)
                    sq = T(esf, nc, "sqf", [128, D], F32)
                    st = T(esf, nc, "stf", [128, 8], F32)
                    P.dma("sp", xb[:, :, :], oblk, r=[ores], w=[xb.r])
                    P.dma("sp", g2[:, :], bc_d[1], r=[bcd_res], w=[g2.r])
                    if final:
                        P.dma("sp", fg[:, :], fing_d[0:1, :].partition_broadcast(128), w=[fg.r])
                    for tt in range(NTT):
                        h.tt("dve", acc[:, tt, :], acc[:, tt, :], g2[:, :], ALU.mult, [acc.r, g2.r], [acc.r])
                        h.tt("pool", xb[:, tt, :], xb[:, tt, :], acc[:, tt, :], ALU.add, [acc.r, xb.r], [xb.r])
                        if final:
                            rms_stats(P, h, xb[:, tt, :], sq[:, :], st[:, 0:1], st[:, 1:2], [xb.r], sq.r, st.r, D)
                            h.ts("dve", xb[:, tt, :], xb[:, tt, :], st[:, 1:2], None, ALU.mult, None, [xb.r, st.r], [xb.r])
                            h.tt("pool", xb[:, tt, :], xb[:, tt, :], fg[:, :], ALU.mult, [xb.r, fg.r], [xb.r])
                    tk = P.dma("sp", oblk, xb[:, :, :], r=[xb.r], w=[ores])
                    P.wait("sp", [tk])
                P.barrier()
        if ctx is None:
            P.emit()
        print("tail instrs", P.ninstr, {e: len(P.q[e]) for e in P.ENGS})
    return nc


def build_attn(S_LEN=4096, NH=8, HG=4, ctx=None, env=None, pfx="", tokmajor=False):
    from contextlib import ExitStack
    nc = ctx.nc if ctx else bass.Bass("TRN2", target_bir_lowering=False)
    NBK = S_LEN // TB
    NKT = S_LEN // 128
    din, dout = _io(nc, env, pfx)

    x_d = din("x", [S_LEN, D])
    cfm_d = din("c_fm", [128, 16])
    adaw_d = din("ada_w", [D, 6 * D])
    adabfm_d = din("ada_b_fm", [128, 96])
    adab_d = din("ada_b", [1, 6 * D])
    gmix_d = din("g_mix_fm", [128, 16])
    wqkv_d = din("wqkv", [D, 3 * NH * 128])
    ident_d = din("ident", [128, 128])
    masks_d = din("masks", [128, 4, 512])
    ntri_d = din("ntri", [128, 128])
    if tokmajor:
        o_d = dout("o", [S_LEN, NH * 128])
    else:
        oT_d = dout("oT", [NH * 128, S_LEN])
    scale = 128.0 ** -0.5

    with ExitStack() as es:
        if ctx is None:
            P = Prog(nc)
            h = H(P)
            ps = [T(es, nc, f"ps{i}", [128, 512], F32, psum=True) for i in range(8)]
        else:
            P, h, ps = ctx.P, ctx.h, ctx.ps
        ident = T(es, nc, "ident", [128, 128], F32)
        masks = T(es, nc, "masks", [128, 4, 512], F32)
        ntri = T(es, nc, "ntri", [128, 128], F32)
        nones = T(es, nc, "nones", [128, 128], F32)
        gmix = T(es, nc, "gmix", [128, 16], F32)
        gs1 = T(es, nc, "gs1", [128, 16], F32)
        sh1 = T(es, nc, "sh1", [128, 16], F32)
        P.dma("sp", ident[:, :], ident_d, w=[ident.r])
        P.dma("sp", masks[:, :, :], masks_d, w=[masks.r])
        P.dma("sp", ntri[:, :], ntri_d, w=[ntri.r])
        P.dma("sp", gmix[:, :], gmix_d, w=[gmix.r])
        h.memset("pool", nones[:, :], -1.0, [nones.r])
        with ExitStack() as es1:
            ada = ada_compute(es1, nc, P, h, ps, cfm_d, adaw_d, adabfm_d, adab_d, fm_secs=[0, 1], bc_secs=[], tag="a")
            h.cp("dve", sh1[:, :], ada[0][:, :], [ada[0].r], [sh1.r])
            h.stt(gs1[:, :], ada[1][:, :], 1.0, gmix[:, :], ALU.add, ALU.mult, [ada[1].r, gmix.r], [gs1.r])
            P.barrier()
        out_toks = []
        for hg in range(NH // HG):
            with ExitStack() as esg:
                QT = T(esg, nc, "QT", [128, HG, S_LEN], BF16)
                KT = T(esg, nc, "KT", [128, HG, S_LEN], BF16)
                Vt = T(esg, nc, "Vt", [128, NKT, HG * 128], BF16)
                with ExitStack() as e1:
                    xb = T(e1, nc, "xb", [128, NTT, D], F32)
                    hT = T(e1, nc, "hT", [128, 16, TB], BF16)
                    sq = T(e1, nc, "sq", [128, D], F32)
                    st = T(e1, nc, "st", [128, 8], F32)
                    wp = [T(e1, nc, f"wp{i}", [128, 16, 128], BF16) for i in range(2)]
                    wv = T(e1, nc, "wv", [128, 16, HG * 128], BF16)
                    c0v = 2 * NH * 128 + hg * HG * 128
                    P.dma("pool", wv[:, :, :], wqkv_d[:, c0v:c0v + HG * 128].rearrange("(k p) n -> p k n", p=128), w=[wv.r])
                    wi = 0
                    for b in range(NBK):
                        t0 = b * TB
                        P.dma("sp", xb[:, :, :], x_d[t0:t0 + TB, :].rearrange("(t p) d -> p t d", p=128), w=[xb.r])
                        for tt in range(NTT):
                            rms_stats(P, h, xb[:, tt, :], sq[:, :], st[:, 0:1], st[:, 1:2], [xb.r], sq.r, st.r, D)
                            h.ts("dve", sq[:, :], xb[:, tt, :], st[:, 1:2], None, ALU.mult, None, [xb.r, st.r], [sq.r])

                            def evac(q, pb, tt=tt):
                                for j in range(4):
                                    kc = q * 4 + j
                                    h.act(hT[:, kc, tt * 128:(tt + 1) * 128], pb[:, j * 128:(j + 1) * 128], AF.Identity,
                                          [pb.r, gs1.r, sh1.r], [hT.r], bias=sh1[:, kc:kc + 1], scale=gs1[:, kc:kc + 1])
                            to_fm(P, h, ps, ident, sq, sq.r, 16, hT, tt, evac)
                        for hl in range(HG):
                            for which, dst in ((0, QT), (1, KT)):
                                wb = wp[wi % 2]
                                wi += 1
                                c0 = which * NH * 128 + (hg * HG + hl) * 128
                                P.dma("pool", wb[:, :, :], wqkv_d[:, c0:c0 + 128].rearrange("(k p) n -> p k n", p=128), w=[wb.r])
                                pb = ps[2 + wi % 2]
                                for k in range(16):
                                    h.mm(pb[:, :], wb[:, k, :], hT[:, k, :], k == 0, k == 15, [wb.r, hT.r], [pb.r])
                                if which == 0:
                                    h.act(dst[:, hl, t0:t0 + TB], pb[:, :], AF.Copy, [pb.r], [dst.r], scale=scale)
                                else:
                                    h.cp("dve", dst[:, hl, t0:t0 + TB], pb[:, :], [pb.r], [dst.r])
                        for tt in range(NTT):
                            pb = ps[4 + tt % 2]
                            for k in range(16):
                                h.mm(pb[:, 0:HG * 128], hT[:, k, tt * 128:(tt + 1) * 128], wv[:, k, :], k == 0, k == 15, [hT.r, wv.r], [pb.r])
                            h.cp("dve" if tt % 2 else "act", Vt[:, b * NTT + tt, :], pb[:, 0:HG * 128], [pb.r], [Vt.r])
                P.barrier()
                with ExitStack() as e2:
                    Racc = T(e2, nc, "Racc", [128, 512], F32)
                    ex = [T(e2, nc, f"ex{i}", [128, 512], F32) for i in range(2)]
                    spb = [T(e2, nc, f"sp{i}", [128, 512], F32) for i in range(2)]
                    Wt = [T(e2, nc, f"Wt{i}", [128, 512], BF16) for i in range(2)]
                    osb = [T(e2, nc, f"osb{i}", [128, 512], F32) for i in range(2)]
                    osT = [T(e2, nc, f"osT{i}", [128, 512], F32) for i in range(2)]
                    it = 0
                    for hl in range(HG):
                        for qb in range(NBK):
                            po = ps[6 + qb % 2]
                            q_ap = QT[:, hl, qb * 512:(qb + 1) * 512]
                            nk = 4 * qb + 4
                            for idx in range(nk):
                                kt = nk - 1 - idx
                                jd = kt - 4 * qb
                                k_ap = KT[:, hl, kt * 128:(kt + 1) * 128]
                                pL = ps[it % 2]
                                pE = ps[2 + it % 2]
                                e_ = ex[it % 2]
                                s_ = spb[it % 2]
                                w_ = Wt[it % 2]
                                it += 1
                                h.mm(pL[:, :], k_ap, q_ap, True, True, [KT.r, QT.r], [pL.r])
                                h.act(e_[:, :], pL[:, :], AF.Exp, [pL.r], [e_.r])
                                h.act(s_[:, :], e_[:, :], AF.Ln, [e_.r], [s_.r], bias=1.0)
                                if jd >= 0:
                                    h.tt("dve", s_[:, :], s_[:, :], masks[:, jd, :], ALU.mult, [s_.r, masks.r], [s_.r])
                                h.mm(pE[:, :], k_ap, q_ap, True, False, [KT.r, QT.r], [pE.r])
                                h.mm(pE[:, :], ntri[:, :], s_[:, :], False, idx == 0, [ntri.r, s_.r], [pE.r])
                                if idx > 0:
                                    h.mm(pE[:, :], nones[:, :], Racc[:, :], False, True, [nones.r, Racc.r], [pE.r])
                                h.act(w_[:, :], pE[:, :], AF.Exp, [pE.r], [w_.r])
                                if jd >= 0:
                                    h.tt("dve", w_[:, :], w_[:, :], masks[:, jd, :], ALU.mult, [w_.r, masks.r], [w_.r])
                                if idx == 0:
                                    h.cp("pool", Racc[:, :], s_[:, :], [s_.r], [Racc.r])
                                elif idx < nk - 1:
                                    h.tt("pool", Racc[:, :], Racc[:, :], s_[:, :], ALU.add, [s_.r, Racc.r], [Racc.r])
                                h.mm(po[:, :], Vt[:, kt, hl * 128:(hl + 1) * 128], w_[:, :], idx == 0, idx == nk - 1, [Vt.r, w_.r], [po.r])
                            ob = osb[qb % 2]
                            h.cp("dve", ob[:, :], po[:, :], [po.r], [ob.r])
                            hh = hg * HG + hl
                            if tokmajor:
                                pt = ps[4 + qb % 2]
                                for tt in range(4):
                                    h.tr(pt[:, tt * 128:(tt + 1) * 128], ob[:, tt * 128:(tt + 1) * 128], ident[:, :], [ob.r, ident.r], [pt.r])
                                ot = osT[qb % 2]
                                h.cp("act", ot[:, :], pt[:, :], [pt.r], [ot.r])
                                out_toks.append(P.dma("sp", o_d[qb * 512:(qb + 1) * 512, hh * 128:(hh + 1) * 128].rearrange("(t p) d -> p t d", p=128),
                                                      ot[:, :].rearrange("p (t d) -> p t d", t=4), r=[ot.r]))
                            else:
                                out_toks.append(P.dma("sp", oT_d[hh * 128:(hh + 1) * 128, qb * 512:(qb + 1) * 512], ob[:, :], r=[ob.r]))
                P.barrier()
        P.wait("sp", out_toks[-48:])
        if ctx is None:
            P.emit()
        print("attn instrs", P.ninstr, {e: len(P.q[e]) for e in P.ENGS})
    return nc


def build_mix0(S_LEN=4096, NHD=16, gm_nblk=4, ctx=None, env=None, pfx="", ygcol=0, ygw=None):
    from contextlib import ExitStack
    nc = ctx.nc if ctx else bass.Bass("TRN2", target_bir_lowering=False)
    din, dout = _io(nc, env, pfx)
    NBK = S_LEN // TB
    NG = NHD // 4
    NX = NHD * 64
    NXC = NX // 128
    NCH = NXC + 2 * NG
    WSSD = 2 * NX + 2 * NG * 128 + NHD

    x_d = din("x", [S_LEN, D])
    xgm_d = din("x_gm", [gm_nblk * TB, D]) if gm_nblk else None
    cfm_d = din("c_fm", [128, 16])
    adaw_d = din("ada_w", [D, 6 * D])
    adabfm_d = din("ada_b_fm", [128, 96])
    adab_d = din("ada_b", [1, 6 * D])
    gmix_d = din("g_mix_fm", [128, 16])
    wssd_d = din("w_ssd", [D, WSSD])
    convw_d = din("conv_w_fm", [128, NCH, 4])
    convb_d = din("conv_b_fm", [128, NCH])
    dtb_d = din("dt_bias", [1, NHD])
    alog_d = din("a_log", [1, NHD])
    dsk_d = din("d_skip", [1, NHD])
    wuv_d = din("w_uv", [D, 4096])
    lng_d = din("ln_g", [1, 2048])
    lnb_d = din("ln_b", [1, 2048])
    wsT_d = din("wsT", [128, 16, 128])
    bsT_d = din("bsT", [128, 16])
    ident_d = din("ident", [128, 128])
    ut_d = din("ut", [128, 128])
    slt_d = din("slt", [128, 128])
    yg_d = dout("yg", [S_LEN, NX])
    yb_d = dout("yb", [gm_nblk * TB, 2048]) if gm_nblk else None

    with ExitStack() as es:
        if ctx is None:
            P = Prog(nc)
            h = H(P)
            ps = [T(es, nc, f"ps{i}", [128, 512], F32, psum=True) for i in range(8)]
        else:
            P, h, ps = ctx.P, ctx.h, ctx.ps

        def cst(name, shape, src, dt=F32):
            t = T(es, nc, name, shape, dt)
            P.dma("sp", t[tuple(slice(None) for _ in shape)], src, w=[t.r])
            return t
        ident = cst("ident", [128, 128], ident_d)
        ut = cst("ut", [128, 128], ut_d)
        slt = cst("slt", [128, 128], slt_d)
        gmix = cst("gmix", [128, 16], gmix_d)
        convw = cst("convw", [128, NCH, 4], convw_d)
        convb = cst("convb", [128, NCH], convb_d)
        dtb = cst("dtb", [128, NHD], dtb_d[0:1, :].partition_broadcast(128))
        aneg = cst("aneg", [128, NHD], alog_d[0:1, :].partition_broadcast(128))
        dsk = cst("dsk", [128, NHD], dsk_d[0:1, :].partition_broadcast(128))
        bsT = cst("bsT", [128, 16], bsT_d)
        wsT = T(es, nc, "wsT", [128, 16, 128], BF16)
        ones = T(es, nc, "ones", [128, 128], F32)
        gs1 = T(es, nc, "gs1", [128, 16], F32)
        sh1 = T(es, nc, "sh1", [128, 16], F32)
        halo = T(es, nc, "halo", [128, NCH, 3], F32)
        Hs = T(es, nc, "Hs", [128, NX], F32)
        Hb = T(es, nc, "Hb", [128, NX], BF16)
        h.memset("pool", ones[:, :], 1.0, [ones.r])
        h.memset("pool", halo[:, :, :], 0.0, [halo.r])
        h.memset("pool", Hs[:, :], 0.0, [Hs.r])
        h.memset("pool", Hb[:, :], 0.0, [Hb.r])
        h.act(aneg[:, :], aneg[:, :], AF.Exp, [aneg.r], [aneg.r])
        h.ts("dve", aneg[:, :], aneg[:, :], -1.0, None, ALU.mult, None, [aneg.r], [aneg.r])
        with ExitStack() as es1:
            wraw = T(es1, nc, "wsraw", [128, 16, 128], F32)
            P.dma("sp", wraw[:, :, :], wsT_d, w=[wraw.r])
            h.tt("dve", wsT[:, :, :], wraw[:, :, :], ut[:, :].unsqueeze(1).to_broadcast([128, 16, 128]), ALU.mult,
                 [wraw.r, ut.r], [wsT.r])
            ada = ada_compute(es1, nc, P, h, ps, cfm_d, adaw_d, adabfm_d, adab_d, fm_secs=[0, 1], bc_secs=[], tag="m")
            h.cp("dve", sh1[:, :], ada[0][:, :], [ada[0].r], [sh1.r])
            h.stt(gs1[:, :], ada[1][:, :], 1.0, gmix[:, :], ALU.add, ALU.mult, [ada[1].r, gmix.r], [gs1.r])
            P.barrier()
        out_toks = []
        for kind, b in [("ssd", i) for i in range(NBK)] + [("gm", i) for i in range(gm_nblk)]:
            t0 = b * TB
            xsrc = x_d if kind == "ssd" else xgm_d
            with ExitStack() as esb:
                hT = T(esb, nc, "hT", [128, 16, TB], BF16)
                with ExitStack() as e1:
                    xb = T(e1, nc, "xb", [128, NTT, D], F32)
                    sq = T(e1, nc, "sq", [128, D], F32)
                    st = T(e1, nc, "st", [128, 8], F32)
                    P.dma("sp", xb[:, :, :], xsrc[t0:t0 + TB, :].rearrange("(t p) d -> p t d", p=128), w=[xb.r])
                    for tt in range(NTT):
                        rms_stats(P, h, xb[:, tt, :], sq[:, :], st[:, 0:1], st[:, 1:2], [xb.r], sq.r, st.r, D)
                        h.ts("dve", sq[:, :], xb[:, tt, :], st[:, 1:2], None, ALU.mult, None, [xb.r, st.r], [sq.r])

                        def evac(q, pb, tt=tt):
                            for j in range(4):
                                kc = q * 4 + j
                                h.act(hT[:, kc, tt * 128:(tt + 1) * 128], pb[:, j * 128:(j + 1) * 128], AF.Identity,
                                      [pb.r, gs1.r, sh1.r], [hT.r], bias=sh1[:, kc:kc + 1], scale=gs1[:, kc:kc + 1])
                        to_fm(P, h, ps, ident, sq, sq.r, 16, hT, tt, evac)
                    P.barrier()
                if kind == "ssd":
                    with ExitStack() as e2:
                        zs = T(e2, nc, "zs", [128, NTT, NX], F32)
                        xcf = T(e2, nc, "xcf", [128, NXC, TB], F32)
                        BCb = T(e2, nc, "BCb", [128, 2 * NG, TB], BF16)
                        Bf = T(e2, nc, "Bf", [128, NG, TB], F32)
                        xtok = T(e2, nc, "xtok", [128, NTT, NX], F32)
                        Btok = T(e2, nc, "Btok", [128, NTT, NG * 128], BF16)
                        dt = T(e2, nc, "dt", [128, NTT, NHD], F32)
                        with ExitStack() as e3:
                            wst = [T(e3, nc, f"wst{i}", [128, 16, 256], BF16) for i in range(2)]
                            wdt = T(e3, nc, "wdt", [128, 16, NHD], BF16)
                            raw = [T(e3, nc, f"raw{i}", [128, 3 + TB], F32) for i in range(2)]
                            cacc = [T(e3, nc, f"cacc{i}", [128, TB], F32) for i in range(2)]
                            wi = 0
                            for gz in range(NX // 256):
                                wb = wst[wi % 2]
                                wi += 1
                                P.dma("pool", wb[:, :, :], wssd_d[:, gz * 256:(gz + 1) * 256].rearrange("(k p) n -> p k n", p=128), w=[wb.r])
                                for tt in range(NTT):
                                    pb = ps[2 + tt % 2]
                                    for k in range(16):
                                        h.mm(pb[:, 0:256], hT[:, k, tt * 128:(tt + 1) * 128], wb[:, k, :], k == 0, k == 15, [hT.r, wb.r], [pb.r])
                                    h.act(zs[:, tt, gz * 256:(gz + 1) * 256], pb[:, 0:256], AF.Silu, [pb.r], [zs.r])
                            for gx in range(NCH // 2):
                                wb = wst[wi % 2]
                                wi += 1
                                c0 = NX + gx * 256
                                P.dma("pool", wb[:, :, :], wssd_d[:, c0:c0 + 256].rearrange("(k p) n -> p k n", p=128), w=[wb.r])
                                for half in range(2):
                                    cc = gx * 2 + half
                                    pb = ps[4 + cc % 2]
                                    rw = raw[cc % 2]
                                    ca = cacc[cc % 2]
                                    for k in range(16):
                                        h.mm(pb[:, :], wb[:, k, half * 128:(half + 1) * 128], hT[:, k, :], k == 0, k == 15, [wb.r, hT.r], [pb.r])
                                    h.cp("pool", rw[:, 0:3], halo[:, cc, :], [halo.r], [rw.r])
                                    h.cp("act", rw[:, 3:3 + TB], pb[:, :], [pb.r], [rw.r])
                                    h.cp("pool", halo[:, cc, :], rw[:, TB:TB + 3], [rw.r], [halo.r])
                                    h.ts("dve", ca[:, :], rw[:, 0:TB], convw[:, cc, 0:1], None, ALU.mult, None, [rw.r, convw.r], [ca.r])
                                    for k in range(1, 4):
                                        h.stt(ca[:, :], rw[:, k:k + TB], convw[:, cc, k:k + 1], ca[:, :], ALU.mult, ALU.add,
                                              [rw.r, convw.r, ca.r], [ca.r])
                                    if cc < NXC:
                                        h.act(xcf[:, cc, :], ca[:, :], AF.Silu, [ca.r, convb.r], [xcf.r], bias=convb[:, cc:cc + 1])
                                    else:
                                        h.act(BCb[:, cc - NXC, :], ca[:, :], AF.Silu, [ca.r, convb.r], [BCb.r], bias=convb[:, cc:cc + 1])
                                        if cc - NXC < NG:
                                            h.act(Bf[:, cc - NXC, :], ca[:, :], AF.Silu, [ca.r, convb.r], [Bf.r], bias=convb[:, cc:cc + 1])
                            P.dma("pool", wdt[:, :, :], wssd_d[:, WSSD - NHD:WSSD].rearrange("(k p) n -> p k n", p=128), w=[wdt.r])
                            for tt in range(NTT):
                                pb = ps[6 + tt % 2]
                                for k in range(16):
                                    h.mm(pb[:, 0:NHD], hT[:, k, tt * 128:(tt + 1) * 128], wdt[:, k, :], k == 0, k == 15, [hT.r, wdt.r], [pb.r])
                                h.tt("dve", dt[:, tt, :], pb[:, 0:NHD], dtb[:, :], ALU.add, [pb.r, dtb.r], [dt.r])
                            h.act(dt[:, :, :], dt[:, :, :], AF.Exp, [dt.r], [dt.r])
                            h.act(dt[:, :, :], dt[:, :, :], AF.Ln, [dt.r], [dt.r], bias=1.0)
                            for tt in range(NTT):
                                for q in range(NXC // 4):
                                    pb = ps[q % 2]
                                    for j in range(4):
                                        h.tr(pb[:, j * 128:(j + 1) * 128], xcf[:, q * 4 + j, tt * 128:(tt + 1) * 128], ident[:, :],
                                             [xcf.r, ident.r], [pb.r])
                                    h.cp("dve" if q % 2 else "act", xtok[:, tt, q * 512:(q + 1) * 512], pb[:, :], [pb.r], [xtok.r])
                                pb = ps[2 + tt % 2]
                                for g in range(NG):
                                    h.tr(pb[:, g * 128:(g + 1) * 128], Bf[:, g, tt * 128:(tt + 1) * 128], ident[:, :], [Bf.r, ident.r], [pb.r])
                                h.cp("dve", Btok[:, tt, :], pb[:, 0:NG * 128], [pb.r], [Btok.r])
                        P.barrier()
                        with ExitStack() as e4:
                            a_sb = T(e4, nc, "a_sb", [128, NHD], F32)
                            acs = T(e4, nc, "acs", [128, 4, NHD], F32)
                            CBm = T(e4, nc, "CBm", [128, NG, 128], F32)
                            aU = [T(e4, nc, f"aU{i}", [128, 128], F32) for i in range(4)]
                            Eq = [T(e4, nc, f"Eq{i}", [128, 4, 128], F32) for i in range(2)]
                            Mq = [T(e4, nc, f"Mq{i}", [128, 4, 128], BF16) for i in range(2)]
                            xdt = T(e4, nc, "xdt", [128, NX], BF16)
                            xs = T(e4, nc, "xs", [128, NX], BF16)
                            t1 = T(e4, nc, "t1", [128, NX], F32)
                            t3 = T(e4, nc, "t3", [128, NX], F32)
                            yo = [T(e4, nc, f"yo{i}", [128, NX], F32) for i in range(2)]
                            pA, pCB = ps[2], ps[3]
                            pYd = [ps[0], ps[1]]
                            pYo = [ps[6], ps[7]]
                            for tt in range(NTT):
                                cols = slice(tt * 128, (tt + 1) * 128)
                                h.tt("dve", a_sb[:, :], dt[:, tt, :], aneg[:, :], ALU.mult, [dt.r, aneg.r], [a_sb.r])
                                h.mm(pA[:, 0:NHD], ut[:, :], a_sb[:, :], True, True, [ut.r, a_sb.r], [pA.r])
                                h.mm(pA[:, 32:32 + NHD], ones[:, :], a_sb[:, :], True, True, [ones.r, a_sb.r], [pA.r])
                                h.cp("dve", acs[:, 0, :], pA[:, 0:NHD], [pA.r], [acs.r])
                                h.act(acs[:, 1, :], pA[:, 0:NHD], AF.Exp, [pA.r], [acs.r])
                                h.act(acs[:, 2, :], pA[:, 32:32 + NHD], AF.Exp, [pA.r], [acs.r])
                                h.tt("dve", acs[:, 3, :], pA[:, 32:32 + NHD], acs[:, 0, :], ALU.subtract, [pA.r, acs.r], [acs.r])
                                h.act(acs[:, 3, :], acs[:, 3, :], AF.Exp, [acs.r], [acs.r])
                                h.tt("dve", acs[:, 3, :], acs[:, 3, :], dt[:, tt, :], ALU.mult, [acs.r, dt.r], [acs.r])
                                for g in range(NG):
                                    h.mm(pCB[:, g * 128:(g + 1) * 128], BCb[:, g, cols], BCb[:, NG + g, cols], True, True, [BCb.r], [pCB.r])
                                h.tt("dve", CBm[:, :, :], pCB[:, 0:NG * 128].rearrange("p (g l) -> p g l", g=NG),
                                     ut[:, :].unsqueeze(1).to_broadcast([128, NG, 128]), ALU.mult, [pCB.r, ut.r], [CBm.r])
                                h.tt("dve", xdt[:, :].rearrange("p (a b) -> p a b", a=NHD), xtok[:, tt, :].rearrange("p (a b) -> p a b", a=NHD),
                                     dt[:, tt, :].unsqueeze(2).to_broadcast([128, NHD, 64]), ALU.mult, [xtok.r, dt.r], [xdt.r])
                                h.tt("pool", xs[:, :].rearrange("p (a b) -> p a b", a=NHD), xtok[:, tt, :].rearrange("p (a b) -> p a b", a=NHD),
                                     acs[:, 3, :].unsqueeze(2).to_broadcast([128, NHD, 64]), ALU.mult, [xtok.r, acs.r], [xs.r])
                                for g in range(NG):
                                    pS = ps[4 + g % 2]
                                    E_ = Eq[g % 2]
                                    M_ = Mq[g % 2]
                                    for r in range(4):
                                        hd = g * 4 + r
                                        au = aU[r]
                                        h.ts("dve", au[:, :], ut[:, :], a_sb[:, hd:hd + 1], None, ALU.mult, None, [ut.r, a_sb.r], [au.r])
                                        h.mm(pS[:, r * 128:(r + 1) * 128], slt[:, :], au[:, :], True, True, [slt.r, au.r], [pS.r])
                                    h.act(E_[:, :, :], pS[:, :].rearrange("p (a b) -> p a b", a=4), AF.Exp, [pS.r], [E_.r])
                                    h.tt("dve", M_[:, :, :], E_[:, :, :], CBm[:, g, :].unsqueeze(1).to_broadcast([128, 4, 128]), ALU.mult,
                                         [E_.r, CBm.r], [M_.r])
                                    for r in range(4):
                                        hd = g * 4 + r
                                        bank, c_ = hd // 8, (hd % 8) * 64
                                        h.mm(pYd[bank][:, c_:c_ + 64], M_[:, r, :], xdt[:, hd * 64:(hd + 1) * 64], True, True,
                                             [M_.r, xdt.r], [pYd[bank].r])
                                        h.mm(pYo[bank][:, c_:c_ + 64], BCb[:, NG + g, cols], Hb[:, hd * 64:(hd + 1) * 64], True, True,
                                             [BCb.r, Hb.r], [pYo[bank].r])
                                y_ = yo[tt % 2]
                                for bank in range(NHD // 8):
                                    cs = slice(bank * 512, (bank + 1) * 512)
                                    hs = slice(bank * 8, (bank + 1) * 8)
                                    h.tt("dve", t1[:, cs].rearrange("p (a b) -> p a b", a=8), pYo[bank][:, :].rearrange("p (a b) -> p a b", a=8),
                                         acs[:, 1, hs].unsqueeze(2).to_broadcast([128, 8, 64]), ALU.mult, [pYo[bank].r, acs.r], [t1.r])
                                    h.tt("dve", t1[:, cs], t1[:, cs], pYd[bank][:, :], ALU.add, [t1.r, pYd[bank].r], [t1.r])
                                    h.tt("pool", t3[:, cs].rearrange("p (a b) -> p a b", a=8), xtok[:, tt, cs].rearrange("p (a b) -> p a b", a=8),
                                         dsk[:, hs].unsqueeze(2).to_broadcast([128, 8, 64]), ALU.mult, [xtok.r, dsk.r], [t3.r])
                                    h.tt("pool", t3[:, cs], t3[:, cs], t1[:, cs], ALU.add, [t1.r, t3.r], [t3.r])
                                    h.tt("pool", y_[:, cs], t3[:, cs], zs[:, tt, cs], ALU.mult, [t3.r, zs.r], [y_.r])
                                out_toks.append(P.dma("sp", yg_d[t0 + tt * 128:t0 + (tt + 1) * 128, :], y_[:, :], r=[y_.r]))
                                for hd in range(NHD):
                                    g = hd // 4
                                    bank, c_ = hd // 8, (hd % 8) * 64
                                    h.mm(pYd[bank][:, c_:c_ + 64], Btok[:, tt, g * 128:(g + 1) * 128], xs[:, hd * 64:(hd + 1) * 64], True, True,
                                         [Btok.r, xs.r], [pYd[bank].r])
                                h.tt("dve", Hs[:, :].rearrange("p (a b) -> p a b", a=NHD), Hs[:, :].rearrange("p (a b) -> p a b", a=NHD),
                                     acs[:, 2, :].unsqueeze(2).to_broadcast([128, NHD, 64]), ALU.mult, [Hs.r, acs.r], [Hs.r])
                                for bank in range(NHD // 8):
                                    cs = slice(bank * 512, (bank + 1) * 512)
                                    h.tt("dve", Hs[:, cs], Hs[:, cs], pYd[bank][:, :], ALU.add, [Hs.r, pYd[bank].r], [Hs.r])
                                h.cp("act", Hb[:, :], Hs[:, :], [Hs.r], [Hb.r])
                        P.barrier()
                if kind == "gm":
                    r0 = b * TB
                    with ExitStack() as e5:
                        wst = [T(e5, nc, f"wuv{i}", [128, 16, 256], BF16) for i in range(2)]
                        ug = T(e5, nc, "ug", [128, NTT, 2048], F32)
                        vg = T(e5, nc, "vg", [128, NTT, 2048], F32)
                        lng = T(e5, nc, "lng", [128, 2048], F32)
                        lnb = T(e5, nc, "lnb", [128, 2048], F32)
                        vn = T(e5, nc, "vn", [128, 2048], BF16)
                        bst = T(e5, nc, "bst", [128, 8, 6], F32)
                        mv = T(e5, nc, "mv", [128, 4], F32)
                        P.dma("sp", lng[:, :], lng_d[0:1, :].partition_broadcast(128), w=[lng.r])
                        P.dma("sp", lnb[:, :], lnb_d[0:1, :].partition_broadcast(128), w=[lnb.r])
                        wi = 0
                        for gu in range(16):
                            wb = wst[wi % 2]
                            wi += 1
                            P.dma("pool", wb[:, :, :], wuv_d[:, gu * 256:(gu + 1) * 256].rearrange("(k p) n -> p k n", p=128), w=[wb.r])
                            dst = ug if gu < 8 else vg
                            cg = (gu % 8) * 256
                            for tt in range(NTT):
                                pb = ps[2 + tt % 2]
                                for k in range(16):
                                    h.mm(pb[:, 0:256], hT[:, k, tt * 128:(tt + 1) * 128], wb[:, k, :], k == 0, k == 15, [hT.r, wb.r], [pb.r])
                                h.act(dst[:, tt, cg:cg + 256], pb[:, 0:256], AF.Gelu, [pb.r], [dst.r])
                        for tt in range(NTT):
                            for q in range(4):
                                h.P.op("dve", lambda e, q=q, tt=tt: e.bn_stats(out=bst[:, q, :], in_=vg[:, tt, q * 512:(q + 1) * 512]),
                                       [vg.r], [bst.r])
                            h.P.op("dve", lambda e: e.bn_aggr(out=mv[:, 0:2], in_=bst[:, 0:4, :].rearrange("p a b -> p (a b)")), [bst.r], [mv.r])
                            h.ts("dve", mv[:, 2:3], mv[:, 1:2], EPS, None, ALU.add, None, [mv.r], [mv.r])
                            h.act(mv[:, 2:3], mv[:, 2:3], AF.Sqrt, [mv.r], [mv.r])
                            h.recip(mv[:, 2:3], mv[:, 2:3], [mv.r], [mv.r])
                            h.ts("dve", vg[:, tt, :], vg[:, tt, :], mv[:, 0:1], mv[:, 2:3], ALU.subtract, ALU.mult, [vg.r, mv.r], [vg.r])
                            h.tt("pool", vg[:, tt, :], vg[:, tt, :], lng[:, :], ALU.mult, [vg.r, lng.r], [vg.r])
                            h.tt("pool", vn[:, :], vg[:, tt, :], lnb[:, :], ALU.add, [vg.r, lnb.r], [vn.r])
                            pv = [ps[0], ps[1], ps[6], ps[7]]
                            for g in range(16):
                                pb = pv[g // 4]
                                h.mm(pb[:, (g % 4) * 128:(g % 4 + 1) * 128], wsT[:, g, :], vn[:, g * 128:(g + 1) * 128], True, True,
                                     [wsT.r, vn.r], [pb.r])
                            for q in range(4):
                                cs = slice(q * 512, (q + 1) * 512)
                                h.tt("dve", vg[:, tt, cs].rearrange("p (a b) -> p a b", a=4), pv[q][:, :].rearrange("p (a b) -> p a b", a=4),
                                     bsT[:, q * 4:(q + 1) * 4].unsqueeze(2).to_broadcast([128, 4, 128]), ALU.add, [pv[q].r, bsT.r], [vg.r])
                            h.tt("pool", vg[:, tt, :], vg[:, tt, :], ug[:, tt, :], ALU.mult, [vg.r, ug.r], [vg.r])
                            out_toks.append(P.dma("sp", yb_d[r0 + tt * 128:r0 + (tt + 1) * 128, :], vg[:, tt, :], r=[vg.r]))
                    P.barrier()
        P.wait("sp", out_toks[-48:])
        if ctx is None:
            P.emit()
        print("mix0 instrs", P.ninstr, {e: len(P.q[e]) for e in P.ENGS})
    return nc


def _fm(v):
    return np.ascontiguousarray(np.asarray(v, dtype=np.float32).reshape(-1, 128).T)


def _c(a):
    return np.ascontiguousarray(np.asarray(a, dtype=np.float32))


def _consts():
    f = np.float32
    jj = np.arange(128)
    s_ = jj[:, None, None]
    j_ = np.arange(4)[None, :, None]
    t_ = np.arange(512)[None, None, :]
    return dict(
        ident=np.eye(128, dtype=f),
        ut=(jj[:, None] <= jj[None, :]).astype(f),
        slt=(jj[:, None] > jj[None, :]).astype(f),
        masks=((j_ * 128 + s_) < t_).astype(f),
        ntri=-(jj[:, None] >= jj[None, :]).astype(f),
        summ=(jj[:, None] // 8 == np.arange(16)[None, :]).astype(f),
    )


def _mix0_inputs(p, in0_w, conv_w, conv_b, dt_bias, a_log, d_skip, ln_g, ln_b, ws, bs):
    zc = slice(p * 1024, (p + 1) * 1024)
    xc = slice(2048 + p * 1024, 2048 + (p + 1) * 1024)
    Bc = slice(4096 + p * 512, 4096 + (p + 1) * 512)
    Cc = slice(5120 + p * 512, 5120 + (p + 1) * 512)
    dc = slice(6144 + p * 16, 6144 + (p + 1) * 16)
    w_ssd = np.concatenate([in0_w[:, zc], in0_w[:, xc], in0_w[:, Bc], in0_w[:, Cc], in0_w[:, dc]], axis=1)
    cch = np.concatenate([np.arange(p * 1024, (p + 1) * 1024), 2048 + np.arange(p * 512, (p + 1) * 512),
                          3072 + np.arange(p * 512, (p + 1) * 512)])
    cw = conv_w[:, cch]
    cb = conv_b[cch]
    hs = slice(p * 16, (p + 1) * 16)
    return dict(w_ssd=_c(w_ssd), conv_w_fm=_c(cw.T.reshape(16, 128, 4).transpose(1, 0, 2)), conv_b_fm=_fm(cb),
                dt_bias=_c(dt_bias[None, hs]), a_log=_c(a_log[None, hs]), d_skip=_c(d_skip[None, hs]),
                w_uv=_c(in0_w[:, 6176:]), ln_g=_c(ln_g[None]), ln_b=_c(ln_b[None]),
                wsT=_c(ws.transpose(2, 0, 1)), bsT=_c(bs.T))


def kernel_unfused(x, c, ada_w, ada_b, norm_mix_g, norm_ffn_g, in0_w, conv_w, conv_b, dt_bias, a_log, d_skip, ssd_norm_g,
           gmlp_ln_g, gmlp_ln_b, gmlp_ws, gmlp_bs, out0_w, sb_qkv_w, sb_out_w, peer_wq, peer_keys, peer_u, peer_v, final_g):
    g = {k: np.asarray(v) for k, v in locals().items()}
    x = g["x"]
    c = g["c"]
    K = _consts()
    cores = list(range(8))
    HALF = 2048

    def ada_in(layer, b):
        return dict(c_fm=_fm(c[b]), ada_w=_c(g["ada_w"][layer]), ada_b_fm=_fm(g["ada_b"][layer]), ada_b=_c(g["ada_b"][layer][None]))

    nc1 = build_mix0(4096, 16, 4)
    mi = [_mix0_inputs(p, g["in0_w"][0], g["conv_w"][0], g["conv_b"][0], g["dt_bias"][0], g["a_log"][0], g["d_skip"][0],
                       g["gmlp_ln_g"][0], g["gmlp_ln_b"][0], g["gmlp_ws"][0], g["gmlp_bs"][0]) for p in range(2)]
    maps = []
    for core in cores:
        b, p = divmod(core, 2)
        d = dict(x=_c(x[b]), x_gm=_c(x[b, p * HALF:(p + 1) * HALF]), g_mix_fm=_fm(g["norm_mix_g"][0]),
                 ident=K["ident"], ut=K["ut"], slt=K["slt"])
        d.update(ada_in(0, b))
        d.update(mi[p])
        maps.append(d)
    r1 = run_bass_kernel_spmd(nc1, maps, core_ids=cores).results
    del maps
    nc2 = build_tail(HALF, 4096, final=False, ya_norm=True)
    maps = []
    for core in cores:
        b, p = divmod(core, 2)
        rows = slice(p * HALF, (p + 1) * HALF)
        mix = np.concatenate([r1[2 * b]["yg"][rows], r1[2 * b + 1]["yg"][rows], r1[core]["yb"]], axis=1)
        d = dict(x=_c(x[b, rows]), mix=_c(mix), wo=_c(g["out0_w"][0]), g_ffn_fm=_fm(g["norm_ffn_g"][0]), wq=_c(g["peer_wq"][0]),
                 keys=_c(g["peer_keys"][0].reshape(16, 128, 128)), U=_c(g["peer_u"][0]), V=_c(g["peer_v"][0]),
                 final_g=_c(g["final_g"][None]), g_ya_fm=_fm(g["ssd_norm_g"][0]), ident=K["ident"], summ=K["summ"])
        d.update(ada_in(0, b))
        maps.append(d)
    r2 = run_bass_kernel_spmd(nc2, maps, core_ids=cores).results
    del maps, r1
    nc3 = build_attn(4096, 8, 4)
    qkv = g["sb_qkv_w"][0]
    maps = []
    for core in cores:
        b, p = divmod(core, 2)
        hs = slice(p * 1024, (p + 1) * 1024)
        wqkv = np.concatenate([qkv[:, 0:2048][:, hs], qkv[:, 2048:4096][:, hs], qkv[:, 4096:6144][:, hs]], axis=1)
        xf = np.concatenate([r2[2 * b]["out"], r2[2 * b + 1]["out"]], axis=0)
        d = dict(x=_c(xf), g_mix_fm=_fm(g["norm_mix_g"][1]), wqkv=_c(wqkv), ident=K["ident"], masks=K["masks"], ntri=K["ntri"])
        d.update(ada_in(1, b))
        maps.append(d)
    r3 = run_bass_kernel_spmd(nc3, maps, core_ids=cores).results
    del maps
    nc4 = build_tail(HALF, 2048, final=True, ya_norm=False)
    maps = []
    for core in cores:
        b, p = divmod(core, 2)
        rows = slice(p * HALF, (p + 1) * HALF)
        mix = np.concatenate([r3[2 * b]["oT"][:, rows].T, r3[2 * b + 1]["oT"][:, rows].T], axis=1)
        d = dict(x=_c(r2[core]["out"]), mix=_c(mix), wo=_c(g["sb_out_w"][0]), g_ffn_fm=_fm(g["norm_ffn_g"][1]), wq=_c(g["peer_wq"][1]),
                 keys=_c(g["peer_keys"][1].reshape(16, 128, 128)), U=_c(g["peer_u"][1]), V=_c(g["peer_v"][1]),
                 final_g=_c(g["final_g"][None]), g_ya_fm=_fm(g["ssd_norm_g"][0]), ident=K["ident"], summ=K["summ"])
        d.update(ada_in(1, b))
        maps.append(d)
    r4 = run_bass_kernel_spmd(nc4, maps, core_ids=cores).results
    out = np.empty((4, 4096, 2048), dtype=np.float32)
    for core in cores:
        b, p = divmod(core, 2)
        out[b, p * HALF:(p + 1) * HALF] = r4[core]["out"]
    return out


def build_fused(S_LEN=4096):
    from contextlib import ExitStack
    nc = bass.Bass("TRN2", target_bir_lowering=False)
    HALF = S_LEN // 2

    def ein(name, shape):
        return nc.dram_tensor(name, shape, F32, kind="ExternalInput").ap()

    x_d = ein("x", [S_LEN, D])
    cfm = ein("c_fm", [128, 16])
    adaw = ein("ada_w", [2, D, 6 * D])
    adabfm = ein("ada_b_fm", [2, 128, 96])
    adab = ein("ada_b", [2, 1, 6 * D])
    gmix = ein("g_mix_fm", [2, 128, 16])
    gffn = ein("g_ffn_fm", [2, 128, 16])
    wssd = ein("w_ssd", [2, D, 3088])
    convw = ein("conv_w_fm", [2, 128, 16, 4])
    convb = ein("conv_b_fm", [2, 128, 16])
    dtb = ein("dt_bias", [2, 1, 16])
    alog = ein("a_log", [2, 1, 16])
    dsk = ein("d_skip", [2, 1, 16])
    wuv = ein("w_uv", [D, 4096])
    lng = ein("ln_g", [1, 2048])
    lnb = ein("ln_b", [1, 2048])
    wsT = ein("wsT", [128, 16, 128])
    bsT = ein("bsT", [128, 16])
    wo0 = ein("out0_w", [4096, D])
    gya = ein("g_ya_fm", [128, 16])
    wq = ein("wq", [2, D, D])
    keys = ein("keys", [2, 16, 128, 128])
    U = ein("U", [2, 16384, D])
    V = ein("V", [2, 16384, D])
    fing = ein("final_g", [1, D])
    wqkv = ein("wqkv", [D, 3 * D])
    wo1 = ein("sb_out_w", [D, D])
    sel = ein("sel", [128, 2])
    ident = ein("ident", [128, 128])
    ut = ein("ut", [128, 128])
    slt = ein("slt", [128, 128])
    masks = ein("masks", [128, 4, 512])
    ntri = ein("ntri", [128, 128])
    summ = ein("summ", [128, 16])
    mixA = nc.dram_tensor("mixA", [S_LEN, 4096], F32, kind="Internal").ap()
    x2 = nc.dram_tensor("x2", [S_LEN, D], F32, kind="Internal").ap()
    o_d = nc.dram_tensor("o_int", [S_LEN, D], F32, kind="Internal").ap()
    out_d = nc.dram_tensor("out", [HALF, D], F32, kind="ExternalOutput").ap()

    def ada_env(layer):
        return dict(c_fm=cfm, ada_w=adaw[layer], ada_b_fm=adabfm[layer], ada_b=adab[layer])

    with ExitStack() as es:
        ctx = Ctx(nc, es)
        for p in range(2):
            env = dict(x=x_d, x_gm=x_d, g_mix_fm=gmix[0], w_ssd=wssd[p], conv_w_fm=convw[p], conv_b_fm=convb[p],
                       dt_bias=dtb[p], a_log=alog[p], d_skip=dsk[p], w_uv=wuv, ln_g=lng, ln_b=lnb, wsT=wsT, bsT=bsT,
                       ident=ident, ut=ut, slt=slt, yg=mixA[:, p * 1024:(p + 1) * 1024], yb=mixA[:, 2048:4096])
            env.update(ada_env(0))
            build_mix0(S_LEN, 16, gm_nblk=(S_LEN // TB if p == 0 else 0), ctx=ctx, env=env, pfx=f"m{p}_")
        env = dict(x=x_d, mix=mixA, wo=wo0, g_ffn_fm=gffn[0], wq=wq[0], keys=keys[0], U=U[0], V=V[0], final_g=fing,
                   g_ya_fm=gya, ident=ident, summ=summ, out=x2)
        env.update(ada_env(0))
        build_tail(S_LEN, 4096, final=False, ya_norm=True, ctx=ctx, env=env, pfx="t0_")
        env = dict(x=x2, g_mix_fm=gmix[1], wqkv=wqkv, ident=ident, masks=masks, ntri=ntri, o=o_d)
        env.update(ada_env(1))
        build_attn(S_LEN, 16, 4, ctx=ctx, env=env, pfx="a_", tokmajor=True)
        env = dict(x=x2, mix=o_d, wo=wo1, g_ffn_fm=gffn[1], wq=wq[1], keys=keys[1], U=U[1], V=V[1], final_g=fing,
                   g_ya_fm=gya, ident=ident, summ=summ, out=out_d, sel=sel)
        env.update(ada_env(1))
        build_tail(HALF, 2048, final=True, ya_norm=False, ctx=ctx, env=env, pfx="t1_", blend=True)
        ctx.P.emit()
        print("fused instrs", ctx.P.ninstr, {e: len(ctx.P.q[e]) for e in ctx.P.ENGS})
    return nc


def kernel(x, c, ada_w, ada_b, norm_mix_g, norm_ffn_g, in0_w, conv_w, conv_b, dt_bias, a_log, d_skip, ssd_norm_g,
           gmlp_ln_g, gmlp_ln_b, gmlp_ws, gmlp_bs, out0_w, sb_qkv_w, sb_out_w, peer_wq, peer_keys, peer_u, peer_v, final_g):
    g = {k: np.asarray(v) for k, v in locals().items()}
    K = _consts()
    cores = list(range(8))
    mi = [_mix0_inputs(p, g["in0_w"][0], g["conv_w"][0], g["conv_b"][0], g["dt_bias"][0], g["a_log"][0], g["d_skip"][0],
                       g["gmlp_ln_g"][0], g["gmlp_ln_b"][0], g["gmlp_ws"][0], g["gmlp_bs"][0]) for p in range(2)]
    shared = dict(
        ada_w=_c(g["ada_w"]), ada_b_fm=_c(np.stack([_fm(g["ada_b"][l]) for l in range(2)])), ada_b=_c(g["ada_b"][:, None, :]),
        g_mix_fm=_c(np.stack([_fm(g["norm_mix_g"][l]) for l in range(2)])),
        g_ffn_fm=_c(np.stack([_fm(g["norm_ffn_g"][l]) for l in range(2)])),
        w_uv=mi[0]["w_uv"], ln_g=mi[0]["ln_g"], ln_b=mi[0]["ln_b"], wsT=mi[0]["wsT"], bsT=mi[0]["bsT"],
        out0_w=_c(g["out0_w"][0]), g_ya_fm=_fm(g["ssd_norm_g"][0]), wq=_c(g["peer_wq"]),
        keys=_c(g["peer_keys"].reshape(2, 16, 128, 128)), U=_c(g["peer_u"]), V=_c(g["peer_v"]),
        final_g=_c(g["final_g"][None]), wqkv=_c(g["sb_qkv_w"][0]), sb_out_w=_c(g["sb_out_w"][0]),
        ident=K["ident"], ut=K["ut"], slt=K["slt"], masks=K["masks"], ntri=K["ntri"], summ=K["summ"])
    for k in ("w_ssd", "conv_w_fm", "conv_b_fm", "dt_bias", "a_log", "d_skip"):
        shared[k] = _c(np.stack([mi[0][k], mi[1][k]]))
    nc = build_fused(4096)
    maps = []
    for core in cores:
        b, p = divmod(core, 2)
        d = dict(shared)
        d["x"] = _c(g["x"][b])
        d["c_fm"] = _fm(g["c"][b])
        selv = np.zeros((128, 2), dtype=np.float32)
        selv[:, p] = 1.0
        d["sel"] = selv
        maps.append(d)
    res = run_bass_kernel_spmd(nc, maps, core_ids=cores).results
    out = np.empty((4, 4096, 2048), dtype=np.float32)
    for core in cores:
        b, p = divmod(core, 2)
        out[b, p * 2048:(p + 1) * 2048] = res[core]["out"]
    return out
```

```python
import numpy as np
import concourse.bass as bass
import concourse.mybir as mybir
from concourse.bass_utils import run_bass_kernel_spmd

F32 = mybir.dt.float32
BF16 = mybir.dt.bfloat16
AF = mybir.ActivationFunctionType
ALU = mybir.AluOpType
AX = mybir.AxisListType

D = 2048
DC = 16
EPS = 1e-6
NEG = -1.0e30


class Res:
    __slots__ = ("w", "rs", "name")

    def __init__(self, name=""):
        self.w = None
        self.rs = {}
        self.name = name


class Prog:
    ENGS = ["pe", "act", "dve", "pool", "sp"]

    def __init__(self, nc, n_dma_sems=40):
        self.nc = nc
        self.q = {e: [] for e in self.ENGS}
        self.sem = {e: nc.alloc_semaphore(name=f"sem_{e}") for e in self.ENGS}
        self.cnt = {e: 0 for e in self.ENGS}
        self.seen = {e: {} for e in self.ENGS}
        self.dsem = [nc.alloc_semaphore(name=f"dsem{i}") for i in range(n_dma_sems)]
        self.dcnt = [0] * n_dma_sems
        self.dnext = 0
        self.ninstr = 0

    def _need(self, e, waits, tok):
        if tok is None:
            return
        sem, val = tok
        if sem.num == self.sem[e].num and e == "pe":
            return
        if self.seen[e].get(sem.num, 0) >= val:
            return
        self.seen[e][sem.num] = val
        waits.append((sem, val))

    def _deps(self, e, r, w):
        waits = []
        for res in r:
            self._need(e, waits, res.w)
        for res in w:
            self._need(e, waits, res.w)
            for t in res.rs.values():
                self._need(e, waits, t)
        return waits

    def _commit(self, tok, r, w):
        for res in r:
            res.rs[tok[0].num] = tok
        for res in w:
            res.w = tok
            res.rs = {}

    def op(self, e, fn, r=(), w=()):
        waits = self._deps(e, r, w)
        self.cnt[e] += 1
        tok = (self.sem[e], self.cnt[e])
        self.q[e].append((waits, fn, self.sem[e], 1))
        self._commit(tok, r, w)
        self.ninstr += 1 + len(waits)
        return tok

    def dma(self, e, out, in_, r=(), w=(), **kw):
        waits = self._deps(e, r, w)
        i = self.dnext
        self.dnext = (self.dnext + 1) % len(self.dsem)
        if self.dcnt[i] > 0:
            self._need(e, waits, (self.dsem[i], self.dcnt[i]))
        self.dcnt[i] += 16
        tok = (self.dsem[i], self.dcnt[i])
        self.q[e].append((waits, lambda eng: eng.dma_start(out=out, in_=in_, **kw), self.dsem[i], 16))
        self._commit(tok, r, w)
        self.ninstr += 1 + len(waits)
        return tok

    def wait(self, e, toks):
        waits = []
        for t in toks:
            self._need(e, waits, t)
        if waits:
            self.q[e].append((waits, None, None, 0))

    def barrier(self):
        toks = [(self.sem[e], self.cnt[e]) for e in self.ENGS if self.cnt[e] > 0]
        toks += [(self.dsem[i], self.dcnt[i]) for i in range(len(self.dsem)) if self.dcnt[i] > 0]
        for e in self.ENGS:
            self.wait(e, toks)

    def emit(self):
        nc = self.nc
        q = self.q

        def run(eng, items):
            for waits, fn, sem, inc in items:
                for s, v in waits:
                    eng.wait_ge(s, v)
                if fn is not None:
                    fn(eng).then_inc(sem, inc)

        with nc.Block() as block:
            @block.tensor
            def _(t):
                run(t, q["pe"])

            @block.scalar
            def _(t):
                run(t, q["act"])

            @block.vector
            def _(t):
                run(t, q["dve"])

            @block.gpsimd
            def _(t):
                run(t, q["pool"])

            @block.sync
            def _(t):
                run(t, q["sp"])


class T:
    _n = [0]

    def __init__(self, es, nc, name, shape, dtype, psum=False):
        T._n[0] += 1
        name = f"{name}_u{T._n[0]}"
        cm = nc.psum_tensor(name, shape, dtype) if psum else nc.sbuf_tensor(name, shape, dtype)
        self.t = es.enter_context(cm)
        self.r = Res(name)

    def __getitem__(self, k):
        return self.t[k]


class Ctx:
    def __init__(self, nc, es):
        self.nc = nc
        self.P = Prog(nc)
        self.h = H(self.P)
        self.ps = [T(es, nc, f"ps{i}", [128, 512], F32, psum=True) for i in range(8)]
        self.out_toks = []


def _io(nc, env, pfx):
    def din(name, shape):
        if env is not None and name in env:
            return env[name]
        return nc.dram_tensor(pfx + name, shape, F32, kind="ExternalInput").ap()

    def dout(name, shape):
        if env is not None and name in env:
            return env[name]
        return nc.dram_tensor(pfx + name, shape, F32, kind="ExternalOutput").ap()
    return din, dout


class H:
    def __init__(self, P):
        self.P = P

    def mm(self, out, lhsT, rhs, start, stop, r, w):
        return self.P.op("pe", lambda e: e.matmul(out, lhsT=lhsT, rhs=rhs, start=start, stop=stop), r, w)

    def tr(self, out, in_, ident, r, w):
        return self.P.op("pe", lambda e: e.transpose(out, in_, ident), r, w)

    def act(self, out, in_, func, r, w, bias=None, scale=None, accum=None):
        kw = {}
        if bias is not None:
            kw["bias"] = bias
        if scale is not None:
            kw["scale"] = scale
        if accum is not None:
            kw["accum_out"] = accum
        return self.P.op("act", lambda e: e.activation(out=out, in_=in_, func=func, **kw), r, w)

    def tt(self, eng, out, in0, in1, op, r, w):
        return self.P.op(eng, lambda e: e.tensor_tensor(out=out, in0=in0, in1=in1, op=op), r, w)

    def ts(self, eng, out, in0, s1, s2, op0, op1, r, w, accum=None):
        if accum is not None:
            return self.P.op(eng, lambda e: e.tensor_scalar(out=out, in0=in0, scalar1=s1, scalar2=s2, op0=op0, op1=op1, accum_out=accum), r, w)
        if op1 is None:
            return self.P.op(eng, lambda e: e.tensor_scalar(out=out, in0=in0, scalar1=s1, scalar2=None, op0=op0), r, w)
        return self.P.op(eng, lambda e: e.tensor_scalar(out=out, in0=in0, scalar1=s1, scalar2=s2, op0=op0, op1=op1), r, w)

    def stt(self, out, in0, scalar, in1, op0, op1, r, w):
        return self.P.op("dve", lambda e: e.scalar_tensor_tensor(out=out, in0=in0, scalar=scalar, in1=in1, op0=op0, op1=op1), r, w)

    def cp(self, eng, out, in_, r, w):
        if eng == "act":
            return self.P.op("act", lambda e: e.copy(out=out, in_=in_), r, w)
        return self.P.op(eng, lambda e: e.tensor_copy(out=out, in_=in_), r, w)

    def memset(self, eng, ap, val, w):
        return self.P.op(eng, lambda e: e.memset(ap, val), (), w)

    def max8(self, out, in_, r, w):
        return self.P.op("dve", lambda e: e.max(out=out, in_=in_), r, w)

    def mrep(self, out, rep, vals, r, w):
        return self.P.op("dve", lambda e: e.match_replace(out=out, in_to_replace=rep, in_values=vals, imm_value=NEG), r, w)

    def recip(self, out, in_, r, w):
        return self.P.op("dve", lambda e: e.reciprocal(out=out, in_=in_), r, w)

    def red(self, out, in_, op, r, w):
        return self.P.op("dve", lambda e: e.tensor_reduce(out=out, in_=in_, axis=AX.X, op=op), r, w)


TB = 512
NTT = TB // 128
NJ = TB // 16


def ada_compute(es0, nc, P, h, ps, cfm_d, adaw_d, adabfm_d, adab_d, fm_secs, bc_secs, tag):
    from contextlib import ExitStack
    outs = {}
    for s in fm_secs:
        outs[s] = T(es0, nc, f"ada_fm{tag}_{s}", [128, 16], F32)
    for s in bc_secs:
        outs[s] = T(es0, nc, f"ada_bc{tag}_{s}", [128, 2048], F32)
    with ExitStack() as es:
        condT = T(es, nc, f"condT{tag}", [128, 16], F32)
        condB = T(es, nc, f"condB{tag}", [128, 16, 128], F32)
        abfm = T(es, nc, f"abfm{tag}", [128, 96], F32)
        wblk = [T(es, nc, f"adaw{tag}_{i}", [128, 16, 512], F32) for i in range(2)]
        abb = [T(es, nc, f"adabb{tag}_{i}", [128, 512], F32) for i in range(2)]
        P.dma("sp", condT[:, :], cfm_d, w=[condT.r])
        P.dma("sp", abfm[:, :], adabfm_d, w=[abfm.r])
        h.act(condT[:, :], condT[:, :], AF.Silu, [condT.r], [condT.r])
        h.cp("dve", condB[:, :, :], condT[:, :].unsqueeze(2).to_broadcast([128, 16, 128]), [condT.r], [condB.r])
        it = 0
        for s in sorted(set(fm_secs) | set(bc_secs)):
            for cb in range(4):
                wb = wblk[it % 2]
                ab = abb[it % 2]
                it += 1
                c0 = s * 2048 + cb * 512
                P.dma("sp", wb[:, :, :], adaw_d[:, c0:c0 + 512].rearrange("(k p) n -> p k n", p=128), w=[wb.r])
                if s in bc_secs:
                    P.dma("sp", ab[:, :], adab_d[0:1, c0:c0 + 512].partition_broadcast(128), w=[ab.r])
                    pb = ps[it % 2]
                    for k in range(16):
                        h.mm(pb[:, :], condB[:, k, :], wb[:, k, :], k == 0, k == 15, [condB.r, wb.r], [pb.r])
                    h.tt("dve", outs[s][:, cb * 512:(cb + 1) * 512], pb[:, :], ab[:, :], ALU.add, [pb.r, ab.r], [outs[s].r])
                if s in fm_secs:
                    pf = ps[2 + (it % 2)]
                    for jj in range(4):
                        for k in range(16):
                            h.mm(pf[:, jj:jj + 1], wb[:, k, jj * 128:(jj + 1) * 128], condT[:, k:k + 1], k == 0, k == 15,
                                 [condT.r, wb.r], [pf.r])
                    j0 = cb * 4
                    h.tt("dve", outs[s][:, j0:j0 + 4], pf[:, 0:4], abfm[:, s * 16 + j0:s * 16 + j0 + 4], ALU.add,
                         [pf.r, abfm.r], [outs[s].r])
    P.barrier()
    return outs


def rms_stats(P, h, x_ap, sq_scr, ssq_ap, rstd_ap, r, scr_res, st_res, n):
    h.act(sq_scr, x_ap, AF.Square, r, [scr_res, st_res], accum=ssq_ap)
    h.ts("dve", rstd_ap, ssq_ap, 1.0 / n, EPS, ALU.mult, ALU.add, [st_res], [st_res])
    h.act(rstd_ap, rstd_ap, AF.Sqrt, [st_res], [st_res])
    h.recip(rstd_ap, rstd_ap, [st_res], [st_res])


def to_fm(P, h, ps, ident, src, src_res, ncol, dst, tt, evac):
    for q in range(ncol // 4):
        pb = ps[q % 2]
        for j in range(4):
            kc = q * 4 + j
            h.tr(pb[:, j * 128:(j + 1) * 128], src[:, kc * 128:(kc + 1) * 128], ident[:, :], [src_res, ident.r], [pb.r])
        evac(q, pb)


def build_tail(NT, KMIX, final, ya_norm, n_i1=128, ctx=None, env=None, pfx="", blend=False):
    from contextlib import ExitStack
    nc = ctx.nc if ctx else bass.Bass("TRN2", target_bir_lowering=False)
    NB = NT // TB
    KC = KMIX // 128
    GV = 4
    din, dout = _io(nc, env, pfx)
    NSRC = 2 * NT if blend else NT
    sel_d = din("sel", [128, 2]) if blend else None
    x_d = din("x", [NSRC, D])
    mix_d = din("mix", [NSRC, KMIX])
    wo_d = din("wo", [KMIX, D])
    cfm_d = din("c_fm", [128, 16])
    adaw_d = din("ada_w", [D, 6 * D])
    adabfm_d = din("ada_b_fm", [128, 96])
    adab_d = din("ada_b", [1, 6 * D])
    gffn_d = din("g_ffn_fm", [128, 16])
    wq_d = din("wq", [D, D])
    keys_d = din("keys", [16, 128, 128])
    U_d = din("U", [16384, D])
    V_d = din("V", [16384, D])
    fing_d = din("final_g", [1, D])
    gya_d = din("g_ya_fm", [128, 16])
    ident_d = din("ident", [128, 128])
    summ_d = din("summ", [128, 16])
    out_d = dout("out", [NT, D])
    sc_d = nc.dram_tensor(pfx + "sc_scr", [TB, 2048], F32, kind="Internal").ap()
    bc_d = nc.dram_tensor(pfx + "bc_scr", [2, 128, 2048], F32, kind="Internal").ap()
    scd_res = Res("sc_d")
    bcd_res = Res("bc_d")
    UT_d = nc.dram_tensor(pfx + "UT_scr", [n_i1, 128, 2048], BF16, kind="Internal").ap()
    Vb_d = nc.dram_tensor(pfx + "Vb_scr", [n_i1 * 128, D], BF16, kind="Internal").ap()

    with ExitStack() as es:
        if ctx is None:
            P = Prog(nc)
            h = H(P)
            ps = [T(es, nc, f"ps{i}", [128, 512], F32, psum=True) for i in range(8)]
        else:
            P, h, ps = ctx.P, ctx.h, ctx.ps
        ident = T(es, nc, "ident", [128, 128], F32)
        summ = T(es, nc, "summ", [128, 16], F32)
        sel = T(es, nc, "sel", [128, 2], F32)
        if blend:
            P.dma("sp", sel[:, :], sel_d, w=[sel.r])
        gffn = T(es, nc, "gffn", [128, 16], F32)
        gya = T(es, nc, "gya", [128, 16], F32)
        gs2 = T(es, nc, "gs2", [128, 16], F32)
        keysT = T(es, nc, "keysT", [128, 16, 128], F32)
        P.dma("sp", ident[:, :], ident_d, w=[ident.r])
        P.dma("sp", summ[:, :], summ_d, w=[summ.r])
        P.dma("sp", gffn[:, :], gffn_d, w=[gffn.r])
        P.dma("sp", gya[:, :], gya_d, w=[gya.r])

        sh2 = T(es, nc, "sh2", [128, 16], F32)
        with ExitStack() as es1:
            ada = ada_compute(es1, nc, P, h, ps, cfm_d, adaw_d, adabfm_d, adab_d, fm_secs=[3, 4], bc_secs=[2, 5], tag="t")
            h.cp("dve", sh2[:, :], ada[3][:, :], [ada[3].r], [sh2.r])
            h.stt(gs2[:, :], ada[4][:, :], 1.0, gffn[:, :], ALU.add, ALU.mult, [ada[4].r, gffn.r], [gs2.r])
            P.dma("sp", bc_d[0], ada[2][:, :], r=[ada[2].r], w=[bcd_res])
            P.dma("sp", bc_d[1], ada[5][:, :], r=[ada[5].r], w=[bcd_res])
            kraw = T(es1, nc, "kraw", [128, 16, 128], F32)
            P.dma("sp", kraw[:, :, :], keys_d.rearrange("a k c -> k a c"), w=[kraw.r])
            for q in range(4):
                pb = ps[4 + q % 2]
                for j in range(4):
                    h.tr(pb[:, j * 128:(j + 1) * 128], kraw[:, q * 4 + j, :], ident[:, :], [kraw.r, ident.r], [pb.r])
                h.cp("dve", keysT[:, q * 4:(q + 1) * 4, :], pb[:, :].rearrange("p (a b) -> p a b", a=4), [pb.r], [keysT.r])
            P.barrier()

        with ExitStack() as esp:
            Uraw = [T(esp, nc, f"Uraw{i}", [128, D], F32) for i in range(2)]
            utb = [T(esp, nc, f"utb{i}", [128, 16, 128], BF16) for i in range(2)]
            VR = 1024
            for r0 in range(0, n_i1 * 128, VR):
                P.dma("pool", Vb_d[r0:r0 + VR, :], V_d[r0:r0 + VR, :])
            for i1 in range(n_i1):
                u = Uraw[i1 % 2]
                ut = utb[i1 % 2]
                P.dma("sp", u[:, :], U_d[i1 * 128:(i1 + 1) * 128, :], w=[u.r])
                for q in range(4):
                    pb = ps[(i1 * 4 + q) % 4]
                    for j in range(4):
                        h.tr(pb[:, j * 128:(j + 1) * 128], u[:, (q * 4 + j) * 128:(q * 4 + j + 1) * 128], ident[:, :],
                             [u.r, ident.r], [pb.r])
                    h.cp("act" if q % 2 else "dve", ut[:, q * 4:(q + 1) * 4, :], pb[:, :].rearrange("p (a b) -> p a b", a=4),
                         [pb.r], [ut.r])
                P.dma("sp", UT_d[i1].rearrange("p (k e) -> p k e", k=16), ut[:, :, :], r=[ut.r])
            P.barrier()

        for b in range(NB):
            t0 = b * TB
            ores = Res(f"out_blk{b}")
            oblk = out_d[t0:t0 + TB, :].rearrange("(t p) d -> p t d", p=128)
            with ExitStack() as esb:
                hT = T(esb, nc, f"hT", [128, 16, TB], BF16)
                with ExitStack() as esa:
                    xb = T(esa, nc, "xb", [128, NTT, D], F32)
                    P.dma("sp", xb[:, :, :], x_d[t0:t0 + TB, :].rearrange("(t p) d -> p t d", p=128), w=[xb.r])
                    if blend:
                        with ExitStack() as esx:
                            xb2 = T(esx, nc, "xb2", [128, NTT, D], F32)
                            P.dma("sp", xb2[:, :, :], x_d[NT + t0:NT + t0 + TB, :].rearrange("(t p) d -> p t d", p=128), w=[xb2.r])
                            for tt in range(NTT):
                                h.ts("dve", xb[:, tt, :], xb[:, tt, :], sel[:, 0:1], None, ALU.mult, None, [xb.r, sel.r], [xb.r])
                                h.stt(xb[:, tt, :], xb2[:, tt, :], sel[:, 1:2], xb[:, tt, :], ALU.mult, ALU.add, [xb2.r, sel.r, xb.r], [xb.r])
                            P.barrier()
                    g1 = T(esa, nc, "g1", [128, D], F32)
                    P.dma("sp", g1[:, :], bc_d[0], r=[bcd_res], w=[g1.r])
                    with ExitStack() as esa2:
                        mixT = T(esa2, nc, "mixT", [128, KC, TB], BF16)
                        mraw = [T(esa2, nc, f"mraw{i}", [128, KMIX], F32) for i in range(2)]
                        sta = T(esa2, nc, "sta", [128, 8], F32)
                        sqa = T(esa2, nc, "sqa", [128, 2048], F32)
                        tmp = T(esa2, nc, "tmpa", [128, 512], F32)
                        wob = [T(esa2, nc, f"wob{i}", [128, KC, 512], BF16) for i in range(2)]
                        mrB = T(esa2, nc, "mrB", [128, KMIX], F32) if blend else None
                        for tt in range(NTT):
                            mr = mraw[tt % 2]
                            P.dma("sp", mr[:, :], mix_d[t0 + tt * 128:t0 + (tt + 1) * 128, :], w=[mr.r])
                            if blend:
                                P.dma("sp", mrB[:, :], mix_d[NT + t0 + tt * 128:NT + t0 + (tt + 1) * 128, :], w=[mrB.r])
                                h.ts("dve", mr[:, :], mr[:, :], sel[:, 0:1], None, ALU.mult, None, [mr.r, sel.r], [mr.r])
                                h.stt(mr[:, :], mrB[:, :], sel[:, 1:2], mr[:, :], ALU.mult, ALU.add, [mrB.r, sel.r, mr.r], [mr.r])
                            if ya_norm:
                                rms_stats(P, h, mr[:, 0:2048], sqa[:, :], sta[:, 0:1], sta[:, 1:2], [mr.r], sqa.r, sta.r, 2048)
                                h.ts("dve", mr[:, 0:2048], mr[:, 0:2048], sta[:, 1:2], None, ALU.mult, None, [mr.r, sta.r], [mr.r])

                            def evac(q, pb, tt=tt):
                                dst = mixT[:, q * 4:(q + 1) * 4, tt * 128:(tt + 1) * 128]
                                src = pb[:, :].rearrange("p (a b) -> p a b", a=4)
                                if ya_norm and q < 4:
                                    h.tt("dve", dst, src, gya[:, q * 4:(q + 1) * 4].unsqueeze(2).to_broadcast([128, 4, 128]),
                                         ALU.mult, [pb.r, gya.r], [mixT.r])
                                elif q % 2 == 0:
                                    h.cp("dve", dst, src, [pb.r], [mixT.r])
                                else:
                                    h.cp("act", dst, src, [pb.r], [mixT.r])
                            to_fm(P, h, ps, ident, mr, mr.r, KC, mixT, tt, evac)
                        for db in range(4):
                            wb = wob[db % 2]
                            P.dma("pool", wb[:, :, :], wo_d[:, db * 512:(db + 1) * 512].rearrange("(k p) n -> p k n", p=128), w=[wb.r])
                            for tt in range(NTT):
                                pb = ps[4 + (tt % 2)]
                                for kc in range(KC):
                                    h.mm(pb[:, :], mixT[:, kc, tt * 128:(tt + 1) * 128], wb[:, kc, :], kc == 0, kc == KC - 1,
                                         [mixT.r, wb.r], [pb.r])
                                h.tt("dve", tmp[:, :], pb[:, :], g1[:, db * 512:(db + 1) * 512], ALU.mult, [pb.r, g1.r], [tmp.r])
                                h.tt("dve", xb[:, tt, db * 512:(db + 1) * 512], xb[:, tt, db * 512:(db + 1) * 512], tmp[:, :], ALU.add,
                                     [tmp.r, xb.r], [xb.r])
                    P.barrier()
                    P.dma("sp", oblk, xb[:, :, :], r=[xb.r], w=[ores])
                    with ExitStack() as esn:
                        sq = T(esn, nc, "sq", [128, D], F32)
                        st = T(esn, nc, "st", [128, 8], F32)
                        for tt in range(NTT):
                            rms_stats(P, h, xb[:, tt, :], sq[:, :], st[:, 0:1], st[:, 1:2], [xb.r], sq.r, st.r, D)
                            h.ts("dve", sq[:, :], xb[:, tt, :], st[:, 1:2], None, ALU.mult, None, [xb.r, st.r], [sq.r])

                            def evac(q, pb, tt=tt):
                                for j in range(4):
                                    kc = q * 4 + j
                                    h.act(hT[:, kc, tt * 128:(tt + 1) * 128], pb[:, j * 128:(j + 1) * 128], AF.Identity,
                                          [pb.r, gs2.r, sh2.r], [hT.r], bias=sh2[:, kc:kc + 1], scale=gs2[:, kc:kc + 1])
                            to_fm(P, h, ps, ident, sq, sq.r, 16, hT, tt, evac)
                    P.barrier()
                s2 = T(esb, nc, "s2", [128, NJ, 128], F32)
                theta = T(esb, nc, "theta", [128, NJ, 128], F32)
                e1 = T(esb, nc, "e1", [128, NJ, 128], BF16)
                e2 = T(esb, nc, "e2", [128, NJ, 128], BF16)
                with ExitStack() as esc:
                    qT = T(esc, nc, "qT", [128, 16, TB], F32)
                    wqb = [T(esc, nc, f"wqb{i}", [128, 16, 128], BF16) for i in range(2)]
                    sc = [T(esc, nc, f"sc{i}", [128, 2048], F32) for i in range(2)]
                    for oc in range(16):
                        wb = wqb[oc % 2]
                        P.dma("pool", wb[:, :, :], wq_d[:, oc * 128:(oc + 1) * 128].rearrange("(k p) n -> p k n", p=128), w=[wb.r])
                        pb = ps[oc % 2]
                        for k in range(16):
                            h.mm(pb[:, :], wb[:, k, :], hT[:, k, :], k == 0, k == 15, [wb.r, hT.r], [pb.r])
                        h.cp("act" if oc % 2 else "dve", qT[:, oc, :], pb[:, :], [pb.r], [qT.r])
                    for tt in range(NTT):
                        s_ = sc[tt % 2]
                        for q4 in range(4):
                            pb = ps[2 + q4 % 2]
                            for j in range(4):
                                hi = q4 * 4 + j
                                h.mm(pb[:, j * 128:(j + 1) * 128], qT[:, hi, tt * 128:(tt + 1) * 128], keysT[:, hi, :], True, True,
                                     [qT.r, keysT.r], [pb.r])
                            h.cp("act" if q4 % 2 else "dve", s_[:, q4 * 512:(q4 + 1) * 512], pb[:, :], [pb.r], [s_.r])
                        P.dma("sp", sc_d[tt * 128:(tt + 1) * 128, :], s_[:, :], r=[s_.r], w=[scd_res])
                P.barrier()
                with ExitStack() as esk:
                    S = T(esk, nc, "S", [128, NJ, 256], F32)
                    P.dma("sp", S[:, :, :], sc_d.rearrange("t (h x) -> (t h) x", h=8).rearrange("(j p) x -> p j x", p=128),
                          r=[scd_res], w=[S.r])
                    A16 = T(esk, nc, "A16", [128, NJ, 16], F32)
                    B16 = T(esk, nc, "B16", [128, NJ, 16], F32)
                    C24 = T(esk, nc, "C24", [128, NJ, 24], F32)
                    wk = T(esk, nc, "wk", [128, 128], F32)
                    cand = T(esk, nc, "cand", [128, 256], F32)
                    cw = T(esk, nc, "cw", [128, 256], F32)
                    cw2 = T(esk, nc, "cw2", [128, 256], F32)
                    sm_ = T(esk, nc, "smalls", [128, 4, NJ], F32)
                    ce = T(esk, nc, "ce", [128, NJ, 16], F32)
                    tmpf = T(esk, nc, "tmpf", [128, NJ, 128], F32)
                    for j in range(NJ):
                        for half, dst in ((0, A16), (1, B16)):
                            src = S[:, j, half * 128:(half + 1) * 128]
                            h.max8(dst[:, j, 0:8], src, [S.r], [dst.r])
                            h.mrep(wk[:, :], dst[:, j, 0:8], src, [S.r, dst.r], [wk.r])
                            h.max8(dst[:, j, 8:16], wk[:, :], [wk.r], [dst.r])
                        h.tt("dve", cand[:, :].rearrange("p (a b) -> p a b", a=16),
                             A16[:, j, :].unsqueeze(2).to_broadcast([128, 16, 16]),
                             B16[:, j, :].unsqueeze(1).to_broadcast([128, 16, 16]), ALU.add, [A16.r, B16.r], [cand.r])
                        h.max8(C24[:, j, 0:8], cand[:, :], [cand.r], [C24.r])
                        h.mrep(cw[:, :], C24[:, j, 0:8], cand[:, :], [cand.r, C24.r], [cw.r])
                        h.max8(C24[:, j, 8:16], cw[:, :], [cw.r], [C24.r])
                        h.mrep(cw2[:, :], C24[:, j, 8:16], cw[:, :], [cw.r, C24.r], [cw2.r])
                        h.max8(C24[:, j, 16:24], cw2[:, :], [cw2.r], [C24.r])
                    mt = sm_[:, 0, :]
                    thr = sm_[:, 1, :]
                    zz = sm_[:, 2, :]
                    h.tt("dve", mt, A16[:, :, 0], B16[:, :, 0], ALU.add, [A16.r, B16.r], [sm_.r])
                    h.tt("dve", thr, C24[:, :, 15], C24[:, :, 16], ALU.add, [C24.r], [sm_.r])
                    h.ts("dve", thr, thr, 0.5, None, ALU.mult, None, [sm_.r], [sm_.r])
                    h.tt("dve", ce[:, :, :], C24[:, :, 0:16], mt.unsqueeze(2).to_broadcast([128, NJ, 16]), ALU.subtract,
                         [C24.r, sm_.r], [ce.r])
                    h.act(ce[:, :, :], ce[:, :, :], AF.Exp, [ce.r], [ce.r])
                    h.red(zz, ce[:, :, :], ALU.add, [ce.r], [sm_.r])
                    h.recip(zz, zz, [sm_.r], [sm_.r])
                    h.tt("dve", theta[:, :, :], thr.unsqueeze(2).to_broadcast([128, NJ, 128]), S[:, :, 0:128], ALU.subtract,
                         [sm_.r, S.r], [theta.r])
                    h.tt("dve", tmpf[:, :, :], S[:, :, 0:128], A16[:, :, 0:1].to_broadcast([128, NJ, 128]), ALU.subtract,
                         [S.r, A16.r], [tmpf.r])
                    h.act(e1[:, :, :], tmpf[:, :, :], AF.Exp, [tmpf.r], [e1.r])
                    h.tt("dve", tmpf[:, :, :], S[:, :, 128:256], B16[:, :, 0:1].to_broadcast([128, NJ, 128]), ALU.subtract,
                         [S.r, B16.r], [tmpf.r])
                    h.act(tmpf[:, :, :], tmpf[:, :, :], AF.Exp, [tmpf.r], [tmpf.r])
                    h.tt("dve", e2[:, :, :], tmpf[:, :, :], zz.unsqueeze(2).to_broadcast([128, NJ, 128]), ALU.mult,
                         [tmpf.r, sm_.r], [e2.r])
                    h.cp("dve", s2[:, :, :], S[:, :, 128:256], [S.r], [s2.r])
                P.barrier()
                acc = T(esb, nc, "acc", [128, NTT, D], F32)
                h.memset("pool", acc[:, :, :], 0.0, [acc.r])
                with ExitStack() as esl:
                    UT = [T(esl, nc, f"UT{i}", [128, 16, 128], BF16) for i in range(3)]
                    Vg = [T(esl, nc, f"Vg{i}", [128, GV, D], BF16) for i in range(2)]
                    GA = [T(esl, nc, f"GA{i}", [128, TB], BF16) for i in range(2)]
                    AG = [T(esl, nc, f"AG{i}", [128, GV, TB], BF16) for i in range(2)]
                    mks = [T(esl, nc, f"mk{i}", [128, NJ, 128], BF16) for i in range(2)]
                    Gh = [T(esl, nc, f"Gh{i}", [128, NJ, 128], BF16) for i in range(2)]
                    sums = [T(esl, nc, f"sums{i}", [128, NJ, 16], BF16) for i in range(2)]
                    def load_v(g):
                        vg = Vg[g % 2]
                        P.dma("sp", vg[:, :, :], Vb_d[g * GV * 128:(g + 1) * GV * 128, :].rearrange("(c p) d -> p c d", p=128), w=[vg.r])

                    def stage1(i1):
                        ut = UT[i1 % 3]
                        P.dma("sp", ut[:, :, :], UT_d[i1].rearrange("p (k e) -> p k e", k=16), w=[ut.r])
                        pa = ps[2 + i1 % 2]
                        for k in range(16):
                            h.mm(pa[:, :], ut[:, k, :], hT[:, k, :], k == 0, k == 15, [ut.r, hT.r], [pa.r])
                        ga = GA[i1 % 2]
                        h.act(ga[:, :], pa[:, :], AF.Gelu, [pa.r], [ga.r])
                        gh = Gh[i1 % 2]
                        sm = sums[i1 % 2]
                        mk = mks[i1 % 2]
                        h.tt("dve", mk[:, :, :], s2[:, :, :], theta[:, :, i1:i1 + 1].to_broadcast([128, NJ, 128]), ALU.is_ge,
                             [s2.r, theta.r], [mk.r])
                        h.tt("pool", gh[:, :, :], mk[:, :, :], e2[:, :, :], ALU.mult, [mk.r, e2.r], [gh.r])
                        h.tt("pool", sm[:, :, :], summ[:, :].unsqueeze(1).to_broadcast([128, NJ, 16]),
                             e1[:, :, i1:i1 + 1].to_broadcast([128, NJ, 16]), ALU.mult, [summ.r, e1.r], [sm.r])

                    def stage2(i1):
                        g, c = divmod(i1, GV)
                        ag = AG[g % 2]
                        ga = GA[i1 % 2]
                        gh = Gh[i1 % 2]
                        sm = sums[i1 % 2]
                        pg = ps[4 + i1 % 2]
                        for j in range(NJ):
                            h.mm(pg[:, j * 16:(j + 1) * 16], gh[:, j, :], sm[:, j, :], True, True, [gh.r, sm.r], [pg.r])
                        h.tt("dve", ag[:, c, :], ga[:, :], pg[:, :], ALU.mult, [ga.r, pg.r], [ag.r])

                    def stage3(g):
                        vg = Vg[g % 2]
                        ag = AG[g % 2]
                        for tt in range(NTT):
                            for db in range(4):
                                po = [ps[6], ps[7], ps[0], ps[1]][(tt * 4 + db) % 4]
                                for c in range(GV):
                                    h.mm(po[:, :], ag[:, c, tt * 128:(tt + 1) * 128], vg[:, c, db * 512:(db + 1) * 512], c == 0, c == GV - 1,
                                         [ag.r, vg.r], [po.r])
                                h.tt("dve", acc[:, tt, db * 512:(db + 1) * 512], acc[:, tt, db * 512:(db + 1) * 512], po[:, :], ALU.add,
                                     [po.r, acc.r], [acc.r])

                    load_v(0)
                    stage1(0)
                    for i1 in range(n_i1):
                        g, c = divmod(i1, GV)
                        if c == 0 and (g + 1) * GV < n_i1:
                            load_v(g + 1)
                        if i1 + 1 < n_i1:
                            stage1(i1 + 1)
                        stage2(i1)
                        if c == GV - 1:
                            stage3(g)
                P.barrier()
                with ExitStack() as esf:
                    xb = T(esf, nc, "xbf", [128, NTT, D], F32)
                    g2 = T(esf, nc, "g2", [128, D], F32)
                    fg = T(esf, nc, "fg", [128, D], F32)
                    sq = T(esf, nc, "sqf", [128, D], F32)
                    st = T(esf, nc, "stf", [128, 8], F32)
                    P.dma("sp", xb[:, :, :], oblk, r=[ores], w=[xb.r])
                    P.dma("sp", g2[:, :], bc_d[1], r=[bcd_res], w=[g2.r])
                    if final:
                        P.dma("sp", fg[:, :], fing_d[0:1, :].partition_broadcast(128), w=[fg.r])
                    for tt in range(NTT):
                        h.tt("dve", acc[:, tt, :], acc[:, tt, :], g2[:, :], ALU.mult, [acc.r, g2.r], [acc.r])
                        h.tt("pool", xb[:, tt, :], xb[:, tt, :], acc[:, tt, :], ALU.add, [acc.r, xb.r], [xb.r])
                        if final:
                            rms_stats(P, h, xb[:, tt, :], sq[:, :], st[:, 0:1], st[:, 1:2], [xb.r], sq.r, st.r, D)
                            h.ts("dve", xb[:, tt, :], xb[:, tt, :], st[:, 1:2], None, ALU.mult, None, [xb.r, st.r], [xb.r])
                            h.tt("pool", xb[:, tt, :], xb[:, tt, :], fg[:, :], ALU.mult, [xb.r, fg.r], [xb.r])
                    tk = P.dma("sp", oblk, xb[:, :, :], r=[xb.r], w=[ores])
                    P.wait("sp", [tk])
                P.barrier()
        if ctx is None:
            P.emit()
        print("tail instrs", P.ninstr, {e: len(P.q[e]) for e in P.ENGS})
    return nc


def build_attn(S_LEN=4096, NH=8, HG=4, ctx=None, env=None, pfx="", tokmajor=False):
    from contextlib import ExitStack
    nc = ctx.nc if ctx else bass.Bass("TRN2", target_bir_lowering=False)
    NBK = S_LEN // TB
    NKT = S_LEN // 128
    din, dout = _io(nc, env, pfx)

    x_d = din("x", [S_LEN, D])
    cfm_d = din("c_fm", [128, 16])
    adaw_d = din("ada_w", [D, 6 * D])
    adabfm_d = din("ada_b_fm", [128, 96])
    adab_d = din("ada_b", [1, 6 * D])
    gmix_d = din("g_mix_fm", [128, 16])
    wqkv_d = din("wqkv", [D, 3 * NH * 128])
    ident_d = din("ident", [128, 128])
    masks_d = din("masks", [128, 4, 512])
    ntri_d = din("ntri", [128, 128])
    if tokmajor:
        o_d = dout("o", [S_LEN, NH * 128])
    else:
        oT_d = dout("oT", [NH * 128, S_LEN])
    scale = 128.0 ** -0.5

    with ExitStack() as es:
        if ctx is None:
            P = Prog(nc)
            h = H(P)
            ps = [T(es, nc, f"ps{i}", [128, 512], F32, psum=True) for i in range(8)]
        else:
            P, h, ps = ctx.P, ctx.h, ctx.ps
        ident = T(es, nc, "ident", [128, 128], F32)
        masks = T(es, nc, "masks", [128, 4, 512], F32)
        ntri = T(es, nc, "ntri", [128, 128], F32)
        nones = T(es, nc, "nones", [128, 128], F32)
        gmix = T(es, nc, "gmix", [128, 16], F32)
        gs1 = T(es, nc, "gs1", [128, 16], F32)
        sh1 = T(es, nc, "sh1", [128, 16], F32)
        P.dma("sp", ident[:, :], ident_d, w=[ident.r])
        P.dma("sp", masks[:, :, :], masks_d, w=[masks.r])
        P.dma("sp", ntri[:, :], ntri_d, w=[ntri.r])
        P.dma("sp", gmix[:, :], gmix_d, w=[gmix.r])
        h.memset("pool", nones[:, :], -1.0, [nones.r])
        ntrib = T(es, nc, "ntrib", [128, 128], BF16)
        nonesb = T(es, nc, "nonesb", [128, 128], BF16)
        h.cp("dve", ntrib[:, :], ntri[:, :], [ntri.r], [ntrib.r])
        h.memset("pool", nonesb[:, :], -1.0, [nonesb.r])
        with ExitStack() as es1:
            ada = ada_compute(es1, nc, P, h, ps, cfm_d, adaw_d, adabfm_d, adab_d, fm_secs=[0, 1], bc_secs=[], tag="a")
            h.cp("dve", sh1[:, :], ada[0][:, :], [ada[0].r], [sh1.r])
            h.stt(gs1[:, :], ada[1][:, :], 1.0, gmix[:, :], ALU.add, ALU.mult, [ada[1].r, gmix.r], [gs1.r])
            P.barrier()
        out_toks = []
        for hg in range(NH // HG):
            with ExitStack() as esg:
                QT = T(esg, nc, "QT", [128, HG, S_LEN], BF16)
                KT = T(esg, nc, "KT", [128, HG, S_LEN], BF16)
                Vt = T(esg, nc, "Vt", [128, NKT, HG * 128], BF16)
                with ExitStack() as e1:
                    xb = T(e1, nc, "xb", [128, NTT, D], F32)
                    hT = T(e1, nc, "hT", [128, 16, TB], BF16)
                    sq = T(e1, nc, "sq", [128, D], F32)
                    st = T(e1, nc, "st", [128, 8], F32)
                    wp = [T(e1, nc, f"wp{i}", [128, 16, 128], BF16) for i in range(2)]
                    wv = T(e1, nc, "wv", [128, 16, HG * 128], BF16)
                    c0v = 2 * NH * 128 + hg * HG * 128
                    P.dma("pool", wv[:, :, :], wqkv_d[:, c0v:c0v + HG * 128].rearrange("(k p) n -> p k n", p=128), w=[wv.r])
                    wi = 0
                    for b in range(NBK):
                        t0 = b * TB
                        P.dma("sp", xb[:, :, :], x_d[t0:t0 + TB, :].rearrange("(t p) d -> p t d", p=128), w=[xb.r])
                        for tt in range(NTT):
                            rms_stats(P, h, xb[:, tt, :], sq[:, :], st[:, 0:1], st[:, 1:2], [xb.r], sq.r, st.r, D)
                            h.ts("dve", sq[:, :], xb[:, tt, :], st[:, 1:2], None, ALU.mult, None, [xb.r, st.r], [sq.r])

                            def evac(q, pb, tt=tt):
                                for j in range(4):
                                    kc = q * 4 + j
                                    h.act(hT[:, kc, tt * 128:(tt + 1) * 128], pb[:, j * 128:(j + 1) * 128], AF.Identity,
                                          [pb.r, gs1.r, sh1.r], [hT.r], bias=sh1[:, kc:kc + 1], scale=gs1[:, kc:kc + 1])
                            to_fm(P, h, ps, ident, sq, sq.r, 16, hT, tt, evac)
                        for hl in range(HG):
                            for which, dst in ((0, QT), (1, KT)):
                                wb = wp[wi % 2]
                                wi += 1
                                c0 = which * NH * 128 + (hg * HG + hl) * 128
                                P.dma("pool", wb[:, :, :], wqkv_d[:, c0:c0 + 128].rearrange("(k p) n -> p k n", p=128), w=[wb.r])
                                pb = ps[2 + wi % 2]
                                for k in range(16):
                                    h.mm(pb[:, :], wb[:, k, :], hT[:, k, :], k == 0, k == 15, [wb.r, hT.r], [pb.r])
                                if which == 0:
                                    h.act(dst[:, hl, t0:t0 + TB], pb[:, :], AF.Copy, [pb.r], [dst.r], scale=scale)
                                else:
                                    h.cp("dve", dst[:, hl, t0:t0 + TB], pb[:, :], [pb.r], [dst.r])
                        for tt in range(NTT):
                            pb = ps[4 + tt % 2]
                            for k in range(16):
                                h.mm(pb[:, 0:HG * 128], hT[:, k, tt * 128:(tt + 1) * 128], wv[:, k, :], k == 0, k == 15, [hT.r, wv.r], [pb.r])
                            h.cp("dve" if tt % 2 else "act", Vt[:, b * NTT + tt, :], pb[:, 0:HG * 128], [pb.r], [Vt.r])
                P.barrier()
                with ExitStack() as e2:
                    ex = [T(e2, nc, f"ex{i}", [128, 512], F32) for i in range(2)]
                    spb = [T(e2, nc, f"sp{i}", [128, 512], F32) for i in range(3)]
                    shi = [T(e2, nc, f"shi{i}", [128, 512], BF16) for i in range(4)]
                    slo = [T(e2, nc, f"slo{i}", [128, 512], BF16) for i in range(4)]
                    Rsb = [T(e2, nc, f"Rsb{i}", [128, 512], F32) for i in range(2)]
                    tmpx = [T(e2, nc, f"tmpx{i}", [128, 512], F32) for i in range(2)]
                    Wt = [T(e2, nc, f"Wt{i}", [128, 512], BF16) for i in range(3)]
                    osb = [T(e2, nc, f"osb{i}", [128, 512], F32) for i in range(2)]
                    osT = [T(e2, nc, f"osT{i}", [128, 512], F32) for i in range(2)]
                    tiles = [(hl, qb, idx) for hl in range(HG) for qb in range(NBK) for idx in range(4 * qb + 4)]

                    def geom(tile):
                        hl, qb, idx = tile
                        nk = 4 * qb + 4
                        kt = nk - 1 - idx
                        return hl, qb, idx, nk, kt, kt - 4 * qb

                    def S0(tile, it):
                        hl, qb, idx, nk, kt, jd = geom(tile)
                        pL = ps[it % 2]
                        h.mm(pL[:, :], KT[:, hl, kt * 128:(kt + 1) * 128], QT[:, hl, qb * 512:(qb + 1) * 512], True, True, [KT.r, QT.r], [pL.r])

                    def S1(tile, it):
                        pL = ps[it % 2]
                        e_ = ex[it % 2]
                        s_ = spb[it % 3]
                        h.act(e_[:, :], pL[:, :], AF.Exp, [pL.r], [e_.r])
                        h.act(s_[:, :], e_[:, :], AF.Ln, [e_.r], [s_.r], bias=1.0)

                    def S2(tile, it):
                        hl, qb, idx, nk, kt, jd = geom(tile)
                        s_ = spb[it % 3]
                        if jd >= 0:
                            h.tt("dve", s_[:, :], s_[:, :], masks[:, jd, :], ALU.mult, [s_.r, masks.r], [s_.r])
                        h.cp("dve", shi[it % 4][:, :], s_[:, :], [s_.r], [shi[it % 4].r])
                        h.tt("pool", slo[it % 4][:, :], s_[:, :], shi[it % 4][:, :], ALU.subtract, [s_.r, shi[it % 4].r], [slo[it % 4].r])

                    def S3(tile, it):
                        hl, qb, idx, nk, kt, jd = geom(tile)
                        pE = ps[2 + it % 2]
                        pR = ps[4 + qb % 2]
                        hi_, lo_ = shi[it % 4], slo[it % 4]
                        h.mm(pE[:, :], KT[:, hl, kt * 128:(kt + 1) * 128], QT[:, hl, qb * 512:(qb + 1) * 512], True, False, [KT.r, QT.r], [pE.r])
                        h.mm(pE[:, :], ntrib[:, :], hi_[:, :], False, False, [ntrib.r, hi_.r], [pE.r])
                        h.mm(pE[:, :], ntrib[:, :], lo_[:, :], False, True, [ntrib.r, lo_.r], [pE.r])
                        if idx > 0:
                            h.cp("act", Rsb[it % 2][:, :], pR[:, :], [pR.r], [Rsb[it % 2].r])

                    def S4r(tile, it):
                        hl, qb, idx, nk, kt, jd = geom(tile)
                        pR = ps[4 + qb % 2]
                        hi_, lo_ = shi[it % 4], slo[it % 4]
                        if idx < nk - 1:
                            h.mm(pR[:, :], nonesb[:, :], hi_[:, :], idx == 0, False, [nonesb.r, hi_.r], [pR.r])
                            h.mm(pR[:, :], nonesb[:, :], lo_[:, :], False, idx == nk - 2, [nonesb.r, lo_.r], [pR.r])

                    def S4(tile, it):
                        hl, qb, idx, nk, kt, jd = geom(tile)
                        pE = ps[2 + it % 2]
                        if idx > 0:
                            h.tt("dve", tmpx[it % 2][:, :], pE[:, :], Rsb[it % 2][:, :], ALU.add, [pE.r, Rsb[it % 2].r], [tmpx[it % 2].r])
                        else:
                            h.cp("dve", tmpx[it % 2][:, :], pE[:, :], [pE.r], [tmpx[it % 2].r])

                    def S5(tile, it):
                        w_ = Wt[it % 3]
                        h.act(w_[:, :], tmpx[it % 2][:, :], AF.Exp, [tmpx[it % 2].r], [w_.r])

                    def S6(tile, it):
                        hl, qb, idx, nk, kt, jd = geom(tile)
                        po = ps[6 + qb % 2]
                        w_ = Wt[it % 3]
                        if jd >= 0:
                            h.tt("dve", w_[:, :], w_[:, :], masks[:, jd, :], ALU.mult, [w_.r, masks.r], [w_.r])
                        h.mm(po[:, :], Vt[:, kt, hl * 128:(hl + 1) * 128], w_[:, :], idx == 0, idx == nk - 1, [Vt.r, w_.r], [po.r])
                        if idx == nk - 1:
                            ob = osb[qb % 2]
                            h.cp("dve", ob[:, :], po[:, :], [po.r], [ob.r])
                            hh = hg * HG + hl
                            if tokmajor:
                                pt = po
                                for tt in range(4):
                                    h.tr(pt[:, tt * 128:(tt + 1) * 128], ob[:, tt * 128:(tt + 1) * 128], ident[:, :], [ob.r, ident.r], [pt.r])
                                ot = osT[qb % 2]
                                h.cp("act", ot[:, :], pt[:, :], [pt.r], [ot.r])
                                out_toks.append(P.dma("sp", o_d[qb * 512:(qb + 1) * 512, hh * 128:(hh + 1) * 128].rearrange("(t p) d -> p t d", p=128),
                                                      ot[:, :].rearrange("p (t d) -> p t d", t=4), r=[ot.r]))
                            else:
                                out_toks.append(P.dma("sp", oT_d[hh * 128:(hh + 1) * 128, qb * 512:(qb + 1) * 512], ob[:, :], r=[ob.r]))

                    sched = [(S0, 0), (S1, 1), (S2, 2), (S4r, 4), (S3, 3), (S4, 4), (S5, 5), (S6, 6)]
                    n_t = len(tiles)
                    for n in range(n_t + 6):
                        for fn, off in sched:
                            t = n - off
                            if 0 <= t < n_t:
                                fn(tiles[t], t)
                P.barrier()
        P.wait("sp", out_toks[-48:])
        if ctx is None:
            P.emit()
        print("attn instrs", P.ninstr, {e: len(P.q[e]) for e in P.ENGS})
    return nc


def build_mix0(S_LEN=4096, NHD=16, gm_nblk=4, ctx=None, env=None, pfx="", ygcol=0, ygw=None):
    from contextlib import ExitStack
    nc = ctx.nc if ctx else bass.Bass("TRN2", target_bir_lowering=False)
    din, dout = _io(nc, env, pfx)
    NBK = S_LEN // TB
    NG = NHD // 4
    NX = NHD * 64
    NXC = NX // 128
    NCH = NXC + 2 * NG
    WSSD = 2 * NX + 2 * NG * 128 + NHD

    x_d = din("x", [S_LEN, D])
    xgm_d = din("x_gm", [gm_nblk * TB, D]) if gm_nblk else None
    cfm_d = din("c_fm", [128, 16])
    adaw_d = din("ada_w", [D, 6 * D])
    adabfm_d = din("ada_b_fm", [128, 96])
    adab_d = din("ada_b", [1, 6 * D])
    gmix_d = din("g_mix_fm", [128, 16])
    wssd_d = din("w_ssd", [D, WSSD])
    convw_d = din("conv_w_fm", [128, NCH, 4])
    convb_d = din("conv_b_fm", [128, NCH])
    dtb_d = din("dt_bias", [1, NHD])
    alog_d = din("a_log", [1, NHD])
    dsk_d = din("d_skip", [1, NHD])
    wuv_d = din("w_uv", [D, 4096])
    lng_d = din("ln_g", [1, 2048])
    lnb_d = din("ln_b", [1, 2048])
    wsT_d = din("wsT", [128, 16, 128])
    bsT_d = din("bsT", [128, 16])
    ident_d = din("ident", [128, 128])
    ut_d = din("ut", [128, 128])
    slt_d = din("slt", [128, 128])
    yg_d = dout("yg", [S_LEN, NX])
    yb_d = dout("yb", [gm_nblk * TB, 2048]) if gm_nblk else None

    with ExitStack() as es:
        if ctx is None:
            P = Prog(nc)
            h = H(P)
            ps = [T(es, nc, f"ps{i}", [128, 512], F32, psum=True) for i in range(8)]
        else:
            P, h, ps = ctx.P, ctx.h, ctx.ps

        def cst(name, shape, src, dt=F32):
            t = T(es, nc, name, shape, dt)
            P.dma("sp", t[tuple(slice(None) for _ in shape)], src, w=[t.r])
            return t
        ident = cst("ident", [128, 128], ident_d)
        ut = cst("ut", [128, 128], ut_d)
        slt = cst("slt", [128, 128], slt_d)
        gmix = cst("gmix", [128, 16], gmix_d)
        convw = cst("convw", [128, NCH, 4], convw_d)
        convb = cst("convb", [128, NCH], convb_d)
        dtb = cst("dtb", [128, NHD], dtb_d[0:1, :].partition_broadcast(128))
        aneg = cst("aneg", [128, NHD], alog_d[0:1, :].partition_broadcast(128))
        dsk = cst("dsk", [128, NHD], dsk_d[0:1, :].partition_broadcast(128))
        bsT = cst("bsT", [128, 16], bsT_d)
        wsT = T(es, nc, "wsT", [128, 16, 128], BF16)
        ones = T(es, nc, "ones", [128, 128], F32)
        gs1 = T(es, nc, "gs1", [128, 16], F32)
        sh1 = T(es, nc, "sh1", [128, 16], F32)
        halo = T(es, nc, "halo", [128, NCH, 3], F32)
        Hs = T(es, nc, "Hs", [128, NX], F32)
        Hb = T(es, nc, "Hb", [128, NX], BF16)
        h.memset("pool", ones[:, :], 1.0, [ones.r])
        h.memset("pool", halo[:, :, :], 0.0, [halo.r])
        h.memset("pool", Hs[:, :], 0.0, [Hs.r])
        h.memset("pool", Hb[:, :], 0.0, [Hb.r])
        h.act(aneg[:, :], aneg[:, :], AF.Exp, [aneg.r], [aneg.r])
        h.ts("dve", aneg[:, :], aneg[:, :], -1.0, None, ALU.mult, None, [aneg.r], [aneg.r])
        with ExitStack() as es1:
            wraw = T(es1, nc, "wsraw", [128, 16, 128], F32)
            P.dma("sp", wraw[:, :, :], wsT_d, w=[wraw.r])
            h.tt("dve", wsT[:, :, :], wraw[:, :, :], ut[:, :].unsqueeze(1).to_broadcast([128, 16, 128]), ALU.mult,
                 [wraw.r, ut.r], [wsT.r])
            ada = ada_compute(es1, nc, P, h, ps, cfm_d, adaw_d, adabfm_d, adab_d, fm_secs=[0, 1], bc_secs=[], tag="m")
            h.cp("dve", sh1[:, :], ada[0][:, :], [ada[0].r], [sh1.r])
            h.stt(gs1[:, :], ada[1][:, :], 1.0, gmix[:, :], ALU.add, ALU.mult, [ada[1].r, gmix.r], [gs1.r])
            P.barrier()
        out_toks = []
        for kind, b in [("ssd", i) for i in range(NBK)] + [("gm", i) for i in range(gm_nblk)]:
            t0 = b * TB
            xsrc = x_d if kind == "ssd" else xgm_d
            with ExitStack() as esb:
                hT = T(esb, nc, "hT", [128, 16, TB], BF16)
                with ExitStack() as e1:
                    xb = T(e1, nc, "xb", [128, NTT, D], F32)
                    sq = T(e1, nc, "sq", [128, D], F32)
                    st = T(e1, nc, "st", [128, 8], F32)
                    P.dma("sp", xb[:, :, :], xsrc[t0:t0 + TB, :].rearrange("(t p) d -> p t d", p=128), w=[xb.r])
                    for tt in range(NTT):
                        rms_stats(P, h, xb[:, tt, :], sq[:, :], st[:, 0:1], st[:, 1:2], [xb.r], sq.r, st.r, D)
                        h.ts("dve", sq[:, :], xb[:, tt, :], st[:, 1:2], None, ALU.mult, None, [xb.r, st.r], [sq.r])

                        def evac(q, pb, tt=tt):
                            for j in range(4):
                                kc = q * 4 + j
                                h.act(hT[:, kc, tt * 128:(tt + 1) * 128], pb[:, j * 128:(j + 1) * 128], AF.Identity,
                                      [pb.r, gs1.r, sh1.r], [hT.r], bias=sh1[:, kc:kc + 1], scale=gs1[:, kc:kc + 1])
                        to_fm(P, h, ps, ident, sq, sq.r, 16, hT, tt, evac)
                    P.barrier()
                if kind == "ssd":
                    with ExitStack() as e2:
                        zs = T(e2, nc, "zs", [128, NTT, NX], F32)
                        xcf = T(e2, nc, "xcf", [128, NXC, TB], F32)
                        BCb = T(e2, nc, "BCb", [128, 2 * NG, TB], BF16)
                        Bf = T(e2, nc, "Bf", [128, NG, TB], F32)
                        xtok = T(e2, nc, "xtok", [128, NTT, NX], F32)
                        Btok = T(e2, nc, "Btok", [128, NTT, NG * 128], BF16)
                        dt = T(e2, nc, "dt", [128, NTT, NHD], F32)
                        with ExitStack() as e3:
                            wst = [T(e3, nc, f"wst{i}", [128, 16, 256], BF16) for i in range(2)]
                            wdt = T(e3, nc, "wdt", [128, 16, NHD], BF16)
                            raw = [T(e3, nc, f"raw{i}", [128, 3 + TB], F32) for i in range(2)]
                            cacc = [T(e3, nc, f"cacc{i}", [128, TB], F32) for i in range(2)]
                            wi = 0
                            for gz in range(NX // 256):
                                wb = wst[wi % 2]
                                wi += 1
                                P.dma("pool", wb[:, :, :], wssd_d[:, gz * 256:(gz + 1) * 256].rearrange("(k p) n -> p k n", p=128), w=[wb.r])
                                for tt in range(NTT):
                                    pb = ps[2 + tt % 2]
                                    for k in range(16):
                                        h.mm(pb[:, 0:256], hT[:, k, tt * 128:(tt + 1) * 128], wb[:, k, :], k == 0, k == 15, [hT.r, wb.r], [pb.r])
                                    h.act(zs[:, tt, gz * 256:(gz + 1) * 256], pb[:, 0:256], AF.Silu, [pb.r], [zs.r])
                            for gx in range(NCH // 2):
                                wb = wst[wi % 2]
                                wi += 1
                                c0 = NX + gx * 256
                                P.dma("pool", wb[:, :, :], wssd_d[:, c0:c0 + 256].rearrange("(k p) n -> p k n", p=128), w=[wb.r])
                                for half in range(2):
                                    cc = gx * 2 + half
                                    pb = ps[4 + cc % 2]
                                    rw = raw[cc % 2]
                                    ca = cacc[cc % 2]
                                    for k in range(16):
                                        h.mm(pb[:, :], wb[:, k, half * 128:(half + 1) * 128], hT[:, k, :], k == 0, k == 15, [wb.r, hT.r], [pb.r])
                                    h.cp("pool", rw[:, 0:3], halo[:, cc, :], [halo.r], [rw.r])
                                    h.cp("act", rw[:, 3:3 + TB], pb[:, :], [pb.r], [rw.r])
                                    h.cp("pool", halo[:, cc, :], rw[:, TB:TB + 3], [rw.r], [halo.r])
                                    h.ts("dve", ca[:, :], rw[:, 0:TB], convw[:, cc, 0:1], None, ALU.mult, None, [rw.r, convw.r], [ca.r])
                                    for k in range(1, 4):
                                        h.stt(ca[:, :], rw[:, k:k + TB], convw[:, cc, k:k + 1], ca[:, :], ALU.mult, ALU.add,
                                              [rw.r, convw.r, ca.r], [ca.r])
                                    if cc < NXC:
                                        h.act(xcf[:, cc, :], ca[:, :], AF.Silu, [ca.r, convb.r], [xcf.r], bias=convb[:, cc:cc + 1])
                                    else:
                                        h.act(BCb[:, cc - NXC, :], ca[:, :], AF.Silu, [ca.r, convb.r], [BCb.r], bias=convb[:, cc:cc + 1])
                                        if cc - NXC < NG:
                                            h.act(Bf[:, cc - NXC, :], ca[:, :], AF.Silu, [ca.r, convb.r], [Bf.r], bias=convb[:, cc:cc + 1])
                            P.dma("pool", wdt[:, :, :], wssd_d[:, WSSD - NHD:WSSD].rearrange("(k p) n -> p k n", p=128), w=[wdt.r])
                            for tt in range(NTT):
                                pb = ps[6 + tt % 2]
                                for k in range(16):
                                    h.mm(pb[:, 0:NHD], hT[:, k, tt * 128:(tt + 1) * 128], wdt[:, k, :], k == 0, k == 15, [hT.r, wdt.r], [pb.r])
                                h.tt("dve", dt[:, tt, :], pb[:, 0:NHD], dtb[:, :], ALU.add, [pb.r, dtb.r], [dt.r])
                            h.act(dt[:, :, :], dt[:, :, :], AF.Exp, [dt.r], [dt.r])
                            h.act(dt[:, :, :], dt[:, :, :], AF.Ln, [dt.r], [dt.r], bias=1.0)
                            for tt in range(NTT):
                                for q in range(NXC // 4):
                                    pb = ps[q % 2]
                                    for j in range(4):
                                        h.tr(pb[:, j * 128:(j + 1) * 128], xcf[:, q * 4 + j, tt * 128:(tt + 1) * 128], ident[:, :],
                                             [xcf.r, ident.r], [pb.r])
                                    h.cp("dve" if q % 2 else "act", xtok[:, tt, q * 512:(q + 1) * 512], pb[:, :], [pb.r], [xtok.r])
                                pb = ps[2 + tt % 2]
                                for g in range(NG):
                                    h.tr(pb[:, g * 128:(g + 1) * 128], Bf[:, g, tt * 128:(tt + 1) * 128], ident[:, :], [Bf.r, ident.r], [pb.r])
                                h.cp("dve", Btok[:, tt, :], pb[:, 0:NG * 128], [pb.r], [Btok.r])
                        P.barrier()
                        with ExitStack() as e4:
                            a_sb = T(e4, nc, "a_sb", [128, NHD], F32)
                            acs = T(e4, nc, "acs", [128, 4, NHD], F32)
                            CBm = T(e4, nc, "CBm", [128, NG, 128], F32)
                            aU = [T(e4, nc, f"aU{i}", [128, 128], F32) for i in range(4)]
                            Eq = [T(e4, nc, f"Eq{i}", [128, 4, 128], F32) for i in range(2)]
                            Mq = [T(e4, nc, f"Mq{i}", [128, 4, 128], BF16) for i in range(2)]
                            xdt = T(e4, nc, "xdt", [128, NX], BF16)
                            xs = T(e4, nc, "xs", [128, NX], BF16)
                            t1 = T(e4, nc, "t1", [128, NX], F32)
                            t3 = T(e4, nc, "t3", [128, NX], F32)
                            yo = [T(e4, nc, f"yo{i}", [128, NX], F32) for i in range(2)]
                            pA, pCB = ps[2], ps[3]
                            pYd = [ps[0], ps[1]]
                            pYo = [ps[6], ps[7]]
                            for tt in range(NTT):
                                cols = slice(tt * 128, (tt + 1) * 128)
                                h.tt("dve", a_sb[:, :], dt[:, tt, :], aneg[:, :], ALU.mult, [dt.r, aneg.r], [a_sb.r])
                                h.mm(pA[:, 0:NHD], ut[:, :], a_sb[:, :], True, True, [ut.r, a_sb.r], [pA.r])
                                h.mm(pA[:, 32:32 + NHD], ones[:, :], a_sb[:, :], True, True, [ones.r, a_sb.r], [pA.r])
                                h.cp("dve", acs[:, 0, :], pA[:, 0:NHD], [pA.r], [acs.r])
                                h.act(acs[:, 1, :], pA[:, 0:NHD], AF.Exp, [pA.r], [acs.r])
                                h.act(acs[:, 2, :], pA[:, 32:32 + NHD], AF.Exp, [pA.r], [acs.r])
                                h.tt("dve", acs[:, 3, :], pA[:, 32:32 + NHD], acs[:, 0, :], ALU.subtract, [pA.r, acs.r], [acs.r])
                                h.act(acs[:, 3, :], acs[:, 3, :], AF.Exp, [acs.r], [acs.r])
                                h.tt("dve", acs[:, 3, :], acs[:, 3, :], dt[:, tt, :], ALU.mult, [acs.r, dt.r], [acs.r])
                                for g in range(NG):
                                    h.mm(pCB[:, g * 128:(g + 1) * 128], BCb[:, g, cols], BCb[:, NG + g, cols], True, True, [BCb.r], [pCB.r])
                                h.tt("dve", CBm[:, :, :], pCB[:, 0:NG * 128].rearrange("p (g l) -> p g l", g=NG),
                                     ut[:, :].unsqueeze(1).to_broadcast([128, NG, 128]), ALU.mult, [pCB.r, ut.r], [CBm.r])
                                h.tt("dve", xdt[:, :].rearrange("p (a b) -> p a b", a=NHD), xtok[:, tt, :].rearrange("p (a b) -> p a b", a=NHD),
                                     dt[:, tt, :].unsqueeze(2).to_broadcast([128, NHD, 64]), ALU.mult, [xtok.r, dt.r], [xdt.r])
                                h.tt("pool", xs[:, :].rearrange("p (a b) -> p a b", a=NHD), xtok[:, tt, :].rearrange("p (a b) -> p a b", a=NHD),
                                     acs[:, 3, :].unsqueeze(2).to_broadcast([128, NHD, 64]), ALU.mult, [xtok.r, acs.r], [xs.r])
                                for g in range(NG):
                                    pS = ps[4 + g % 2]
                                    E_ = Eq[g % 2]
                                    M_ = Mq[g % 2]
                                    for r in range(4):
                                        hd = g * 4 + r
                                        au = aU[r]
                                        h.ts("dve", au[:, :], ut[:, :], a_sb[:, hd:hd + 1], None, ALU.mult, None, [ut.r, a_sb.r], [au.r])
                                        h.mm(pS[:, r * 128:(r + 1) * 128], slt[:, :], au[:, :], True, True, [slt.r, au.r], [pS.r])
                                    h.act(E_[:, :, :], pS[:, :].rearrange("p (a b) -> p a b", a=4), AF.Exp, [pS.r], [E_.r])
                                    h.tt("dve", M_[:, :, :], E_[:, :, :], CBm[:, g, :].unsqueeze(1).to_broadcast([128, 4, 128]), ALU.mult,
                                         [E_.r, CBm.r], [M_.r])
                                    for r in range(4):
                                        hd = g * 4 + r
                                        bank, c_ = hd // 8, (hd % 8) * 64
                                        h.mm(pYd[bank][:, c_:c_ + 64], M_[:, r, :], xdt[:, hd * 64:(hd + 1) * 64], True, True,
                                             [M_.r, xdt.r], [pYd[bank].r])
                                        h.mm(pYo[bank][:, c_:c_ + 64], BCb[:, NG + g, cols], Hb[:, hd * 64:(hd + 1) * 64], True, True,
                                             [BCb.r, Hb.r], [pYo[bank].r])
                                y_ = yo[tt % 2]
                                for bank in range(NHD // 8):
                                    cs = slice(bank * 512, (bank + 1) * 512)
                                    hs = slice(bank * 8, (bank + 1) * 8)
                                    h.tt("dve", t1[:, cs].rearrange("p (a b) -> p a b", a=8), pYo[bank][:, :].rearrange("p (a b) -> p a b", a=8),
                                         acs[:, 1, hs].unsqueeze(2).to_broadcast([128, 8, 64]), ALU.mult, [pYo[bank].r, acs.r], [t1.r])
                                    h.tt("dve", t1[:, cs], t1[:, cs], pYd[bank][:, :], ALU.add, [t1.r, pYd[bank].r], [t1.r])
                                    h.tt("pool", t3[:, cs].rearrange("p (a b) -> p a b", a=8), xtok[:, tt, cs].rearrange("p (a b) -> p a b", a=8),
                                         dsk[:, hs].unsqueeze(2).to_broadcast([128, 8, 64]), ALU.mult, [xtok.r, dsk.r], [t3.r])
                                    h.tt("pool", t3[:, cs], t3[:, cs], t1[:, cs], ALU.add, [t1.r, t3.r], [t3.r])
                                    h.tt("pool", y_[:, cs], t3[:, cs], zs[:, tt, cs], ALU.mult, [t3.r, zs.r], [y_.r])
                                out_toks.append(P.dma("sp", yg_d[t0 + tt * 128:t0 + (tt + 1) * 128, :], y_[:, :], r=[y_.r]))
                                for hd in range(NHD):
                                    g = hd // 4
                                    bank, c_ = hd // 8, (hd % 8) * 64
                                    h.mm(pYd[bank][:, c_:c_ + 64], Btok[:, tt, g * 128:(g + 1) * 128], xs[:, hd * 64:(hd + 1) * 64], True, True,
                                         [Btok.r, xs.r], [pYd[bank].r])
                                h.tt("dve", Hs[:, :].rearrange("p (a b) -> p a b", a=NHD), Hs[:, :].rearrange("p (a b) -> p a b", a=NHD),
                                     acs[:, 2, :].unsqueeze(2).to_broadcast([128, NHD, 64]), ALU.mult, [Hs.r, acs.r], [Hs.r])
                                for bank in range(NHD // 8):
                                    cs = slice(bank * 512, (bank + 1) * 512)
                                    h.tt("dve", Hs[:, cs], Hs[:, cs], pYd[bank][:, :], ALU.add, [Hs.r, pYd[bank].r], [Hs.r])
                                h.cp("act", Hb[:, :], Hs[:, :], [Hs.r], [Hb.r])
                        P.barrier()
                if kind == "gm":
                    r0 = b * TB
                    with ExitStack() as e5:
                        wst = [T(e5, nc, f"wuv{i}", [128, 16, 256], BF16) for i in range(2)]
                        ug = T(e5, nc, "ug", [128, NTT, 2048], F32)
                        vg = T(e5, nc, "vg", [128, NTT, 2048], F32)
                        lng = T(e5, nc, "lng", [128, 2048], F32)
                        lnb = T(e5, nc, "lnb", [128, 2048], F32)
                        vn = T(e5, nc, "vn", [128, 2048], BF16)
                        bst = T(e5, nc, "bst", [128, 8, 6], F32)
                        mv = T(e5, nc, "mv", [128, 4], F32)
                        P.dma("sp", lng[:, :], lng_d[0:1, :].partition_broadcast(128), w=[lng.r])
                        P.dma("sp", lnb[:, :], lnb_d[0:1, :].partition_broadcast(128), w=[lnb.r])
                        wi = 0
                        for gu in range(16):
                            wb = wst[wi % 2]
                            wi += 1
                            P.dma("pool", wb[:, :, :], wuv_d[:, gu * 256:(gu + 1) * 256].rearrange("(k p) n -> p k n", p=128), w=[wb.r])
                            dst = ug if gu < 8 else vg
                            cg = (gu % 8) * 256
                            for tt in range(NTT):
                                pb = ps[2 + tt % 2]
                                for k in range(16):
                                    h.mm(pb[:, 0:256], hT[:, k, tt * 128:(tt + 1) * 128], wb[:, k, :], k == 0, k == 15, [hT.r, wb.r], [pb.r])
                                h.act(dst[:, tt, cg:cg + 256], pb[:, 0:256], AF.Gelu, [pb.r], [dst.r])
                        for tt in range(NTT):
                            for q in range(4):
                                h.P.op("dve", lambda e, q=q, tt=tt: e.bn_stats(out=bst[:, q, :], in_=vg[:, tt, q * 512:(q + 1) * 512]),
                                       [vg.r], [bst.r])
                            h.P.op("dve", lambda e: e.bn_aggr(out=mv[:, 0:2], in_=bst[:, 0:4, :].rearrange("p a b -> p (a b)")), [bst.r], [mv.r])
                            h.ts("dve", mv[:, 2:3], mv[:, 1:2], EPS, None, ALU.add, None, [mv.r], [mv.r])
                            h.act(mv[:, 2:3], mv[:, 2:3], AF.Sqrt, [mv.r], [mv.r])
                            h.recip(mv[:, 2:3], mv[:, 2:3], [mv.r], [mv.r])
                            h.ts("dve", vg[:, tt, :], vg[:, tt, :], mv[:, 0:1], mv[:, 2:3], ALU.subtract, ALU.mult, [vg.r, mv.r], [vg.r])
                            h.tt("pool", vg[:, tt, :], vg[:, tt, :], lng[:, :], ALU.mult, [vg.r, lng.r], [vg.r])
                            h.tt("pool", vn[:, :], vg[:, tt, :], lnb[:, :], ALU.add, [vg.r, lnb.r], [vn.r])
                            pv = [ps[0], ps[1], ps[6], ps[7]]
                            for g in range(16):
                                pb = pv[g // 4]
                                h.mm(pb[:, (g % 4) * 128:(g % 4 + 1) * 128], wsT[:, g, :], vn[:, g * 128:(g + 1) * 128], True, True,
                                     [wsT.r, vn.r], [pb.r])
                            for q in range(4):
                                cs = slice(q * 512, (q + 1) * 512)
                                h.tt("dve", vg[:, tt, cs].rearrange("p (a b) -> p a b", a=4), pv[q][:, :].rearrange("p (a b) -> p a b", a=4),
                                     bsT[:, q * 4:(q + 1) * 4].unsqueeze(2).to_broadcast([128, 4, 128]), ALU.add, [pv[q].r, bsT.r], [vg.r])
                            h.tt("pool", vg[:, tt, :], vg[:, tt, :], ug[:, tt, :], ALU.mult, [vg.r, ug.r], [vg.r])
                            out_toks.append(P.dma("sp", yb_d[r0 + tt * 128:r0 + (tt + 1) * 128, :], vg[:, tt, :], r=[vg.r]))
                    P.barrier()
        P.wait("sp", out_toks[-48:])
        if ctx is None:
            P.emit()
        print("mix0 instrs", P.ninstr, {e: len(P.q[e]) for e in P.ENGS})
    return nc


def _fm(v):
    return np.ascontiguousarray(np.asarray(v, dtype=np.float32).reshape(-1, 128).T)


def _c(a):
    return np.ascontiguousarray(np.asarray(a, dtype=np.float32))


def _consts():
    f = np.float32
    jj = np.arange(128)
    s_ = jj[:, None, None]
    j_ = np.arange(4)[None, :, None]
    t_ = np.arange(512)[None, None, :]
    return dict(
        ident=np.eye(128, dtype=f),
        ut=(jj[:, None] <= jj[None, :]).astype(f),
        slt=(jj[:, None] > jj[None, :]).astype(f),
        masks=((j_ * 128 + s_) < t_).astype(f),
        ntri=-(jj[:, None] >= jj[None, :]).astype(f),
        summ=(jj[:, None] // 8 == np.arange(16)[None, :]).astype(f),
    )


def _mix0_inputs(p, in0_w, conv_w, conv_b, dt_bias, a_log, d_skip, ln_g, ln_b, ws, bs):
    zc = slice(p * 1024, (p + 1) * 1024)
    xc = slice(2048 + p * 1024, 2048 + (p + 1) * 1024)
    Bc = slice(4096 + p * 512, 4096 + (p + 1) * 512)
    Cc = slice(5120 + p * 512, 5120 + (p + 1) * 512)
    dc = slice(6144 + p * 16, 6144 + (p + 1) * 16)
    w_ssd = np.concatenate([in0_w[:, zc], in0_w[:, xc], in0_w[:, Bc], in0_w[:, Cc], in0_w[:, dc]], axis=1)
    cch = np.concatenate([np.arange(p * 1024, (p + 1) * 1024), 2048 + np.arange(p * 512, (p + 1) * 512),
                          3072 + np.arange(p * 512, (p + 1) * 512)])
    cw = conv_w[:, cch]
    cb = conv_b[cch]
    hs = slice(p * 16, (p + 1) * 16)
    return dict(w_ssd=_c(w_ssd), conv_w_fm=_c(cw.T.reshape(16, 128, 4).transpose(1, 0, 2)), conv_b_fm=_fm(cb),
                dt_bias=_c(dt_bias[None, hs]), a_log=_c(a_log[None, hs]), d_skip=_c(d_skip[None, hs]),
                w_uv=_c(in0_w[:, 6176:]), ln_g=_c(ln_g[None]), ln_b=_c(ln_b[None]),
                wsT=_c(ws.transpose(2, 0, 1)), bsT=_c(bs.T))


def kernel_unfused(x, c, ada_w, ada_b, norm_mix_g, norm_ffn_g, in0_w, conv_w, conv_b, dt_bias, a_log, d_skip, ssd_norm_g,
           gmlp_ln_g, gmlp_ln_b, gmlp_ws, gmlp_bs, out0_w, sb_qkv_w, sb_out_w, peer_wq, peer_keys, peer_u, peer_v, final_g):
    g = {k: np.asarray(v) for k, v in locals().items()}
    x = g["x"]
    c = g["c"]
    K = _consts()
    cores = list(range(8))
    HALF = 2048

    def ada_in(layer, b):
        return dict(c_fm=_fm(c[b]), ada_w=_c(g["ada_w"][layer]), ada_b_fm=_fm(g["ada_b"][layer]), ada_b=_c(g["ada_b"][layer][None]))

    nc1 = build_mix0(4096, 16, 4)
    mi = [_mix0_inputs(p, g["in0_w"][0], g["conv_w"][0], g["conv_b"][0], g["dt_bias"][0], g["a_log"][0], g["d_skip"][0],
                       g["gmlp_ln_g"][0], g["gmlp_ln_b"][0], g["gmlp_ws"][0], g["gmlp_bs"][0]) for p in range(2)]
    maps = []
    for core in cores:
        b, p = divmod(core, 2)
        d = dict(x=_c(x[b]), x_gm=_c(x[b, p * HALF:(p + 1) * HALF]), g_mix_fm=_fm(g["norm_mix_g"][0]),
                 ident=K["ident"], ut=K["ut"], slt=K["slt"])
        d.update(ada_in(0, b))
        d.update(mi[p])
        maps.append(d)
    r1 = run_bass_kernel_spmd(nc1, maps, core_ids=cores).results
    del maps
    nc2 = build_tail(HALF, 4096, final=False, ya_norm=True)
    maps = []
    for core in cores:
        b, p = divmod(core, 2)
        rows = slice(p * HALF, (p + 1) * HALF)
        mix = np.concatenate([r1[2 * b]["yg"][rows], r1[2 * b + 1]["yg"][rows], r1[core]["yb"]], axis=1)
        d = dict(x=_c(x[b, rows]), mix=_c(mix), wo=_c(g["out0_w"][0]), g_ffn_fm=_fm(g["norm_ffn_g"][0]), wq=_c(g["peer_wq"][0]),
                 keys=_c(g["peer_keys"][0].reshape(16, 128, 128)), U=_c(g["peer_u"][0]), V=_c(g["peer_v"][0]),
                 final_g=_c(g["final_g"][None]), g_ya_fm=_fm(g["ssd_norm_g"][0]), ident=K["ident"], summ=K["summ"])
        d.update(ada_in(0, b))
        maps.append(d)
    r2 = run_bass_kernel_spmd(nc2, maps, core_ids=cores).results
    del maps, r1
    nc3 = build_attn(4096, 8, 4)
    qkv = g["sb_qkv_w"][0]
    maps = []
    for core in cores:
        b, p = divmod(core, 2)
        hs = slice(p * 1024, (p + 1) * 1024)
        wqkv = np.concatenate([qkv[:, 0:2048][:, hs], qkv[:, 2048:4096][:, hs], qkv[:, 4096:6144][:, hs]], axis=1)
        xf = np.concatenate([r2[2 * b]["out"], r2[2 * b + 1]["out"]], axis=0)
        d = dict(x=_c(xf), g_mix_fm=_fm(g["norm_mix_g"][1]), wqkv=_c(wqkv), ident=K["ident"], masks=K["masks"], ntri=K["ntri"])
        d.update(ada_in(1, b))
        maps.append(d)
    r3 = run_bass_kernel_spmd(nc3, maps, core_ids=cores).results
    del maps
    nc4 = build_tail(HALF, 2048, final=True, ya_norm=False)
    maps = []
    for core in cores:
        b, p = divmod(core, 2)
        rows = slice(p * HALF, (p + 1) * HALF)
        mix = np.concatenate([r3[2 * b]["oT"][:, rows].T, r3[2 * b + 1]["oT"][:, rows].T], axis=1)
        d = dict(x=_c(r2[core]["out"]), mix=_c(mix), wo=_c(g["sb_out_w"][0]), g_ffn_fm=_fm(g["norm_ffn_g"][1]), wq=_c(g["peer_wq"][1]),
                 keys=_c(g["peer_keys"][1].reshape(16, 128, 128)), U=_c(g["peer_u"][1]), V=_c(g["peer_v"][1]),
                 final_g=_c(g["final_g"][None]), g_ya_fm=_fm(g["ssd_norm_g"][0]), ident=K["ident"], summ=K["summ"])
        d.update(ada_in(1, b))
        maps.append(d)
    r4 = run_bass_kernel_spmd(nc4, maps, core_ids=cores).results
    out = np.empty((4, 4096, 2048), dtype=np.float32)
    for core in cores:
        b, p = divmod(core, 2)
        out[b, p * HALF:(p + 1) * HALF] = r4[core]["out"]
    return out


def build_fused(S_LEN=4096):
    from contextlib import ExitStack
    nc = bass.Bass("TRN2", target_bir_lowering=False)
    HALF = S_LEN // 2

    def ein(name, shape):
        return nc.dram_tensor(name, shape, F32, kind="ExternalInput").ap()

    x_d = ein("x", [S_LEN, D])
    cfm = ein("c_fm", [128, 16])
    adaw = ein("ada_w", [2, D, 6 * D])
    adabfm = ein("ada_b_fm", [2, 128, 96])
    adab = ein("ada_b", [2, 1, 6 * D])
    gmix = ein("g_mix_fm", [2, 128, 16])
    gffn = ein("g_ffn_fm", [2, 128, 16])
    wssd = ein("w_ssd", [2, D, 3088])
    convw = ein("conv_w_fm", [2, 128, 16, 4])
    convb = ein("conv_b_fm", [2, 128, 16])
    dtb = ein("dt_bias", [2, 1, 16])
    alog = ein("a_log", [2, 1, 16])
    dsk = ein("d_skip", [2, 1, 16])
    wuv = ein("w_uv", [D, 4096])
    lng = ein("ln_g", [1, 2048])
    lnb = ein("ln_b", [1, 2048])
    wsT = ein("wsT", [128, 16, 128])
    bsT = ein("bsT", [128, 16])
    wo0 = ein("out0_w", [4096, D])
    gya = ein("g_ya_fm", [128, 16])
    wq = ein("wq", [2, D, D])
    keys = ein("keys", [2, 16, 128, 128])
    U = ein("U", [2, 16384, D])
    V = ein("V", [2, 16384, D])
    fing = ein("final_g", [1, D])
    wqkv = ein("wqkv", [D, 3 * D])
    wo1 = ein("sb_out_w", [D, D])
    sel = ein("sel", [128, 2])
    ident = ein("ident", [128, 128])
    ut = ein("ut", [128, 128])
    slt = ein("slt", [128, 128])
    masks = ein("masks", [128, 4, 512])
    ntri = ein("ntri", [128, 128])
    summ = ein("summ", [128, 16])
    mixA = nc.dram_tensor("mixA", [S_LEN, 4096], F32, kind="Internal").ap()
    x2 = nc.dram_tensor("x2", [S_LEN, D], F32, kind="Internal").ap()
    o_d = nc.dram_tensor("o_int", [S_LEN, D], F32, kind="Internal").ap()
    out_d = nc.dram_tensor("out", [HALF, D], F32, kind="ExternalOutput").ap()

    def ada_env(layer):
        return dict(c_fm=cfm, ada_w=adaw[layer], ada_b_fm=adabfm[layer], ada_b=adab[layer])

    with ExitStack() as es:
        ctx = Ctx(nc, es)
        for p in range(2):
            env = dict(x=x_d, x_gm=x_d, g_mix_fm=gmix[0], w_ssd=wssd[p], conv_w_fm=convw[p], conv_b_fm=convb[p],
                       dt_bias=dtb[p], a_log=alog[p], d_skip=dsk[p], w_uv=wuv, ln_g=lng, ln_b=lnb, wsT=wsT, bsT=bsT,
                       ident=ident, ut=ut, slt=slt, yg=mixA[:, p * 1024:(p + 1) * 1024], yb=mixA[:, 2048:4096])
            env.update(ada_env(0))
            build_mix0(S_LEN, 16, gm_nblk=(S_LEN // TB if p == 0 else 0), ctx=ctx, env=env, pfx=f"m{p}_")
        env = dict(x=x_d, mix=mixA, wo=wo0, g_ffn_fm=gffn[0], wq=wq[0], keys=keys[0], U=U[0], V=V[0], final_g=fing,
                   g_ya_fm=gya, ident=ident, summ=summ, out=x2)
        env.update(ada_env(0))
        build_tail(S_LEN, 4096, final=False, ya_norm=True, ctx=ctx, env=env, pfx="t0_")
        env = dict(x=x2, g_mix_fm=gmix[1], wqkv=wqkv, ident=ident, masks=masks, ntri=ntri, o=o_d)
        env.update(ada_env(1))
        build_attn(S_LEN, 16, 4, ctx=ctx, env=env, pfx="a_", tokmajor=True)
        env = dict(x=x2, mix=o_d, wo=wo1, g_ffn_fm=gffn[1], wq=wq[1], keys=keys[1], U=U[1], V=V[1], final_g=fing,
                   g_ya_fm=gya, ident=ident, summ=summ, out=out_d, sel=sel)
        env.update(ada_env(1))
        build_tail(HALF, 2048, final=True, ya_norm=False, ctx=ctx, env=env, pfx="t1_", blend=True)
        ctx.P.emit()
        print("fused instrs", ctx.P.ninstr, {e: len(ctx.P.q[e]) for e in ctx.P.ENGS})
    return nc


def kernel(x, c, ada_w, ada_b, norm_mix_g, norm_ffn_g, in0_w, conv_w, conv_b, dt_bias, a_log, d_skip, ssd_norm_g,
           gmlp_ln_g, gmlp_ln_b, gmlp_ws, gmlp_bs, out0_w, sb_qkv_w, sb_out_w, peer_wq, peer_keys, peer_u, peer_v, final_g):
    g = {k: np.asarray(v) for k, v in locals().items()}
    K = _consts()
    cores = list(range(8))
    mi = [_mix0_inputs(p, g["in0_w"][0], g["conv_w"][0], g["conv_b"][0], g["dt_bias"][0], g["a_log"][0], g["d_skip"][0],
                       g["gmlp_ln_g"][0], g["gmlp_ln_b"][0], g["gmlp_ws"][0], g["gmlp_bs"][0]) for p in range(2)]
    shared = dict(
        ada_w=_c(g["ada_w"]), ada_b_fm=_c(np.stack([_fm(g["ada_b"][l]) for l in range(2)])), ada_b=_c(g["ada_b"][:, None, :]),
        g_mix_fm=_c(np.stack([_fm(g["norm_mix_g"][l]) for l in range(2)])),
        g_ffn_fm=_c(np.stack([_fm(g["norm_ffn_g"][l]) for l in range(2)])),
        w_uv=mi[0]["w_uv"], ln_g=mi[0]["ln_g"], ln_b=mi[0]["ln_b"], wsT=mi[0]["wsT"], bsT=mi[0]["bsT"],
        out0_w=_c(g["out0_w"][0]), g_ya_fm=_fm(g["ssd_norm_g"][0]), wq=_c(g["peer_wq"]),
        keys=_c(g["peer_keys"].reshape(2, 16, 128, 128)), U=_c(g["peer_u"]), V=_c(g["peer_v"]),
        final_g=_c(g["final_g"][None]), wqkv=_c(g["sb_qkv_w"][0]), sb_out_w=_c(g["sb_out_w"][0]),
        ident=K["ident"], ut=K["ut"], slt=K["slt"], masks=K["masks"], ntri=K["ntri"], summ=K["summ"])
    for k in ("w_ssd", "conv_w_fm", "conv_b_fm", "dt_bias", "a_log", "d_skip"):
        shared[k] = _c(np.stack([mi[0][k], mi[1][k]]))
    nc = build_fused(4096)
    maps = []
    for core in cores:
        b, p = divmod(core, 2)
        d = dict(shared)
        d["x"] = _c(g["x"][b])
        d["c_fm"] = _fm(g["c"][b])
        selv = np.zeros((128, 2), dtype=np.float32)
        selv[:, p] = 1.0
        d["sel"] = selv
        maps.append(d)
    res = run_bass_kernel_spmd(nc, maps, core_ids=cores).results
    out = np.empty((4, 4096, 2048), dtype=np.float32)
    for core in cores:
        b, p = divmod(core, 2)
        out[b, p * 2048:(p + 1) * 2048] = res[core]["out"]
    return out
```

```python
import numpy as np
import concourse.bass as bass
import concourse.mybir as mybir
from concourse.bass_utils import run_bass_kernel_spmd

F32 = mybir.dt.float32
BF16 = mybir.dt.bfloat16
AF = mybir.ActivationFunctionType
ALU = mybir.AluOpType
AX = mybir.AxisListType

D = 2048
DC = 16
EPS = 1e-6
NEG = -1.0e30


class Res:
    __slots__ = ("w", "rs", "name")

    def __init__(self, name=""):
        self.w = None
        self.rs = {}
        self.name = name


class Prog:
    ENGS = ["pe", "act", "dve", "pool", "sp"]

    def __init__(self, nc, n_dma_sems=40):
        self.nc = nc
        self.q = {e: [] for e in self.ENGS}
        self.sem = {e: nc.alloc_semaphore(name=f"sem_{e}") for e in self.ENGS}
        self.cnt = {e: 0 for e in self.ENGS}
        self.seen = {e: {} for e in self.ENGS}
        self.dsem = [nc.alloc_semaphore(name=f"dsem{i}") for i in range(n_dma_sems)]
        self.dcnt = [0] * n_dma_sems
        self.dnext = 0
        self.ninstr = 0

    def _need(self, e, waits, tok):
        if tok is None:
            return
        sem, val = tok
        if sem.num == self.sem[e].num and e == "pe":
            return
        if self.seen[e].get(sem.num, 0) >= val:
            return
        self.seen[e][sem.num] = val
        waits.append((sem, val))

    def _deps(self, e, r, w):
        waits = []
        for res in r:
            self._need(e, waits, res.w)
        for res in w:
            self._need(e, waits, res.w)
            for t in res.rs.values():
                self._need(e, waits, t)
        return waits

    def _commit(self, tok, r, w):
        for res in r:
            res.rs[tok[0].num] = tok
        for res in w:
            res.w = tok
            res.rs = {}

    def op(self, e, fn, r=(), w=()):
        waits = self._deps(e, r, w)
        self.cnt[e] += 1
        tok = (self.sem[e], self.cnt[e])
        self.q[e].append((waits, fn, self.sem[e], 1))
        self._commit(tok, r, w)
        self.ninstr += 1 + len(waits)
        return tok

    def dma(self, e, out, in_, r=(), w=(), **kw):
        waits = self._deps(e, r, w)
        i = self.dnext
        self.dnext = (self.dnext + 1) % len(self.dsem)
        if self.dcnt[i] > 0:
            self._need(e, waits, (self.dsem[i], self.dcnt[i]))
        self.dcnt[i] += 16
        tok = (self.dsem[i], self.dcnt[i])
        self.q[e].append((waits, lambda eng: eng.dma_start(out=out, in_=in_, **kw), self.dsem[i], 16))
        self._commit(tok, r, w)
        self.ninstr += 1 + len(waits)
        return tok

    def wait(self, e, toks):
        waits = []
        for t in toks:
            self._need(e, waits, t)
        if waits:
            self.q[e].append((waits, None, None, 0))

    def barrier(self):
        toks = [(self.sem[e], self.cnt[e]) for e in self.ENGS if self.cnt[e] > 0]
        toks += [(self.dsem[i], self.dcnt[i]) for i in range(len(self.dsem)) if self.dcnt[i] > 0]
        for e in self.ENGS:
            self.wait(e, toks)

    def emit(self):
        nc = self.nc
        q = self.q

        def run(eng, items):
            for waits, fn, sem, inc in items:
                for s, v in waits:
                    eng.wait_ge(s, v)
                if fn is not None:
                    fn(eng).then_inc(sem, inc)

        with nc.Block() as block:
            @block.tensor
            def _(t):
                run(t, q["pe"])

            @block.scalar
            def _(t):
                run(t, q["act"])

            @block.vector
            def _(t):
                run(t, q["dve"])

            @block.gpsimd
            def _(t):
                run(t, q["pool"])

            @block.sync
            def _(t):
                run(t, q["sp"])


class T:
    _n = [0]

    def __init__(self, es, nc, name, shape, dtype, psum=False):
        T._n[0] += 1
        name = f"{name}_u{T._n[0]}"
        cm = nc.psum_tensor(name, shape, dtype) if psum else nc.sbuf_tensor(name, shape, dtype)
        self.t = es.enter_context(cm)
        self.r = Res(name)

    def __getitem__(self, k):
        return self.t[k]


class Ctx:
    def __init__(self, nc, es):
        self.nc = nc
        self.P = Prog(nc)
        self.h = H(self.P)
        self.ps = [T(es, nc, f"ps{i}", [128, 512], F32, psum=True) for i in range(8)]
        self.out_toks = []


def _io(nc, env, pfx):
    def din(name, shape):
        if env is not None and name in env:
            return env[name]
        return nc.dram_tensor(pfx + name, shape, F32, kind="ExternalInput").ap()

    def dout(name, shape):
        if env is not None and name in env:
            return env[name]
        return nc.dram_tensor(pfx + name, shape, F32, kind="ExternalOutput").ap()
    return din, dout


class H:
    def __init__(self, P):
        self.P = P

    def mm(self, out, lhsT, rhs, start, stop, r, w):
        return self.P.op("pe", lambda e: e.matmul(out, lhsT=lhsT, rhs=rhs, start=start, stop=stop), r, w)

    def tr(self, out, in_, ident, r, w):
        return self.P.op("pe", lambda e: e.transpose(out, in_, ident), r, w)

    def act(self, out, in_, func, r, w, bias=None, scale=None, accum=None):
        kw = {}
        if bias is not None:
            kw["bias"] = bias
        if scale is not None:
            kw["scale"] = scale
        if accum is not None:
            kw["accum_out"] = accum
        return self.P.op("act", lambda e: e.activation(out=out, in_=in_, func=func, **kw), r, w)

    def tt(self, eng, out, in0, in1, op, r, w):
        return self.P.op(eng, lambda e: e.tensor_tensor(out=out, in0=in0, in1=in1, op=op), r, w)

    def ts(self, eng, out, in0, s1, s2, op0, op1, r, w, accum=None):
        if accum is not None:
            return self.P.op(eng, lambda e: e.tensor_scalar(out=out, in0=in0, scalar1=s1, scalar2=s2, op0=op0, op1=op1, accum_out=accum), r, w)
        if op1 is None:
            return self.P.op(eng, lambda e: e.tensor_scalar(out=out, in0=in0, scalar1=s1, scalar2=None, op0=op0), r, w)
        return self.P.op(eng, lambda e: e.tensor_scalar(out=out, in0=in0, scalar1=s1, scalar2=s2, op0=op0, op1=op1), r, w)

    def stt(self, out, in0, scalar, in1, op0, op1, r, w):
        return self.P.op("dve", lambda e: e.scalar_tensor_tensor(out=out, in0=in0, scalar=scalar, in1=in1, op0=op0, op1=op1), r, w)

    def cp(self, eng, out, in_, r, w):
        if eng == "act":
            return self.P.op("act", lambda e: e.copy(out=out, in_=in_), r, w)
        return self.P.op(eng, lambda e: e.tensor_copy(out=out, in_=in_), r, w)

    def memset(self, eng, ap, val, w):
        return self.P.op(eng, lambda e: e.memset(ap, val), (), w)

    def max8(self, out, in_, r, w):
        return self.P.op("dve", lambda e: e.max(out=out, in_=in_), r, w)

    def mrep(self, out, rep, vals, r, w):
        return self.P.op("dve", lambda e: e.match_replace(out=out, in_to_replace=rep, in_values=vals, imm_value=NEG), r, w)

    def recip(self, out, in_, r, w):
        return self.P.op("dve", lambda e: e.reciprocal(out=out, in_=in_), r, w)

    def red(self, out, in_, op, r, w):
        return self.P.op("dve", lambda e: e.tensor_reduce(out=out, in_=in_, axis=AX.X, op=op), r, w)


TB = 512
NTT = TB // 128
NJ = TB // 16


def ada_compute(es0, nc, P, h, ps, cfm_d, adaw_d, adabfm_d, adab_d, fm_secs, bc_secs, tag):
    from contextlib import ExitStack
    outs = {}
    for s in fm_secs:
        outs[s] = T(es0, nc, f"ada_fm{tag}_{s}", [128, 16], F32)
    for s in bc_secs:
        outs[s] = T(es0, nc, f"ada_bc{tag}_{s}", [128, 2048], F32)
    with ExitStack() as es:
        condT = T(es, nc, f"condT{tag}", [128, 16], F32)
        condB = T(es, nc, f"condB{tag}", [128, 16, 128], F32)
        abfm = T(es, nc, f"abfm{tag}", [128, 96], F32)
        wblk = [T(es, nc, f"adaw{tag}_{i}", [128, 16, 512], F32) for i in range(2)]
        abb = [T(es, nc, f"adabb{tag}_{i}", [128, 512], F32) for i in range(2)]
        P.dma("sp", condT[:, :], cfm_d, w=[condT.r])
        P.dma("sp", abfm[:, :], adabfm_d, w=[abfm.r])
        h.act(condT[:, :], condT[:, :], AF.Silu, [condT.r], [condT.r])
        h.cp("dve", condB[:, :, :], condT[:, :].unsqueeze(2).to_broadcast([128, 16, 128]), [condT.r], [condB.r])
        it = 0
        for s in sorted(set(fm_secs) | set(bc_secs)):
            for cb in range(4):
                wb = wblk[it % 2]
                ab = abb[it % 2]
                it += 1
                c0 = s * 2048 + cb * 512
                P.dma("sp", wb[:, :, :], adaw_d[:, c0:c0 + 512].rearrange("(k p) n -> p k n", p=128), w=[wb.r])
                if s in bc_secs:
                    P.dma("sp", ab[:, :], adab_d[0:1, c0:c0 + 512].partition_broadcast(128), w=[ab.r])
                    pb = ps[it % 2]
                    for k in range(16):
                        h.mm(pb[:, :], condB[:, k, :], wb[:, k, :], k == 0, k == 15, [condB.r, wb.r], [pb.r])
                    h.tt("dve", outs[s][:, cb * 512:(cb + 1) * 512], pb[:, :], ab[:, :], ALU.add, [pb.r, ab.r], [outs[s].r])
                if s in fm_secs:
                    pf = ps[2 + (it % 2)]
                    for jj in range(4):
                        for k in range(16):
                            h.mm(pf[:, jj:jj + 1], wb[:, k, jj * 128:(jj + 1) * 128], condT[:, k:k + 1], k == 0, k == 15,
                                 [condT.r, wb.r], [pf.r])
                    j0 = cb * 4
                    h.tt("dve", outs[s][:, j0:j0 + 4], pf[:, 0:4], abfm[:, s * 16 + j0:s * 16 + j0 + 4], ALU.add,
                         [pf.r, abfm.r], [outs[s].r])
    P.barrier()
    return outs


def rms_stats(P, h, x_ap, sq_scr, ssq_ap, rstd_ap, r, scr_res, st_res, n):
    h.act(sq_scr, x_ap, AF.Square, r, [scr_res, st_res], accum=ssq_ap)
    h.ts("dve", rstd_ap, ssq_ap, 1.0 / n, EPS, ALU.mult, ALU.add, [st_res], [st_res])
    h.act(rstd_ap, rstd_ap, AF.Sqrt, [st_res], [st_res])
    h.recip(rstd_ap, rstd_ap, [st_res], [st_res])


def to_fm(P, h, ps, ident, src, src_res, ncol, dst, tt, evac):
    for q in range(ncol // 4):
        pb = ps[q % 2]
        for j in range(4):
            kc = q * 4 + j
            h.tr(pb[:, j * 128:(j + 1) * 128], src[:, kc * 128:(kc + 1) * 128], ident[:, :], [src_res, ident.r], [pb.r])
        evac(q, pb)


def build_tail(NT, KMIX, final, ya_norm, n_i1=128, ctx=None, env=None, pfx="", blend=False):
    from contextlib import ExitStack
    nc = ctx.nc if ctx else bass.Bass("TRN2", target_bir_lowering=False)
    NB = NT // TB
    KC = KMIX // 128
    GV = 4
    din, dout = _io(nc, env, pfx)
    NSRC = 2 * NT if blend else NT
    sel_d = din("sel", [128, 2]) if blend else None
    x_d = din("x", [NSRC, D])
    mix_d = din("mix", [NSRC, KMIX])
    wo_d = din("wo", [KMIX, D])
    cfm_d = din("c_fm", [128, 16])
    adaw_d = din("ada_w", [D, 6 * D])
    adabfm_d = din("ada_b_fm", [128, 96])
    adab_d = din("ada_b", [1, 6 * D])
    gffn_d = din("g_ffn_fm", [128, 16])
    wq_d = din("wq", [D, D])
    keys_d = din("keys", [16, 128, 128])
    U_d = din("U", [16384, D])
    V_d = din("V", [16384, D])
    fing_d = din("final_g", [1, D])
    gya_d = din("g_ya_fm", [128, 16])
    ident_d = din("ident", [128, 128])
    summ_d = din("summ", [128, 16])
    out_d = dout("out", [NT, D])
    sc_d = nc.dram_tensor(pfx + "sc_scr", [TB, 2048], F32, kind="Internal").ap()
    bc_d = nc.dram_tensor(pfx + "bc_scr", [2, 128, 2048], F32, kind="Internal").ap()
    scd_res = Res("sc_d")
    bcd_res = Res("bc_d")
    UT_d = nc.dram_tensor(pfx + "UT_scr", [n_i1, 128, 2048], BF16, kind="Internal").ap()
    Vb_d = nc.dram_tensor(pfx + "Vb_scr", [n_i1 * 128, D], BF16, kind="Internal").ap()

    with ExitStack() as es:
        if ctx is None:
            P = Prog(nc)
            h = H(P)
            ps = [T(es, nc, f"ps{i}", [128, 512], F32, psum=True) for i in range(8)]
        else:
            P, h, ps = ctx.P, ctx.h, ctx.ps
        ident = T(es, nc, "ident", [128, 128], F32)
        summ = T(es, nc, "summ", [128, 16], F32)
        sel = T(es, nc, "sel", [128, 2], F32)
        if blend:
            P.dma("sp", sel[:, :], sel_d, w=[sel.r])
        gffn = T(es, nc, "gffn", [128, 16], F32)
        gya = T(es, nc, "gya", [128, 16], F32)
        gs2 = T(es, nc, "gs2", [128, 16], F32)
        keysT = T(es, nc, "keysT", [128, 16, 128], F32)
        P.dma("sp", ident[:, :], ident_d, w=[ident.r])
        P.dma("sp", summ[:, :], summ_d, w=[summ.r])
        P.dma("sp", gffn[:, :], gffn_d, w=[gffn.r])
        P.dma("sp", gya[:, :], gya_d, w=[gya.r])

        sh2 = T(es, nc, "sh2", [128, 16], F32)
        with ExitStack() as es1:
            ada = ada_compute(es1, nc, P, h, ps, cfm_d, adaw_d, adabfm_d, adab_d, fm_secs=[3, 4], bc_secs=[2, 5], tag="t")
            h.cp("dve", sh2[:, :], ada[3][:, :], [ada[3].r], [sh2.r])
            h.stt(gs2[:, :], ada[4][:, :], 1.0, gffn[:, :], ALU.add, ALU.mult, [ada[4].r, gffn.r], [gs2.r])
            P.dma("sp", bc_d[0], ada[2][:, :], r=[ada[2].r], w=[bcd_res])
            P.dma("sp", bc_d[1], ada[5][:, :], r=[ada[5].r], w=[bcd_res])
            kraw = T(es1, nc, "kraw", [128, 16, 128], F32)
            P.dma("sp", kraw[:, :, :], keys_d.rearrange("a k c -> k a c"), w=[kraw.r])
            for q in range(4):
                pb = ps[4 + q % 2]
                for j in range(4):
                    h.tr(pb[:, j * 128:(j + 1) * 128], kraw[:, q * 4 + j, :], ident[:, :], [kraw.r, ident.r], [pb.r])
                h.cp("dve", keysT[:, q * 4:(q + 1) * 4, :], pb[:, :].rearrange("p (a b) -> p a b", a=4), [pb.r], [keysT.r])
            P.barrier()

        with ExitStack() as esp:
            Uraw = [T(esp, nc, f"Uraw{i}", [128, D], F32) for i in range(2)]
            utb = [T(esp, nc, f"utb{i}", [128, 16, 128], BF16) for i in range(2)]
            VR = 1024
            for r0 in range(0, n_i1 * 128, VR):
                P.dma("pool", Vb_d[r0:r0 + VR, :], V_d[r0:r0 + VR, :])
            for i1 in range(n_i1):
                u = Uraw[i1 % 2]
                ut = utb[i1 % 2]
                P.dma("sp", u[:, :], U_d[i1 * 128:(i1 + 1) * 128, :], w=[u.r])
                for q in range(4):
                    pb = ps[(i1 * 4 + q) % 4]
                    for j in range(4):
                        h.tr(pb[:, j * 128:(j + 1) * 128], u[:, (q * 4 + j) * 128:(q * 4 + j + 1) * 128], ident[:, :],
                             [u.r, ident.r], [pb.r])
                    h.cp("act" if q % 2 else "dve", ut[:, q * 4:(q + 1) * 4, :], pb[:, :].rearrange("p (a b) -> p a b", a=4),
                         [pb.r], [ut.r])
                P.dma("sp", UT_d[i1].rearrange("p (k e) -> p k e", k=16), ut[:, :, :], r=[ut.r])
            P.barrier()

        for b in range(NB):
            t0 = b * TB
            ores = Res(f"out_blk{b}")
            oblk = out_d[t0:t0 + TB, :].rearrange("(t p) d -> p t d", p=128)
            with ExitStack() as esb:
                hT = T(esb, nc, f"hT", [128, 16, TB], BF16)
                with ExitStack() as esa:
                    xb = T(esa, nc, "xb", [128, NTT, D], F32)
                    P.dma("sp", xb[:, :, :], x_d[t0:t0 + TB, :].rearrange("(t p) d -> p t d", p=128), w=[xb.r])
                    if blend:
                        with ExitStack() as esx:
                            xb2 = T(esx, nc, "xb2", [128, NTT, D], F32)
                            P.dma("sp", xb2[:, :, :], x_d[NT + t0:NT + t0 + TB, :].rearrange("(t p) d -> p t d", p=128), w=[xb2.r])
                            for tt in range(NTT):
                                h.ts("dve", xb[:, tt, :], xb[:, tt, :], sel[:, 0:1], None, ALU.mult, None, [xb.r, sel.r], [xb.r])
                                h.stt(xb[:, tt, :], xb2[:, tt, :], sel[:, 1:2], xb[:, tt, :], ALU.mult, ALU.add, [xb2.r, sel.r, xb.r], [xb.r])
                            P.barrier()
                    g1 = T(esa, nc, "g1", [128, D], F32)
                    P.dma("sp", g1[:, :], bc_d[0], r=[bcd_res], w=[g1.r])
                    with ExitStack() as esa2:
                        mixT = T(esa2, nc, "mixT", [128, KC, TB], BF16)
                        mraw = [T(esa2, nc, f"mraw{i}", [128, KMIX], F32) for i in range(2)]
                        sta = T(esa2, nc, "sta", [128, 8], F32)
                        sqa = T(esa2, nc, "sqa", [128, 2048], F32)
                        tmp = T(esa2, nc, "tmpa", [128, 512], F32)
                        wob = [T(esa2, nc, f"wob{i}", [128, KC, 512], BF16) for i in range(2)]
                        mrB = T(esa2, nc, "mrB", [128, KMIX], F32) if blend else None
                        for tt in range(NTT):
                            mr = mraw[tt % 2]
                            P.dma("sp", mr[:, :], mix_d[t0 + tt * 128:t0 + (tt + 1) * 128, :], w=[mr.r])
                            if blend:
                                P.dma("sp", mrB[:, :], mix_d[NT + t0 + tt * 128:NT + t0 + (tt + 1) * 128, :], w=[mrB.r])
                                h.ts("dve", mr[:, :], mr[:, :], sel[:, 0:1], None, ALU.mult, None, [mr.r, sel.r], [mr.r])
                                h.stt(mr[:, :], mrB[:, :], sel[:, 1:2], mr[:, :], ALU.mult, ALU.add, [mrB.r, sel.r, mr.r], [mr.r])
                            if ya_norm:
                                rms_stats(P, h, mr[:, 0:2048], sqa[:, :], sta[:, 0:1], sta[:, 1:2], [mr.r], sqa.r, sta.r, 2048)
                                h.ts("dve", mr[:, 0:2048], mr[:, 0:2048], sta[:, 1:2], None, ALU.mult, None, [mr.r, sta.r], [mr.r])

                            def evac(q, pb, tt=tt):
                                dst = mixT[:, q * 4:(q + 1) * 4, tt * 128:(tt + 1) * 128]
                                src = pb[:, :].rearrange("p (a b) -> p a b", a=4)
                                if ya_norm and q < 4:
                                    h.tt("dve", dst, src, gya[:, q * 4:(q + 1) * 4].unsqueeze(2).to_broadcast([128, 4, 128]),
                                         ALU.mult, [pb.r, gya.r], [mixT.r])
                                elif q % 2 == 0:
                                    h.cp("dve", dst, src, [pb.r], [mixT.r])
                                else:
                                    h.cp("act", dst, src, [pb.r], [mixT.r])
                            to_fm(P, h, ps, ident, mr, mr.r, KC, mixT, tt, evac)
                        for db in range(4):
                            wb = wob[db % 2]
                            P.dma("pool", wb[:, :, :], wo_d[:, db * 512:(db + 1) * 512].rearrange("(k p) n -> p k n", p=128), w=[wb.r])
                            for tt in range(NTT):
                                pb = ps[4 + (tt % 2)]
                                for kc in range(KC):
                                    h.mm(pb[:, :], mixT[:, kc, tt * 128:(tt + 1) * 128], wb[:, kc, :], kc == 0, kc == KC - 1,
                                         [mixT.r, wb.r], [pb.r])
                                h.tt("dve", tmp[:, :], pb[:, :], g1[:, db * 512:(db + 1) * 512], ALU.mult, [pb.r, g1.r], [tmp.r])
                                h.tt("dve", xb[:, tt, db * 512:(db + 1) * 512], xb[:, tt, db * 512:(db + 1) * 512], tmp[:, :], ALU.add,
                                     [tmp.r, xb.r], [xb.r])
                    P.barrier()
                    P.dma("sp", oblk, xb[:, :, :], r=[xb.r], w=[ores])
                    with ExitStack() as esn:
                        sq = T(esn, nc, "sq", [128, D], F32)
                        st = T(esn, nc, "st", [128, 8], F32)
                        for tt in range(NTT):
                            rms_stats(P, h, xb[:, tt, :], sq[:, :], st[:, 0:1], st[:, 1:2], [xb.r], sq.r, st.r, D)
                            h.ts("dve", sq[:, :], xb[:, tt, :], st[:, 1:2], None, ALU.mult, None, [xb.r, st.r], [sq.r])

                            def evac(q, pb, tt=tt):
                                for j in range(4):
                                    kc = q * 4 + j
                                    h.act(hT[:, kc, tt * 128:(tt + 1) * 128], pb[:, j * 128:(j + 1) * 128], AF.Identity,
                                          [pb.r, gs2.r, sh2.r], [hT.r], bias=sh2[:, kc:kc + 1], scale=gs2[:, kc:kc + 1])
                            to_fm(P, h, ps, ident, sq, sq.r, 16, hT, tt, evac)
                    P.barrier()
                s2 = T(esb, nc, "s2", [128, NJ, 128], F32)
                theta = T(esb, nc, "theta", [128, NJ, 128], F32)
                e1 = T(esb, nc, "e1", [128, NJ, 128], BF16)
                e2 = T(esb, nc, "e2", [128, NJ, 128], BF16)
                with ExitStack() as esc:
                    qT = T(esc, nc, "qT", [128, 16, TB], F32)
                    wqb = [T(esc, nc, f"wqb{i}", [128, 16, 128], BF16) for i in range(2)]
                    sc = [T(esc, nc, f"sc{i}", [128, 2048], F32) for i in range(2)]
                    for oc in range(16):
                        wb = wqb[oc % 2]
                        P.dma("pool", wb[:, :, :], wq_d[:, oc * 128:(oc + 1) * 128].rearrange("(k p) n -> p k n", p=128), w=[wb.r])
                        pb = ps[oc % 2]
                        for k in range(16):
                            h.mm(pb[:, :], wb[:, k, :], hT[:, k, :], k == 0, k == 15, [wb.r, hT.r], [pb.r])
                        h.cp("act" if oc % 2 else "dve", qT[:, oc, :], pb[:, :], [pb.r], [qT.r])
                    for tt in range(NTT):
                        s_ = sc[tt % 2]
                        for q4 in range(4):
                            pb = ps[2 + q4 % 2]
                            for j in range(4):
                                hi = q4 * 4 + j
                                h.mm(pb[:, j * 128:(j + 1) * 128], qT[:, hi, tt * 128:(tt + 1) * 128], keysT[:, hi, :], True, True,
                                     [qT.r, keysT.r], [pb.r])
                            h.cp("act" if q4 % 2 else "dve", s_[:, q4 * 512:(q4 + 1) * 512], pb[:, :], [pb.r], [s_.r])
                        P.dma("sp", sc_d[tt * 128:(tt + 1) * 128, :], s_[:, :], r=[s_.r], w=[scd_res])
                P.barrier()
                with ExitStack() as esk:
                    S = T(esk, nc, "S", [128, NJ, 256], F32)
                    P.dma("sp", S[:, :, :], sc_d.rearrange("t (h x) -> (t h) x", h=8).rearrange("(j p) x -> p j x", p=128),
                          r=[scd_res], w=[S.r])
                    A16 = T(esk, nc, "A16", [128, NJ, 16], F32)
                    B16 = T(esk, nc, "B16", [128, NJ, 16], F32)
                    C24 = T(esk, nc, "C24", [128, NJ, 24], F32)
                    wk = T(esk, nc, "wk", [128, 128], F32)
                    cand = T(esk, nc, "cand", [128, 256], F32)
                    cw = T(esk, nc, "cw", [128, 256], F32)
                    cw2 = T(esk, nc, "cw2", [128, 256], F32)
                    sm_ = T(esk, nc, "smalls", [128, 4, NJ], F32)
                    ce = T(esk, nc, "ce", [128, NJ, 16], F32)
                    tmpf = T(esk, nc, "tmpf", [128, NJ, 128], F32)
                    for j in range(NJ):
                        for half, dst in ((0, A16), (1, B16)):
                            src = S[:, j, half * 128:(half + 1) * 128]
                            h.max8(dst[:, j, 0:8], src, [S.r], [dst.r])
                            h.mrep(wk[:, :], dst[:, j, 0:8], src, [S.r, dst.r], [wk.r])
                            h.max8(dst[:, j, 8:16], wk[:, :], [wk.r], [dst.r])
                        h.tt("dve", cand[:, :].rearrange("p (a b) -> p a b", a=16),
                             A16[:, j, :].unsqueeze(2).to_broadcast([128, 16, 16]),
                             B16[:, j, :].unsqueeze(1).to_broadcast([128, 16, 16]), ALU.add, [A16.r, B16.r], [cand.r])
                        h.max8(C24[:, j, 0:8], cand[:, :], [cand.r], [C24.r])
                        h.mrep(cw[:, :], C24[:, j, 0:8], cand[:, :], [cand.r, C24.r], [cw.r])
                        h.max8(C24[:, j, 8:16], cw[:, :], [cw.r], [C24.r])
                        h.mrep(cw2[:, :], C24[:, j, 8:16], cw[:, :], [cw.r, C24.r], [cw2.r])
                        h.max8(C24[:, j, 16:24], cw2[:, :], [cw2.r], [C24.r])
                    mt = sm_[:, 0, :]
                    thr = sm_[:, 1, :]
                    zz = sm_[:, 2, :]
                    h.tt("dve", mt, A16[:, :, 0], B16[:, :, 0], ALU.add, [A16.r, B16.r], [sm_.r])
                    h.tt("dve", thr, C24[:, :, 15], C24[:, :, 16], ALU.add, [C24.r], [sm_.r])
                    h.ts("dve", thr, thr, 0.5, None, ALU.mult, None, [sm_.r], [sm_.r])
                    h.tt("dve", ce[:, :, :], C24[:, :, 0:16], mt.unsqueeze(2).to_broadcast([128, NJ, 16]), ALU.subtract,
                         [C24.r, sm_.r], [ce.r])
                    h.act(ce[:, :, :], ce[:, :, :], AF.Exp, [ce.r], [ce.r])
                    h.red(zz, ce[:, :, :], ALU.add, [ce.r], [sm_.r])
                    h.recip(zz, zz, [sm_.r], [sm_.r])
                    h.tt("dve", theta[:, :, :], thr.unsqueeze(2).to_broadcast([128, NJ, 128]), S[:, :, 0:128], ALU.subtract,
                         [sm_.r, S.r], [theta.r])
                    h.tt("dve", tmpf[:, :, :], S[:, :, 0:128], A16[:, :, 0:1].to_broadcast([128, NJ, 128]), ALU.subtract,
                         [S.r, A16.r], [tmpf.r])
                    h.act(e1[:, :, :], tmpf[:, :, :], AF.Exp, [tmpf.r], [e1.r])
                    h.tt("dve", tmpf[:, :, :], S[:, :, 128:256], B16[:, :, 0:1].to_broadcast([128, NJ, 128]), ALU.subtract,
                         [S.r, B16.r], [tmpf.r])
                    h.act(tmpf[:, :, :], tmpf[:, :, :], AF.Exp, [tmpf.r], [tmpf.r])
                    h.tt("dve", e2[:, :, :], tmpf[:, :, :], zz.unsqueeze(2).to_broadcast([128, NJ, 128]), ALU.mult,
                         [tmpf.r, sm_.r], [e2.r])
                    h.cp("dve", s2[:, :, :], S[:, :, 128:256], [S.r], [s2.r])
                P.barrier()
                acc = T(esb, nc, "acc", [128, NTT, D], F32)
                h.memset("pool", acc[:, :, :], 0.0, [acc.r])
                with ExitStack() as esl:
                    UT = [T(esl, nc, f"UT{i}", [128, 16, 128], BF16) for i in range(3)]
                    Vg = [T(esl, nc, f"Vg{i}", [128, GV, D], BF16) for i in range(2)]
                    GA = [T(esl, nc, f"GA{i}", [128, TB], BF16) for i in range(2)]
                    AG = [T(esl, nc, f"AG{i}", [128, GV, TB], BF16) for i in range(2)]
                    mks = [T(esl, nc, f"mk{i}", [128, NJ, 128], BF16) for i in range(2)]
                    mkr = [[Res(f"mkr{i}_{q}") for q in range(4)] for i in range(2)]
                    Gh = [T(esl, nc, f"Gh{i}", [128, NJ, 128], BF16) for i in range(2)]
                    sums = [T(esl, nc, f"sums{i}", [128, NJ, 16], BF16) for i in range(2)]
                    def load_v(g):
                        vg = Vg[g % 2]
                        P.dma("sp", vg[:, :, :], Vb_d[g * GV * 128:(g + 1) * GV * 128, :].rearrange("(c p) d -> p c d", p=128), w=[vg.r])

                    NPC = 4
                    JP = NJ // NPC

                    def pre1(i1):
                        ut = UT[i1 % 3]
                        P.dma("sp", ut[:, :, :], UT_d[i1].rearrange("p (k e) -> p k e", k=16), w=[ut.r])
                        pa = ps[2 + i1 % 2]
                        for k in range(16):
                            h.mm(pa[:, :], ut[:, k, :], hT[:, k, :], k == 0, k == 15, [ut.r, hT.r], [pa.r])
                        ga = GA[i1 % 2]
                        h.act(ga[:, :], pa[:, :], AF.Gelu, [pa.r], [ga.r])

                    def piece1(i1, q):
                        gh = Gh[i1 % 2]
                        mk = mks[i1 % 2]
                        js = slice(q * JP, (q + 1) * JP)
                        h.tt("dve", mk[:, js, :], s2[:, js, :], theta[:, js, i1:i1 + 1].to_broadcast([128, JP, 128]), ALU.is_ge,
                             [s2.r, theta.r], [mkr[i1 % 2][q]])
                        h.tt("pool", gh[:, js, :], mk[:, js, :], e2[:, js, :], ALU.mult, [mkr[i1 % 2][q], e2.r], [gh.r])

                    def post1(i1):
                        sm = sums[i1 % 2]
                        h.tt("pool", sm[:, :, :], summ[:, :].unsqueeze(1).to_broadcast([128, NJ, 16]),
                             e1[:, :, i1:i1 + 1].to_broadcast([128, NJ, 16]), ALU.mult, [summ.r, e1.r], [sm.r])

                    def stage2(i1):
                        g, c = divmod(i1, GV)
                        ag = AG[g % 2]
                        ga = GA[i1 % 2]
                        gh = Gh[i1 % 2]
                        sm = sums[i1 % 2]
                        pg = ps[4 + i1 % 2]
                        for j in range(NJ):
                            h.mm(pg[:, j * 16:(j + 1) * 16], gh[:, j, :], sm[:, j, :], True, True, [gh.r, sm.r], [pg.r])
                        h.tt("dve", ag[:, c, :], ga[:, :], pg[:, :], ALU.mult, [ga.r, pg.r], [ag.r])

                    def unit3(g, u):
                        vg = Vg[g % 2]
                        ag = AG[g % 2]
                        tt, db = divmod(u, 4)
                        po = [ps[6], ps[7], ps[0], ps[1]][u % 4]
                        for c in range(GV):
                            h.mm(po[:, :], ag[:, c, tt * 128:(tt + 1) * 128], vg[:, c, db * 512:(db + 1) * 512], c == 0, c == GV - 1,
                                 [ag.r, vg.r], [po.r])
                        h.tt("dve", acc[:, tt, db * 512:(db + 1) * 512], acc[:, tt, db * 512:(db + 1) * 512], po[:, :], ALU.add,
                             [po.r, acc.r], [acc.r])

                    NU = NTT * 4
                    UPC = NU // GV
                    load_v(0)
                    pre1(0)
                    for q in range(NPC):
                        piece1(0, q)
                    post1(0)
                    for i1 in range(n_i1):
                        g, c = divmod(i1, GV)
                        nxt = i1 + 1 < n_i1
                        if nxt:
                            pre1(i1 + 1)
                        for q in range(NPC):
                            if nxt:
                                piece1(i1 + 1, q)
                            if g > 0:
                                for u in range(c * UPC + q * UPC // NPC, c * UPC + (q + 1) * UPC // NPC):
                                    unit3(g - 1, u)
                        if nxt:
                            post1(i1 + 1)
                        stage2(i1)
                        if c == GV - 1 and (g + 1) * GV < n_i1:
                            load_v(g + 1)
                    for u in range(NU):
                        unit3(n_i1 // GV - 1, u)
                P.barrier()
                with ExitStack() as esf:
                    xb = T(esf, nc, "xbf", [128, NTT, D], F32)
                    g2 = T(esf, nc, "g2", [128, D], F32)
                    fg = T(esf, nc, "fg", [128, D], F32)
                    sq = T(esf, nc, "sqf", [128, D], F32)
                    st = T(esf, nc, "stf", [128, 8], F32)
                    P.dma("sp", xb[:, :, :], oblk, r=[ores], w=[xb.r])
                    P.dma("sp", g2[:, :], bc_d[1], r=[bcd_res], w=[g2.r])
                    if final:
                        P.dma("sp", fg[:, :], fing_d[0:1, :].partition_broadcast(128), w=[fg.r])
                    for tt in range(NTT):
                        h.tt("dve", acc[:, tt, :], acc[:, tt, :], g2[:, :], ALU.mult, [acc.r, g2.r], [acc.r])
                        h.tt("pool", xb[:, tt, :], xb[:, tt, :], acc[:, tt, :], ALU.add, [acc.r, xb.r], [xb.r])
                        if final:
                            rms_stats(P, h, xb[:, tt, :], sq[:, :], st[:, 0:1], st[:, 1:2], [xb.r], sq.r, st.r, D)
                            h.ts("dve", xb[:, tt, :], xb[:, tt, :], st[:, 1:2], None, ALU.mult, None, [xb.r, st.r], [xb.r])
                            h.tt("pool", xb[:, tt, :], xb[:, tt, :], fg[:, :], ALU.mult, [xb.r, fg.r], [xb.r])
                    tk = P.dma("sp", oblk, xb[:, :, :], r=[xb.r], w=[ores])
                    P.wait("sp", [tk])
                P.barrier()
        if ctx is None:
            P.emit()
        print("tail instrs", P.ninstr, {e: len(P.q[e]) for e in P.ENGS})
    return nc


def build_attn(S_LEN=4096, NH=8, HG=4, ctx=None, env=None, pfx="", tokmajor=False):
    from contextlib import ExitStack
    nc = ctx.nc if ctx else bass.Bass("TRN2", target_bir_lowering=False)
    NBK = S_LEN // TB
    NKT = S_LEN // 128
    din, dout = _io(nc, env, pfx)

    x_d = din("x", [S_LEN, D])
    cfm_d = din("c_fm", [128, 16])
    adaw_d = din("ada_w", [D, 6 * D])
    adabfm_d = din("ada_b_fm", [128, 96])
    adab_d = din("ada_b", [1, 6 * D])
    gmix_d = din("g_mix_fm", [128, 16])
    wqkv_d = din("wqkv", [D, 3 * NH * 128])
    ident_d = din("ident", [128, 128])
    masks_d = din("masks", [128, 4, 512])
    ntri_d = din("ntri", [128, 128])
    if tokmajor:
        o_d = dout("o", [S_LEN, NH * 128])
    else:
        oT_d = dout("oT", [NH * 128, S_LEN])
    scale = 128.0 ** -0.5

    with ExitStack() as es:
        if ctx is None:
            P = Prog(nc)
            h = H(P)
            ps = [T(es, nc, f"ps{i}", [128, 512], F32, psum=True) for i in range(8)]
        else:
            P, h, ps = ctx.P, ctx.h, ctx.ps
        ident = T(es, nc, "ident", [128, 128], F32)
        masks = T(es, nc, "masks", [128, 4, 512], F32)
        ntri = T(es, nc, "ntri", [128, 128], F32)
        nones = T(es, nc, "nones", [128, 128], F32)
        gmix = T(es, nc, "gmix", [128, 16], F32)
        gs1 = T(es, nc, "gs1", [128, 16], F32)
        sh1 = T(es, nc, "sh1", [128, 16], F32)
        P.dma("sp", ident[:, :], ident_d, w=[ident.r])
        P.dma("sp", masks[:, :, :], masks_d, w=[masks.r])
        P.dma("sp", ntri[:, :], ntri_d, w=[ntri.r])
        P.dma("sp", gmix[:, :], gmix_d, w=[gmix.r])
        h.memset("pool", nones[:, :], -1.0, [nones.r])
        ntrib = T(es, nc, "ntrib", [128, 128], BF16)
        nonesb = T(es, nc, "nonesb", [128, 128], BF16)
        h.cp("dve", ntrib[:, :], ntri[:, :], [ntri.r], [ntrib.r])
        h.memset("pool", nonesb[:, :], -1.0, [nonesb.r])
        with ExitStack() as es1:
            ada = ada_compute(es1, nc, P, h, ps, cfm_d, adaw_d, adabfm_d, adab_d, fm_secs=[0, 1], bc_secs=[], tag="a")
            h.cp("dve", sh1[:, :], ada[0][:, :], [ada[0].r], [sh1.r])
            h.stt(gs1[:, :], ada[1][:, :], 1.0, gmix[:, :], ALU.add, ALU.mult, [ada[1].r, gmix.r], [gs1.r])
            P.barrier()
        out_toks = []
        for hg in range(NH // HG):
            with ExitStack() as esg:
                QT = T(esg, nc, "QT", [128, HG, S_LEN], BF16)
                KT = T(esg, nc, "KT", [128, HG, S_LEN], BF16)
                Vt = T(esg, nc, "Vt", [128, NKT, HG * 128], BF16)
                with ExitStack() as e1:
                    xb = T(e1, nc, "xb", [128, NTT, D], F32)
                    hT = T(e1, nc, "hT", [128, 16, TB], BF16)
                    sq = T(e1, nc, "sq", [128, D], F32)
                    st = T(e1, nc, "st", [128, 8], F32)
                    wp = [T(e1, nc, f"wp{i}", [128, 16, 128], BF16) for i in range(2)]
                    wv = T(e1, nc, "wv", [128, 16, HG * 128], BF16)
                    c0v = 2 * NH * 128 + hg * HG * 128
                    P.dma("pool", wv[:, :, :], wqkv_d[:, c0v:c0v + HG * 128].rearrange("(k p) n -> p k n", p=128), w=[wv.r])
                    wi = 0
                    for b in range(NBK):
                        t0 = b * TB
                        P.dma("sp", xb[:, :, :], x_d[t0:t0 + TB, :].rearrange("(t p) d -> p t d", p=128), w=[xb.r])
                        for tt in range(NTT):
                            rms_stats(P, h, xb[:, tt, :], sq[:, :], st[:, 0:1], st[:, 1:2], [xb.r], sq.r, st.r, D)
                            h.ts("dve", sq[:, :], xb[:, tt, :], st[:, 1:2], None, ALU.mult, None, [xb.r, st.r], [sq.r])

                            def evac(q, pb, tt=tt):
                                for j in range(4):
                                    kc = q * 4 + j
                                    h.act(hT[:, kc, tt * 128:(tt + 1) * 128], pb[:, j * 128:(j + 1) * 128], AF.Identity,
                                          [pb.r, gs1.r, sh1.r], [hT.r], bias=sh1[:, kc:kc + 1], scale=gs1[:, kc:kc + 1])
                            to_fm(P, h, ps, ident, sq, sq.r, 16, hT, tt, evac)
                        for hl in range(HG):
                            for which, dst in ((0, QT), (1, KT)):
                                wb = wp[wi % 2]
                                wi += 1
                                c0 = which * NH * 128 + (hg * HG + hl) * 128
                                P.dma("pool", wb[:, :, :], wqkv_d[:, c0:c0 + 128].rearrange("(k p) n -> p k n", p=128), w=[wb.r])
                                pb = ps[2 + wi % 2]
                                for k in range(16):
                                    h.mm(pb[:, :], wb[:, k, :], hT[:, k, :], k == 0, k == 15, [wb.r, hT.r], [pb.r])
                                if which == 0:
                                    h.act(dst[:, hl, t0:t0 + TB], pb[:, :], AF.Copy, [pb.r], [dst.r], scale=scale)
                                else:
                                    h.cp("dve", dst[:, hl, t0:t0 + TB], pb[:, :], [pb.r], [dst.r])
                        for tt in range(NTT):
                            pb = ps[4 + tt % 2]
                            for k in range(16):
                                h.mm(pb[:, 0:HG * 128], hT[:, k, tt * 128:(tt + 1) * 128], wv[:, k, :], k == 0, k == 15, [hT.r, wv.r], [pb.r])
                            h.cp("dve" if tt % 2 else "act", Vt[:, b * NTT + tt, :], pb[:, 0:HG * 128], [pb.r], [Vt.r])
                P.barrier()
                with ExitStack() as e2:
                    ex = [T(e2, nc, f"ex{i}", [128, 512], F32) for i in range(2)]
                    spb = [T(e2, nc, f"sp{i}", [128, 512], F32) for i in range(3)]
                    shi = [T(e2, nc, f"shi{i}", [128, 512], BF16) for i in range(4)]
                    slo = [T(e2, nc, f"slo{i}", [128, 512], BF16) for i in range(4)]
                    Rsb = [T(e2, nc, f"Rsb{i}", [128, 512], F32) for i in range(2)]
                    tmpx = [T(e2, nc, f"tmpx{i}", [128, 512], F32) for i in range(2)]
                    Wt = [T(e2, nc, f"Wt{i}", [128, 512], BF16) for i in range(3)]
                    osb = [T(e2, nc, f"osb{i}", [128, 512], F32) for i in range(2)]
                    osT = [T(e2, nc, f"osT{i}", [128, 512], F32) for i in range(2)]
                    tiles = [(hl, qb, idx) for hl in range(HG) for qb in range(NBK) for idx in range(4 * qb + 4)]

                    def geom(tile):
                        hl, qb, idx = tile
                        nk = 4 * qb + 4
                        kt = nk - 1 - idx
                        return hl, qb, idx, nk, kt, kt - 4 * qb

                    def S0(tile, it):
                        hl, qb, idx, nk, kt, jd = geom(tile)
                        pL = ps[it % 2]
                        h.mm(pL[:, :], KT[:, hl, kt * 128:(kt + 1) * 128], QT[:, hl, qb * 512:(qb + 1) * 512], True, True, [KT.r, QT.r], [pL.r])

                    def S1(tile, it):
                        pL = ps[it % 2]
                        e_ = ex[it % 2]
                        s_ = spb[it % 3]
                        h.act(e_[:, :], pL[:, :], AF.Exp, [pL.r], [e_.r])
                        h.act(s_[:, :], e_[:, :], AF.Ln, [e_.r], [s_.r], bias=1.0)

                    def S2(tile, it):
                        hl, qb, idx, nk, kt, jd = geom(tile)
                        s_ = spb[it % 3]
                        if jd >= 0:
                            h.tt("dve", s_[:, :], s_[:, :], masks[:, jd, :], ALU.mult, [s_.r, masks.r], [s_.r])
                        h.cp("dve", shi[it % 4][:, :], s_[:, :], [s_.r], [shi[it % 4].r])
                        h.tt("pool", slo[it % 4][:, :], s_[:, :], shi[it % 4][:, :], ALU.subtract, [s_.r, shi[it % 4].r], [slo[it % 4].r])

                    def S3(tile, it):
                        hl, qb, idx, nk, kt, jd = geom(tile)
                        pE = ps[2 + it % 2]
                        pR = ps[4 + qb % 2]
                        hi_, lo_ = shi[it % 4], slo[it % 4]
                        h.mm(pE[:, :], KT[:, hl, kt * 128:(kt + 1) * 128], QT[:, hl, qb * 512:(qb + 1) * 512], True, False, [KT.r, QT.r], [pE.r])
                        h.mm(pE[:, :], ntrib[:, :], hi_[:, :], False, False, [ntrib.r, hi_.r], [pE.r])
                        h.mm(pE[:, :], ntrib[:, :], lo_[:, :], False, True, [ntrib.r, lo_.r], [pE.r])
                        if idx > 0:
                            h.cp("act", Rsb[it % 2][:, :], pR[:, :], [pR.r], [Rsb[it % 2].r])

                    def S4r(tile, it):
                        hl, qb, idx, nk, kt, jd = geom(tile)
                        pR = ps[4 + qb % 2]
                        hi_, lo_ = shi[it % 4], slo[it % 4]
                        if idx < nk - 1:
                            h.mm(pR[:, :], nonesb[:, :], hi_[:, :], idx == 0, False, [nonesb.r, hi_.r], [pR.r])
                            h.mm(pR[:, :], nonesb[:, :], lo_[:, :], False, idx == nk - 2, [nonesb.r, lo_.r], [pR.r])

                    def S4(tile, it):
                        hl, qb, idx, nk, kt, jd = geom(tile)
                        pE = ps[2 + it % 2]
                        if idx > 0:
                            h.tt("dve", tmpx[it % 2][:, :], pE[:, :], Rsb[it % 2][:, :], ALU.add, [pE.r, Rsb[it % 2].r], [tmpx[it % 2].r])
                        else:
                            h.cp("dve", tmpx[it % 2][:, :], pE[:, :], [pE.r], [tmpx[it % 2].r])

                    def S5(tile, it):
                        w_ = Wt[it % 3]
                        h.act(w_[:, :], tmpx[it % 2][:, :], AF.Exp, [tmpx[it % 2].r], [w_.r])

                    def S6(tile, it):
                        hl, qb, idx, nk, kt, jd = geom(tile)
                        po = ps[6 + qb % 2]
                        w_ = Wt[it % 3]
                        if jd >= 0:
                            h.tt("dve", w_[:, :], w_[:, :], masks[:, jd, :], ALU.mult, [w_.r, masks.r], [w_.r])
                        h.mm(po[:, :], Vt[:, kt, hl * 128:(hl + 1) * 128], w_[:, :], idx == 0, idx == nk - 1, [Vt.r, w_.r], [po.r])
                        if idx == nk - 1:
                            ob = osb[qb % 2]
                            h.cp("dve", ob[:, :], po[:, :], [po.r], [ob.r])
                            hh = hg * HG + hl
                            if tokmajor:
                                pt = po
                                for tt in range(4):
                                    h.tr(pt[:, tt * 128:(tt + 1) * 128], ob[:, tt * 128:(tt + 1) * 128], ident[:, :], [ob.r, ident.r], [pt.r])
                                ot = osT[qb % 2]
                                h.cp("act", ot[:, :], pt[:, :], [pt.r], [ot.r])
                                out_toks.append(P.dma("sp", o_d[qb * 512:(qb + 1) * 512, hh * 128:(hh + 1) * 128].rearrange("(t p) d -> p t d", p=128),
                                                      ot[:, :].rearrange("p (t d) -> p t d", t=4), r=[ot.r]))
                            else:
                                out_toks.append(P.dma("sp", oT_d[hh * 128:(hh + 1) * 128, qb * 512:(qb + 1) * 512], ob[:, :], r=[ob.r]))

                    sched = [(S0, 0), (S1, 1), (S2, 2), (S4r, 4), (S3, 3), (S4, 4), (S5, 5), (S6, 6)]
                    n_t = len(tiles)
                    for n in range(n_t + 6):
                        for fn, off in sched:
                            t = n - off
                            if 0 <= t < n_t:
                                fn(tiles[t], t)
                P.barrier()
        P.wait("sp", out_toks[-48:])
        if ctx is None:
            P.emit()
        print("attn instrs", P.ninstr, {e: len(P.q[e]) for e in P.ENGS})
    return nc


def build_mix0(S_LEN=4096, NHD=16, gm_nblk=4, ctx=None, env=None, pfx="", ygcol=0, ygw=None):
    from contextlib import ExitStack
    nc = ctx.nc if ctx else bass.Bass("TRN2", target_bir_lowering=False)
    din, dout = _io(nc, env, pfx)
    NBK = S_LEN // TB
    NG = NHD // 4
    NX = NHD * 64
    NXC = NX // 128
    NCH = NXC + 2 * NG
    WSSD = 2 * NX + 2 * NG * 128 + NHD

    x_d = din("x", [S_LEN, D])
    xgm_d = din("x_gm", [gm_nblk * TB, D]) if gm_nblk else None
    cfm_d = din("c_fm", [128, 16])
    adaw_d = din("ada_w", [D, 6 * D])
    adabfm_d = din("ada_b_fm", [128, 96])
    adab_d = din("ada_b", [1, 6 * D])
    gmix_d = din("g_mix_fm", [128, 16])
    wssd_d = din("w_ssd", [D, WSSD])
    convw_d = din("conv_w_fm", [128, NCH, 4])
    convb_d = din("conv_b_fm", [128, NCH])
    dtb_d = din("dt_bias", [1, NHD])
    alog_d = din("a_log", [1, NHD])
    dsk_d = din("d_skip", [1, NHD])
    wuv_d = din("w_uv", [D, 4096])
    lng_d = din("ln_g", [1, 2048])
    lnb_d = din("ln_b", [1, 2048])
    wsT_d = din("wsT", [128, 16, 128])
    bsT_d = din("bsT", [128, 16])
    ident_d = din("ident", [128, 128])
    ut_d = din("ut", [128, 128])
    slt_d = din("slt", [128, 128])
    yg_d = dout("yg", [S_LEN, NX])
    yb_d = dout("yb", [gm_nblk * TB, 2048]) if gm_nblk else None

    with ExitStack() as es:
        if ctx is None:
            P = Prog(nc)
            h = H(P)
            ps = [T(es, nc, f"ps{i}", [128, 512], F32, psum=True) for i in range(8)]
        else:
            P, h, ps = ctx.P, ctx.h, ctx.ps

        def cst(name, shape, src, dt=F32):
            t = T(es, nc, name, shape, dt)
            P.dma("sp", t[tuple(slice(None) for _ in shape)], src, w=[t.r])
            return t
        ident = cst("ident", [128, 128], ident_d)
        ut = cst("ut", [128, 128], ut_d)
        slt = cst("slt", [128, 128], slt_d)
        gmix = cst("gmix", [128, 16], gmix_d)
        convw = cst("convw", [128, NCH, 4], convw_d)
        convb = cst("convb", [128, NCH], convb_d)
        dtb = cst("dtb", [128, NHD], dtb_d[0:1, :].partition_broadcast(128))
        aneg = cst("aneg", [128, NHD], alog_d[0:1, :].partition_broadcast(128))
        dsk = cst("dsk", [128, NHD], dsk_d[0:1, :].partition_broadcast(128))
        bsT = cst("bsT", [128, 16], bsT_d)
        wsT = T(es, nc, "wsT", [128, 16, 128], BF16)
        ones = T(es, nc, "ones", [128, 128], F32)
        gs1 = T(es, nc, "gs1", [128, 16], F32)
        sh1 = T(es, nc, "sh1", [128, 16], F32)
        halo = T(es, nc, "halo", [128, NCH, 3], F32)
        Hs = T(es, nc, "Hs", [128, NX], F32)
        Hb = T(es, nc, "Hb", [128, NX], BF16)
        h.memset("pool", ones[:, :], 1.0, [ones.r])
        h.memset("pool", halo[:, :, :], 0.0, [halo.r])
        h.memset("pool", Hs[:, :], 0.0, [Hs.r])
        h.memset("pool", Hb[:, :], 0.0, [Hb.r])
        h.act(aneg[:, :], aneg[:, :], AF.Exp, [aneg.r], [aneg.r])
        h.ts("dve", aneg[:, :], aneg[:, :], -1.0, None, ALU.mult, None, [aneg.r], [aneg.r])
        with ExitStack() as es1:
            wraw = T(es1, nc, "wsraw", [128, 16, 128], F32)
            P.dma("sp", wraw[:, :, :], wsT_d, w=[wraw.r])
            h.tt("dve", wsT[:, :, :], wraw[:, :, :], ut[:, :].unsqueeze(1).to_broadcast([128, 16, 128]), ALU.mult,
                 [wraw.r, ut.r], [wsT.r])
            ada = ada_compute(es1, nc, P, h, ps, cfm_d, adaw_d, adabfm_d, adab_d, fm_secs=[0, 1], bc_secs=[], tag="m")
            h.cp("dve", sh1[:, :], ada[0][:, :], [ada[0].r], [sh1.r])
            h.stt(gs1[:, :], ada[1][:, :], 1.0, gmix[:, :], ALU.add, ALU.mult, [ada[1].r, gmix.r], [gs1.r])
            P.barrier()
        out_toks = []
        for kind, b in [("ssd", i) for i in range(NBK)] + [("gm", i) for i in range(gm_nblk)]:
            t0 = b * TB
            xsrc = x_d if kind == "ssd" else xgm_d
            with ExitStack() as esb:
                hT = T(esb, nc, "hT", [128, 16, TB], BF16)
                with ExitStack() as e1:
                    xb = T(e1, nc, "xb", [128, NTT, D], F32)
                    sq = T(e1, nc, "sq", [128, D], F32)
                    st = T(e1, nc, "st", [128, 8], F32)
                    P.dma("sp", xb[:, :, :], xsrc[t0:t0 + TB, :].rearrange("(t p) d -> p t d", p=128), w=[xb.r])
                    for tt in range(NTT):
                        rms_stats(P, h, xb[:, tt, :], sq[:, :], st[:, 0:1], st[:, 1:2], [xb.r], sq.r, st.r, D)
                        h.ts("dve", sq[:, :], xb[:, tt, :], st[:, 1:2], None, ALU.mult, None, [xb.r, st.r], [sq.r])

                        def evac(q, pb, tt=tt):
                            for j in range(4):
                                kc = q * 4 + j
                                h.act(hT[:, kc, tt * 128:(tt + 1) * 128], pb[:, j * 128:(j + 1) * 128], AF.Identity,
                                      [pb.r, gs1.r, sh1.r], [hT.r], bias=sh1[:, kc:kc + 1], scale=gs1[:, kc:kc + 1])
                        to_fm(P, h, ps, ident, sq, sq.r, 16, hT, tt, evac)
                    P.barrier()
                if kind == "ssd":
                    with ExitStack() as e2:
                        zs = T(e2, nc, "zs", [128, NTT, NX], F32)
                        xcf = T(e2, nc, "xcf", [128, NXC, TB], F32)
                        BCb = T(e2, nc, "BCb", [128, 2 * NG, TB], BF16)
                        Bf = T(e2, nc, "Bf", [128, NG, TB], F32)
                        xtok = T(e2, nc, "xtok", [128, NTT, NX], F32)
                        Btok = T(e2, nc, "Btok", [128, NTT, NG * 128], BF16)
                        dt = T(e2, nc, "dt", [128, NTT, NHD], F32)
                        with ExitStack() as e3:
                            wst = [T(e3, nc, f"wst{i}", [128, 16, 256], BF16) for i in range(2)]
                            wdt = T(e3, nc, "wdt", [128, 16, NHD], BF16)
                            raw = [T(e3, nc, f"raw{i}", [128, 3 + TB], F32) for i in range(2)]
                            cacc = [T(e3, nc, f"cacc{i}", [128, TB], F32) for i in range(2)]
                            wi = 0
                            for gz in range(NX // 256):
                                wb = wst[wi % 2]
                                wi += 1
                                P.dma("pool", wb[:, :, :], wssd_d[:, gz * 256:(gz + 1) * 256].rearrange("(k p) n -> p k n", p=128), w=[wb.r])
                                for tt in range(NTT):
                                    pb = ps[2 + tt % 2]
                                    for k in range(16):
                                        h.mm(pb[:, 0:256], hT[:, k, tt * 128:(tt + 1) * 128], wb[:, k, :], k == 0, k == 15, [hT.r, wb.r], [pb.r])
                                    h.act(zs[:, tt, gz * 256:(gz + 1) * 256], pb[:, 0:256], AF.Silu, [pb.r], [zs.r])
                            for gx in range(NCH // 2):
                                wb = wst[wi % 2]
                                wi += 1
                                c0 = NX + gx * 256
                                P.dma("pool", wb[:, :, :], wssd_d[:, c0:c0 + 256].rearrange("(k p) n -> p k n", p=128), w=[wb.r])
                                for half in range(2):
                                    cc = gx * 2 + half
                                    pb = ps[4 + cc % 2]
                                    rw = raw[cc % 2]
                                    ca = cacc[cc % 2]
                                    for k in range(16):
                                        h.mm(pb[:, :], wb[:, k, half * 128:(half + 1) * 128], hT[:, k, :], k == 0, k == 15, [wb.r, hT.r], [pb.r])
                                    h.cp("pool", rw[:, 0:3], halo[:, cc, :], [halo.r], [rw.r])
                                    h.cp("act", rw[:, 3:3 + TB], pb[:, :], [pb.r], [rw.r])
                                    h.cp("pool", halo[:, cc, :], rw[:, TB:TB + 3], [rw.r], [halo.r])
                                    h.ts("dve", ca[:, :], rw[:, 0:TB], convw[:, cc, 0:1], None, ALU.mult, None, [rw.r, convw.r], [ca.r])
                                    for k in range(1, 4):
                                        h.stt(ca[:, :], rw[:, k:k + TB], convw[:, cc, k:k + 1], ca[:, :], ALU.mult, ALU.add,
                                              [rw.r, convw.r, ca.r], [ca.r])
                                    if cc < NXC:
                                        h.act(xcf[:, cc, :], ca[:, :], AF.Silu, [ca.r, convb.r], [xcf.r], bias=convb[:, cc:cc + 1])
                                    else:
                                        h.act(BCb[:, cc - NXC, :], ca[:, :], AF.Silu, [ca.r, convb.r], [BCb.r], bias=convb[:, cc:cc + 1])
                                        if cc - NXC < NG:
                                            h.act(Bf[:, cc - NXC, :], ca[:, :], AF.Silu, [ca.r, convb.r], [Bf.r], bias=convb[:, cc:cc + 1])
                            P.dma("pool", wdt[:, :, :], wssd_d[:, WSSD - NHD:WSSD].rearrange("(k p) n -> p k n", p=128), w=[wdt.r])
                            for tt in range(NTT):
                                pb = ps[6 + tt % 2]
                                for k in range(16):
                                    h.mm(pb[:, 0:NHD], hT[:, k, tt * 128:(tt + 1) * 128], wdt[:, k, :], k == 0, k == 15, [hT.r, wdt.r], [pb.r])
                                h.tt("dve", dt[:, tt, :], pb[:, 0:NHD], dtb[:, :], ALU.add, [pb.r, dtb.r], [dt.r])
                            h.act(dt[:, :, :], dt[:, :, :], AF.Exp, [dt.r], [dt.r])
                            h.act(dt[:, :, :], dt[:, :, :], AF.Ln, [dt.r], [dt.r], bias=1.0)
                            for tt in range(NTT):
                                for q in range(NXC // 4):
                                    pb = ps[q % 2]
                                    for j in range(4):
                                        h.tr(pb[:, j * 128:(j + 1) * 128], xcf[:, q * 4 + j, tt * 128:(tt + 1) * 128], ident[:, :],
                                             [xcf.r, ident.r], [pb.r])
                                    h.cp("dve" if q % 2 else "act", xtok[:, tt, q * 512:(q + 1) * 512], pb[:, :], [pb.r], [xtok.r])
                                pb = ps[2 + tt % 2]
                                for g in range(NG):
                                    h.tr(pb[:, g * 128:(g + 1) * 128], Bf[:, g, tt * 128:(tt + 1) * 128], ident[:, :], [Bf.r, ident.r], [pb.r])
                                h.cp("dve", Btok[:, tt, :], pb[:, 0:NG * 128], [pb.r], [Btok.r])
                        P.barrier()
                        with ExitStack() as e4:
                            a_sb = T(e4, nc, "a_sb", [128, NHD], F32)
                            acs = T(e4, nc, "acs", [128, 4, NHD], F32)
                            CBm = T(e4, nc, "CBm", [128, NG, 128], F32)
                            aU = [T(e4, nc, f"aU{i}", [128, 128], F32) for i in range(4)]
                            Eq = [T(e4, nc, f"Eq{i}", [128, 4, 128], F32) for i in range(2)]
                            Mq = [T(e4, nc, f"Mq{i}", [128, 4, 128], BF16) for i in range(2)]
                            xdt = T(e4, nc, "xdt", [128, NX], BF16)
                            xs = T(e4, nc, "xs", [128, NX], BF16)
                            t1 = T(e4, nc, "t1", [128, NX], F32)
                            t3 = T(e4, nc, "t3", [128, NX], F32)
                            yo = [T(e4, nc, f"yo{i}", [128, NX], F32) for i in range(2)]
                            pA, pCB = ps[2], ps[3]
                            pYd = [ps[0], ps[1]]
                            pYo = [ps[6], ps[7]]
                            for tt in range(NTT):
                                cols = slice(tt * 128, (tt + 1) * 128)
                                h.tt("dve", a_sb[:, :], dt[:, tt, :], aneg[:, :], ALU.mult, [dt.r, aneg.r], [a_sb.r])
                                h.mm(pA[:, 0:NHD], ut[:, :], a_sb[:, :], True, True, [ut.r, a_sb.r], [pA.r])
                                h.mm(pA[:, 32:32 + NHD], ones[:, :], a_sb[:, :], True, True, [ones.r, a_sb.r], [pA.r])
                                h.cp("dve", acs[:, 0, :], pA[:, 0:NHD], [pA.r], [acs.r])
                                h.act(acs[:, 1, :], pA[:, 0:NHD], AF.Exp, [pA.r], [acs.r])
                                h.act(acs[:, 2, :], pA[:, 32:32 + NHD], AF.Exp, [pA.r], [acs.r])
                                h.tt("dve", acs[:, 3, :], pA[:, 32:32 + NHD], acs[:, 0, :], ALU.subtract, [pA.r, acs.r], [acs.r])
                                h.act(acs[:, 3, :], acs[:, 3, :], AF.Exp, [acs.r], [acs.r])
                                h.tt("dve", acs[:, 3, :], acs[:, 3, :], dt[:, tt, :], ALU.mult, [acs.r, dt.r], [acs.r])
                                for g in range(NG):
                                    h.mm(pCB[:, g * 128:(g + 1) * 128], BCb[:, g, cols], BCb[:, NG + g, cols], True, True, [BCb.r], [pCB.r])
                                h.tt("dve", CBm[:, :, :], pCB[:, 0:NG * 128].rearrange("p (g l) -> p g l", g=NG),
                                     ut[:, :].unsqueeze(1).to_broadcast([128, NG, 128]), ALU.mult, [pCB.r, ut.r], [CBm.r])
                                h.tt("dve", xdt[:, :].rearrange("p (a b) -> p a b", a=NHD), xtok[:, tt, :].rearrange("p (a b) -> p a b", a=NHD),
                                     dt[:, tt, :].unsqueeze(2).to_broadcast([128, NHD, 64]), ALU.mult, [xtok.r, dt.r], [xdt.r])
                                h.tt("pool", xs[:, :].rearrange("p (a b) -> p a b", a=NHD), xtok[:, tt, :].rearrange("p (a b) -> p a b", a=NHD),
                                     acs[:, 3, :].unsqueeze(2).to_broadcast([128, NHD, 64]), ALU.mult, [xtok.r, acs.r], [xs.r])
                                for g in range(NG):
                                    pS = ps[4 + g % 2]
                                    E_ = Eq[g % 2]
                                    M_ = Mq[g % 2]
                                    for r in range(4):
                                        hd = g * 4 + r
                                        au = aU[r]
                                        h.ts("dve", au[:, :], ut[:, :], a_sb[:, hd:hd + 1], None, ALU.mult, None, [ut.r, a_sb.r], [au.r])
                                        h.mm(pS[:, r * 128:(r + 1) * 128], slt[:, :], au[:, :], True, True, [slt.r, au.r], [pS.r])
                                    h.act(E_[:, :, :], pS[:, :].rearrange("p (a b) -> p a b", a=4), AF.Exp, [pS.r], [E_.r])
                                    h.tt("dve", M_[:, :, :], E_[:, :, :], CBm[:, g, :].unsqueeze(1).to_broadcast([128, 4, 128]), ALU.mult,
                                         [E_.r, CBm.r], [M_.r])
                                    for r in range(4):
                                        hd = g * 4 + r
                                        bank, c_ = hd // 8, (hd % 8) * 64
                                        h.mm(pYd[bank][:, c_:c_ + 64], M_[:, r, :], xdt[:, hd * 64:(hd + 1) * 64], True, True,
                                             [M_.r, xdt.r], [pYd[bank].r])
                                        h.mm(pYo[bank][:, c_:c_ + 64], BCb[:, NG + g, cols], Hb[:, hd * 64:(hd + 1) * 64], True, True,
                                             [BCb.r, Hb.r], [pYo[bank].r])
                                y_ = yo[tt % 2]
                                for bank in range(NHD // 8):
                                    cs = slice(bank * 512, (bank + 1) * 512)
                                    hs = slice(bank * 8, (bank + 1) * 8)
                                    h.tt("dve", t1[:, cs].rearrange("p (a b) -> p a b", a=8), pYo[bank][:, :].rearrange("p (a b) -> p a b", a=8),
                                         acs[:, 1, hs].unsqueeze(2).to_broadcast([128, 8, 64]), ALU.mult, [pYo[bank].r, acs.r], [t1.r])
                                    h.tt("dve", t1[:, cs], t1[:, cs], pYd[bank][:, :], ALU.add, [t1.r, pYd[bank].r], [t1.r])
                                    h.tt("pool", t3[:, cs].rearrange("p (a b) -> p a b", a=8), xtok[:, tt, cs].rearrange("p (a b) -> p a b", a=8),
                                         dsk[:, hs].unsqueeze(2).to_broadcast([128, 8, 64]), ALU.mult, [xtok.r, dsk.r], [t3.r])
                                    h.tt("pool", t3[:, cs], t3[:, cs], t1[:, cs], ALU.add, [t1.r, t3.r], [t3.r])
                                    h.tt("pool", y_[:, cs], t3[:, cs], zs[:, tt, cs], ALU.mult, [t3.r, zs.r], [y_.r])
                                out_toks.append(P.dma("sp", yg_d[t0 + tt * 128:t0 + (tt + 1) * 128, :], y_[:, :], r=[y_.r]))
                                for hd in range(NHD):
                                    g = hd // 4
                                    bank, c_ = hd // 8, (hd % 8) * 64
                                    h.mm(pYd[bank][:, c_:c_ + 64], Btok[:, tt, g * 128:(g + 1) * 128], xs[:, hd * 64:(hd + 1) * 64], True, True,
                                         [Btok.r, xs.r], [pYd[bank].r])
                                h.tt("dve", Hs[:, :].rearrange("p (a b) -> p a b", a=NHD), Hs[:, :].rearrange("p (a b) -> p a b", a=NHD),
                                     acs[:, 2, :].unsqueeze(2).to_broadcast([128, NHD, 64]), ALU.mult, [Hs.r, acs.r], [Hs.r])
                                for bank in range(NHD // 8):
                                    cs = slice(bank * 512, (bank + 1) * 512)
                                    h.tt("dve", Hs[:, cs], Hs[:, cs], pYd[bank][:, :], ALU.add, [Hs.r, pYd[bank].r], [Hs.r])
                                h.cp("act", Hb[:, :], Hs[:, :], [Hs.r], [Hb.r])
                        P.barrier()
                if kind == "gm":
                    r0 = b * TB
                    with ExitStack() as e5:
                        wst = [T(e5, nc, f"wuv{i}", [128, 16, 256], BF16) for i in range(2)]
                        ug = T(e5, nc, "ug", [128, NTT, 2048], F32)
                        vg = T(e5, nc, "vg", [128, NTT, 2048], F32)
                        lng = T(e5, nc, "lng", [128, 2048], F32)
                        lnb = T(e5, nc, "lnb", [128, 2048], F32)
                        vn = T(e5, nc, "vn", [128, 2048], BF16)
                        bst = T(e5, nc, "bst", [128, 8, 6], F32)
                        mv = T(e5, nc, "mv", [128, 4], F32)
                        P.dma("sp", lng[:, :], lng_d[0:1, :].partition_broadcast(128), w=[lng.r])
                        P.dma("sp", lnb[:, :], lnb_d[0:1, :].partition_broadcast(128), w=[lnb.r])
                        wi = 0
                        for gu in range(16):
                            wb = wst[wi % 2]
                            wi += 1
                            P.dma("pool", wb[:, :, :], wuv_d[:, gu * 256:(gu + 1) * 256].rearrange("(k p) n -> p k n", p=128), w=[wb.r])
                            dst = ug if gu < 8 else vg
                            cg = (gu % 8) * 256
                            for tt in range(NTT):
                                pb = ps[2 + tt % 2]
                                for k in range(16):
                                    h.mm(pb[:, 0:256], hT[:, k, tt * 128:(tt + 1) * 128], wb[:, k, :], k == 0, k == 15, [hT.r, wb.r], [pb.r])
                                h.act(dst[:, tt, cg:cg + 256], pb[:, 0:256], AF.Gelu, [pb.r], [dst.r])
                        for tt in range(NTT):
                            for q in range(4):
                                h.P.op("dve", lambda e, q=q, tt=tt: e.bn_stats(out=bst[:, q, :], in_=vg[:, tt, q * 512:(q + 1) * 512]),
                                       [vg.r], [bst.r])
                            h.P.op("dve", lambda e: e.bn_aggr(out=mv[:, 0:2], in_=bst[:, 0:4, :].rearrange("p a b -> p (a b)")), [bst.r], [mv.r])
                            h.ts("dve", mv[:, 2:3], mv[:, 1:2], EPS, None, ALU.add, None, [mv.r], [mv.r])
                            h.act(mv[:, 2:3], mv[:, 2:3], AF.Sqrt, [mv.r], [mv.r])
                            h.recip(mv[:, 2:3], mv[:, 2:3], [mv.r], [mv.r])
                            h.ts("dve", vg[:, tt, :], vg[:, tt, :], mv[:, 0:1], mv[:, 2:3], ALU.subtract, ALU.mult, [vg.r, mv.r], [vg.r])
                            h.tt("pool", vg[:, tt, :], vg[:, tt, :], lng[:, :], ALU.mult, [vg.r, lng.r], [vg.r])
                            h.tt("pool", vn[:, :], vg[:, tt, :], lnb[:, :], ALU.add, [vg.r, lnb.r], [vn.r])
                            pv = [ps[0], ps[1], ps[6], ps[7]]
                            for g in range(16):
                                pb = pv[g // 4]
                                h.mm(pb[:, (g % 4) * 128:(g % 4 + 1) * 128], wsT[:, g, :], vn[:, g * 128:(g + 1) * 128], True, True,
                                     [wsT.r, vn.r], [pb.r])
                            for q in range(4):
                                cs = slice(q * 512, (q + 1) * 512)
                                h.tt("dve", vg[:, tt, cs].rearrange("p (a b) -> p a b", a=4), pv[q][:, :].rearrange("p (a b) -> p a b", a=4),
                                     bsT[:, q * 4:(q + 1) * 4].unsqueeze(2).to_broadcast([128, 4, 128]), ALU.add, [pv[q].r, bsT.r], [vg.r])
                            h.tt("pool", vg[:, tt, :], vg[:, tt, :], ug[:, tt, :], ALU.mult, [vg.r, ug.r], [vg.r])
                            out_toks.append(P.dma("sp", yb_d[r0 + tt * 128:r0 + (tt + 1) * 128, :], vg[:, tt, :], r=[vg.r]))
                    P.barrier()
        P.wait("sp", out_toks[-48:])
        if ctx is None:
            P.emit()
        print("mix0 instrs", P.ninstr, {e: len(P.q[e]) for e in P.ENGS})
    return nc


def _fm(v):
    return np.ascontiguousarray(np.asarray(v, dtype=np.float32).reshape(-1, 128).T)


def _c(a):
    return np.ascontiguousarray(np.asarray(a, dtype=np.float32))


def _consts():
    f = np.float32
    jj = np.arange(128)
    s_ = jj[:, None, None]
    j_ = np.arange(4)[None, :, None]
    t_ = np.arange(512)[None, None, :]
    return dict(
        ident=np.eye(128, dtype=f),
        ut=(jj[:, None] <= jj[None, :]).astype(f),
        slt=(jj[:, None] > jj[None, :]).astype(f),
        masks=((j_ * 128 + s_) < t_).astype(f),
        ntri=-(jj[:, None] >= jj[None, :]).astype(f),
        summ=(jj[:, None] // 8 == np.arange(16)[None, :]).astype(f),
    )


def _mix0_inputs(p, in0_w, conv_w, conv_b, dt_bias, a_log, d_skip, ln_g, ln_b, ws, bs):
    zc = slice(p * 1024, (p + 1) * 1024)
    xc = slice(2048 + p * 1024, 2048 + (p + 1) * 1024)
    Bc = slice(4096 + p * 512, 4096 + (p + 1) * 512)
    Cc = slice(5120 + p * 512, 5120 + (p + 1) * 512)
    dc = slice(6144 + p * 16, 6144 + (p + 1) * 16)
    w_ssd = np.concatenate([in0_w[:, zc], in0_w[:, xc], in0_w[:, Bc], in0_w[:, Cc], in0_w[:, dc]], axis=1)
    cch = np.concatenate([np.arange(p * 1024, (p + 1) * 1024), 2048 + np.arange(p * 512, (p + 1) * 512),
                          3072 + np.arange(p * 512, (p + 1) * 512)])
    cw = conv_w[:, cch]
    cb = conv_b[cch]
    hs = slice(p * 16, (p + 1) * 16)
    return dict(w_ssd=_c(w_ssd), conv_w_fm=_c(cw.T.reshape(16, 128, 4).transpose(1, 0, 2)), conv_b_fm=_fm(cb),
                dt_bias=_c(dt_bias[None, hs]), a_log=_c(a_log[None, hs]), d_skip=_c(d_skip[None, hs]),
                w_uv=_c(in0_w[:, 6176:]), ln_g=_c(ln_g[None]), ln_b=_c(ln_b[None]),
                wsT=_c(ws.transpose(2, 0, 1)), bsT=_c(bs.T))


def kernel_unfused(x, c, ada_w, ada_b, norm_mix_g, norm_ffn_g, in0_w, conv_w, conv_b, dt_bias, a_log, d_skip, ssd_norm_g,
           gmlp_ln_g, gmlp_ln_b, gmlp_ws, gmlp_bs, out0_w, sb_qkv_w, sb_out_w, peer_wq, peer_keys, peer_u, peer_v, final_g):
    g = {k: np.asarray(v) for k, v in locals().items()}
    x = g["x"]
    c = g["c"]
    K = _consts()
    cores = list(range(8))
    HALF = 2048

    def ada_in(layer, b):
        return dict(c_fm=_fm(c[b]), ada_w=_c(g["ada_w"][layer]), ada_b_fm=_fm(g["ada_b"][layer]), ada_b=_c(g["ada_b"][layer][None]))

    nc1 = build_mix0(4096, 16, 4)
    mi = [_mix0_inputs(p, g["in0_w"][0], g["conv_w"][0], g["conv_b"][0], g["dt_bias"][0], g["a_log"][0], g["d_skip"][0],
                       g["gmlp_ln_g"][0], g["gmlp_ln_b"][0], g["gmlp_ws"][0], g["gmlp_bs"][0]) for p in range(2)]
    maps = []
    for core in cores:
        b, p = divmod(core, 2)
        d = dict(x=_c(x[b]), x_gm=_c(x[b, p * HALF:(p + 1) * HALF]), g_mix_fm=_fm(g["norm_mix_g"][0]),
                 ident=K["ident"], ut=K["ut"], slt=K["slt"])
        d.update(ada_in(0, b))
        d.update(mi[p])
        maps.append(d)
    r1 = run_bass_kernel_spmd(nc1, maps, core_ids=cores).results
    del maps
    nc2 = build_tail(HALF, 4096, final=False, ya_norm=True)
    maps = []
    for core in cores:
        b, p = divmod(core, 2)
        rows = slice(p * HALF, (p + 1) * HALF)
        mix = np.concatenate([r1[2 * b]["yg"][rows], r1[2 * b + 1]["yg"][rows], r1[core]["yb"]], axis=1)
        d = dict(x=_c(x[b, rows]), mix=_c(mix), wo=_c(g["out0_w"][0]), g_ffn_fm=_fm(g["norm_ffn_g"][0]), wq=_c(g["peer_wq"][0]),
                 keys=_c(g["peer_keys"][0].reshape(16, 128, 128)), U=_c(g["peer_u"][0]), V=_c(g["peer_v"][0]),
                 final_g=_c(g["final_g"][None]), g_ya_fm=_fm(g["ssd_norm_g"][0]), ident=K["ident"], summ=K["summ"])
        d.update(ada_in(0, b))
        maps.append(d)
    r2 = run_bass_kernel_spmd(nc2, maps, core_ids=cores).results
    del maps, r1
    nc3 = build_attn(4096, 8, 4)
    qkv = g["sb_qkv_w"][0]
    maps = []
    for core in cores:
        b, p = divmod(core, 2)
        hs = slice(p * 1024, (p + 1) * 1024)
        wqkv = np.concatenate([qkv[:, 0:2048][:, hs], qkv[:, 2048:4096][:, hs], qkv[:, 4096:6144][:, hs]], axis=1)
        xf = np.concatenate([r2[2 * b]["out"], r2[2 * b + 1]["out"]], axis=0)
        d = dict(x=_c(xf), g_mix_fm=_fm(g["norm_mix_g"][1]), wqkv=_c(wqkv), ident=K["ident"], masks=K["masks"], ntri=K["ntri"])
        d.update(ada_in(1, b))
        maps.append(d)
    r3 = run_bass_kernel_spmd(nc3, maps, core_ids=cores).results
    del maps
    nc4 = build_tail(HALF, 2048, final=True, ya_norm=False)
    maps = []
    for core in cores:
        b, p = divmod(core, 2)
        rows = slice(p * HALF, (p + 1) * HALF)
        mix = np.concatenate([r3[2 * b]["oT"][:, rows].T, r3[2 * b + 1]["oT"][:, rows].T], axis=1)
        d = dict(x=_c(r2[core]["out"]), mix=_c(mix), wo=_c(g["sb_out_w"][0]), g_ffn_fm=_fm(g["norm_ffn_g"][1]), wq=_c(g["peer_wq"][1]),
                 keys=_c(g["peer_keys"][1].reshape(16, 128, 128)), U=_c(g["peer_u"][1]), V=_c(g["peer_v"][1]),
                 final_g=_c(g["final_g"][None]), g_ya_fm=_fm(g["ssd_norm_g"][0]), ident=K["ident"], summ=K["summ"])
        d.update(ada_in(1, b))
        maps.append(d)
    r4 = run_bass_kernel_spmd(nc4, maps, core_ids=cores).results
    out = np.empty((4, 4096, 2048), dtype=np.float32)
    for core in cores:
        b, p = divmod(core, 2)
        out[b, p * HALF:(p + 1) * HALF] = r4[core]["out"]
    return out


def build_fused(S_LEN=4096):
    from contextlib import ExitStack
    nc = bass.Bass("TRN2", target_bir_lowering=False)
    HALF = S_LEN // 2

    def ein(name, shape):
        return nc.dram_tensor(name, shape, F32, kind="ExternalInput").ap()

    x_d = ein("x", [S_LEN, D])
    cfm = ein("c_fm", [128, 16])
    adaw = ein("ada_w", [2, D, 6 * D])
    adabfm = ein("ada_b_fm", [2, 128, 96])
    adab = ein("ada_b", [2, 1, 6 * D])
    gmix = ein("g_mix_fm", [2, 128, 16])
    gffn = ein("g_ffn_fm", [2, 128, 16])
    wssd = ein("w_ssd", [2, D, 3088])
    convw = ein("conv_w_fm", [2, 128, 16, 4])
    convb = ein("conv_b_fm", [2, 128, 16])
    dtb = ein("dt_bias", [2, 1, 16])
    alog = ein("a_log", [2, 1, 16])
    dsk = ein("d_skip", [2, 1, 16])
    wuv = ein("w_uv", [D, 4096])
    lng = ein("ln_g", [1, 2048])
    lnb = ein("ln_b", [1, 2048])
    wsT = ein("wsT", [128, 16, 128])
    bsT = ein("bsT", [128, 16])
    wo0 = ein("out0_w", [4096, D])
    gya = ein("g_ya_fm", [128, 16])
    wq = ein("wq", [2, D, D])
    keys = ein("keys", [2, 16, 128, 128])
    U = ein("U", [2, 16384, D])
    V = ein("V", [2, 16384, D])
    fing = ein("final_g", [1, D])
    wqkv = ein("wqkv", [D, 3 * D])
    wo1 = ein("sb_out_w", [D, D])
    sel = ein("sel", [128, 2])
    ident = ein("ident", [128, 128])
    ut = ein("ut", [128, 128])
    slt = ein("slt", [128, 128])
    masks = ein("masks", [128, 4, 512])
    ntri = ein("ntri", [128, 128])
    summ = ein("summ", [128, 16])
    mixA = nc.dram_tensor("mixA", [S_LEN, 4096], F32, kind="Internal").ap()
    x2 = nc.dram_tensor("x2", [S_LEN, D], F32, kind="Internal").ap()
    o_d = nc.dram_tensor("o_int", [S_LEN, D], F32, kind="Internal").ap()
    out_d = nc.dram_tensor("out", [HALF, D], F32, kind="ExternalOutput").ap()

    def ada_env(layer):
        return dict(c_fm=cfm, ada_w=adaw[layer], ada_b_fm=adabfm[layer], ada_b=adab[layer])

    with ExitStack() as es:
        ctx = Ctx(nc, es)
        for p in range(2):
            env = dict(x=x_d, x_gm=x_d, g_mix_fm=gmix[0], w_ssd=wssd[p], conv_w_fm=convw[p], conv_b_fm=convb[p],
                       dt_bias=dtb[p], a_log=alog[p], d_skip=dsk[p], w_uv=wuv, ln_g=lng, ln_b=lnb, wsT=wsT, bsT=bsT,
                       ident=ident, ut=ut, slt=slt, yg=mixA[:, p * 1024:(p + 1) * 1024], yb=mixA[:, 2048:4096])
            env.update(ada_env(0))
            build_mix0(S_LEN, 16, gm_nblk=(S_LEN // TB if p == 0 else 0), ctx=ctx, env=env, pfx=f"m{p}_")
        env = dict(x=x_d, mix=mixA, wo=wo0, g_ffn_fm=gffn[0], wq=wq[0], keys=keys[0], U=U[0], V=V[0], final_g=fing,
                   g_ya_fm=gya, ident=ident, summ=summ, out=x2)
        env.update(ada_env(0))
        build_tail(S_LEN, 4096, final=False, ya_norm=True, ctx=ctx, env=env, pfx="t0_")
        env = dict(x=x2, g_mix_fm=gmix[1], wqkv=wqkv, ident=ident, masks=masks, ntri=ntri, o=o_d)
        env.update(ada_env(1))
        build_attn(S_LEN, 16, 4, ctx=ctx, env=env, pfx="a_", tokmajor=True)
        env = dict(x=x2, mix=o_d, wo=wo1, g_ffn_fm=gffn[1], wq=wq[1], keys=keys[1], U=U[1], V=V[1], final_g=fing,
                   g_ya_fm=gya, ident=ident, summ=summ, out=out_d, sel=sel)
        env.update(ada_env(1))
        build_tail(HALF, 2048, final=True, ya_norm=False, ctx=ctx, env=env, pfx="t1_", blend=True)
        ctx.P.emit()
        print("fused instrs", ctx.P.ninstr, {e: len(ctx.P.q[e]) for e in ctx.P.ENGS})
    return nc


def kernel(x, c, ada_w, ada_b, norm_mix_g, norm_ffn_g, in0_w, conv_w, conv_b, dt_bias, a_log, d_skip, ssd_norm_g,
           gmlp_ln_g, gmlp_ln_b, gmlp_ws, gmlp_bs, out0_w, sb_qkv_w, sb_out_w, peer_wq, peer_keys, peer_u, peer_v, final_g):
    g = {k: np.asarray(v) for k, v in locals().items()}
    K = _consts()
    cores = list(range(8))
    mi = [_mix0_inputs(p, g["in0_w"][0], g["conv_w"][0], g["conv_b"][0], g["dt_bias"][0], g["a_log"][0], g["d_skip"][0],
                       g["gmlp_ln_g"][0], g["gmlp_ln_b"][0], g["gmlp_ws"][0], g["gmlp_bs"][0]) for p in range(2)]
    shared = dict(
        ada_w=_c(g["ada_w"]), ada_b_fm=_c(np.stack([_fm(g["ada_b"][l]) for l in range(2)])), ada_b=_c(g["ada_b"][:, None, :]),
        g_mix_fm=_c(np.stack([_fm(g["norm_mix_g"][l]) for l in range(2)])),
        g_ffn_fm=_c(np.stack([_fm(g["norm_ffn_g"][l]) for l in range(2)])),
        w_uv=mi[0]["w_uv"], ln_g=mi[0]["ln_g"], ln_b=mi[0]["ln_b"], wsT=mi[0]["wsT"], bsT=mi[0]["bsT"],
        out0_w=_c(g["out0_w"][0]), g_ya_fm=_fm(g["ssd_norm_g"][0]), wq=_c(g["peer_wq"]),
        keys=_c(g["peer_keys"].reshape(2, 16, 128, 128)), U=_c(g["peer_u"]), V=_c(g["peer_v"]),
        final_g=_c(g["final_g"][None]), wqkv=_c(g["sb_qkv_w"][0]), sb_out_w=_c(g["sb_out_w"][0]),
        ident=K["ident"], ut=K["ut"], slt=K["slt"], masks=K["masks"], ntri=K["ntri"], summ=K["summ"])
    for k in ("w_ssd", "conv_w_fm", "conv_b_fm", "dt_bias", "a_log", "d_skip"):
        shared[k] = _c(np.stack([mi[0][k], mi[1][k]]))
    nc = build_fused(4096)
    maps = []
    for core in cores:
        b, p = divmod(core, 2)
        d = dict(shared)
        d["x"] = _c(g["x"][b])
        d["c_fm"] = _fm(g["c"][b])
        selv = np.zeros((128, 2), dtype=np.float32)
        selv[:, p] = 1.0
        d["sel"] = selv
        maps.append(d)
    res = run_bass_kernel_spmd(nc, maps, core_ids=cores).results
    out = np.empty((4, 4096, 2048), dtype=np.float32)
    for core in cores:
        b, p = divmod(core, 2)
        out[b, p * 2048:(p + 1) * 2048] = res[core]["out"]
    return out
```

```python
import numpy as np
import concourse.bass as bass
import concourse.mybir as mybir
from concourse.bass_utils import run_bass_kernel_spmd

F32 = mybir.dt.float32
BF16 = mybir.dt.bfloat16
AF = mybir.ActivationFunctionType
ALU = mybir.AluOpType
AX = mybir.AxisListType

D = 2048
DC = 16
EPS = 1e-6
NEG = -1.0e30


class Res:
    __slots__ = ("w", "rs", "name")

    def __init__(self, name=""):
        self.w = None
        self.rs = {}
        self.name = name


class Prog:
    ENGS = ["pe", "act", "dve", "pool", "sp"]

    def __init__(self, nc, n_dma_sems=40):
        self.nc = nc
        self.q = {e: [] for e in self.ENGS}
        self.sem = {e: nc.alloc_semaphore(name=f"sem_{e}") for e in self.ENGS}
        self.cnt = {e: 0 for e in self.ENGS}
        self.seen = {e: {} for e in self.ENGS}
        self.dsem = [nc.alloc_semaphore(name=f"dsem{i}") for i in range(n_dma_sems)]
        self.dcnt = [0] * n_dma_sems
        self.dnext = 0
        self.ninstr = 0

    def _need(self, e, waits, tok):
        if tok is None:
            return
        sem, val = tok
        if sem.num == self.sem[e].num and e == "pe":
            return
        if self.seen[e].get(sem.num, 0) >= val:
            return
        self.seen[e][sem.num] = val
        waits.append((sem, val))

    def _deps(self, e, r, w):
        waits = []
        for res in r:
            self._need(e, waits, res.w)
        for res in w:
            self._need(e, waits, res.w)
            for t in res.rs.values():
                self._need(e, waits, t)
        return waits

    def _commit(self, tok, r, w):
        for res in r:
            res.rs[tok[0].num] = tok
        for res in w:
            res.w = tok
            res.rs = {}

    def op(self, e, fn, r=(), w=()):
        waits = self._deps(e, r, w)
        self.cnt[e] += 1
        tok = (self.sem[e], self.cnt[e])
        self.q[e].append((waits, fn, self.sem[e], 1))
        self._commit(tok, r, w)
        self.ninstr += 1 + len(waits)
        return tok

    def dma(self, e, out, in_, r=(), w=(), **kw):
        waits = self._deps(e, r, w)
        i = self.dnext
        self.dnext = (self.dnext + 1) % len(self.dsem)
        if self.dcnt[i] > 0:
            self._need(e, waits, (self.dsem[i], self.dcnt[i]))
        self.dcnt[i] += 16
        tok = (self.dsem[i], self.dcnt[i])
        self.q[e].append((waits, lambda eng: eng.dma_start(out=out, in_=in_, **kw), self.dsem[i], 16))
        self._commit(tok, r, w)
        self.ninstr += 1 + len(waits)
        return tok

    def wait(self, e, toks):
        waits = []
        for t in toks:
            self._need(e, waits, t)
        if waits:
            self.q[e].append((waits, None, None, 0))

    def barrier(self):
        toks = [(self.sem[e], self.cnt[e]) for e in self.ENGS if self.cnt[e] > 0]
        toks += [(self.dsem[i], self.dcnt[i]) for i in range(len(self.dsem)) if self.dcnt[i] > 0]
        for e in self.ENGS:
            self.wait(e, toks)

    def emit(self):
        nc = self.nc
        q = self.q

        def run(eng, items):
            for waits, fn, sem, inc in items:
                for s, v in waits:
                    eng.wait_ge(s, v)
                if fn is not None:
                    fn(eng).then_inc(sem, inc)

        with nc.Block() as block:
            @block.tensor
            def _(t):
                run(t, q["pe"])

            @block.scalar
            def _(t):
                run(t, q["act"])

            @block.vector
            def _(t):
                run(t, q["dve"])

            @block.gpsimd
            def _(t):
                run(t, q["pool"])

            @block.sync
            def _(t):
                run(t, q["sp"])


class T:
    _n = [0]

    def __init__(self, es, nc, name, shape, dtype, psum=False):
        T._n[0] += 1
        name = f"{name}_u{T._n[0]}"
        cm = nc.psum_tensor(name, shape, dtype) if psum else nc.sbuf_tensor(name, shape, dtype)
        self.t = es.enter_context(cm)
        self.r = Res(name)

    def __getitem__(self, k):
        return self.t[k]


class Ctx:
    def __init__(self, nc, es):
        self.nc = nc
        self.P = Prog(nc)
        self.h = H(self.P)
        self.ps = [T(es, nc, f"ps{i}", [128, 512], F32, psum=True) for i in range(8)]
        self.out_toks = []


def _io(nc, env, pfx):
    def din(name, shape):
        if env is not None and name in env:
            return env[name]
        return nc.dram_tensor(pfx + name, shape, F32, kind="ExternalInput").ap()

    def dout(name, shape):
        if env is not None and name in env:
            return env[name]
        return nc.dram_tensor(pfx + name, shape, F32, kind="ExternalOutput").ap()
    return din, dout


class H:
    def __init__(self, P):
        self.P = P

    def mm(self, out, lhsT, rhs, start, stop, r, w):
        return self.P.op("pe", lambda e: e.matmul(out, lhsT=lhsT, rhs=rhs, start=start, stop=stop), r, w)

    def tr(self, out, in_, ident, r, w):
        return self.P.op("pe", lambda e: e.transpose(out, in_, ident), r, w)

    def act(self, out, in_, func, r, w, bias=None, scale=None, accum=None):
        kw = {}
        if bias is not None:
            kw["bias"] = bias
        if scale is not None:
            kw["scale"] = scale
        if accum is not None:
            kw["accum_out"] = accum
        return self.P.op("act", lambda e: e.activation(out=out, in_=in_, func=func, **kw), r, w)

    def tt(self, eng, out, in0, in1, op, r, w):
        return self.P.op(eng, lambda e: e.tensor_tensor(out=out, in0=in0, in1=in1, op=op), r, w)

    def ts(self, eng, out, in0, s1, s2, op0, op1, r, w, accum=None):
        if accum is not None:
            return self.P.op(eng, lambda e: e.tensor_scalar(out=out, in0=in0, scalar1=s1, scalar2=s2, op0=op0, op1=op1, accum_out=accum), r, w)
        if op1 is None:
            return self.P.op(eng, lambda e: e.tensor_scalar(out=out, in0=in0, scalar1=s1, scalar2=None, op0=op0), r, w)
        return self.P.op(eng, lambda e: e.tensor_scalar(out=out, in0=in0, scalar1=s1, scalar2=s2, op0=op0, op1=op1), r, w)

    def stt(self, out, in0, scalar, in1, op0, op1, r, w):
        return self.P.op("dve", lambda e: e.scalar_tensor_tensor(out=out, in0=in0, scalar=scalar, in1=in1, op0=op0, op1=op1), r, w)

    def cp(self, eng, out, in_, r, w):
        if eng == "act":
            return self.P.op("act", lambda e: e.copy(out=out, in_=in_), r, w)
        return self.P.op(eng, lambda e: e.tensor_copy(out=out, in_=in_), r, w)

    def memset(self, eng, ap, val, w):
        return self.P.op(eng, lambda e: e.memset(ap, val), (), w)

    def max8(self, out, in_, r, w):
        return self.P.op("dve", lambda e: e.max(out=out, in_=in_), r, w)

    def mrep(self, out, rep, vals, r, w):
        return self.P.op("dve", lambda e: e.match_replace(out=out, in_to_replace=rep, in_values=vals, imm_value=NEG), r, w)

    def recip(self, out, in_, r, w):
        return self.P.op("dve", lambda e: e.reciprocal(out=out, in_=in_), r, w)

    def red(self, out, in_, op, r, w):
        return self.P.op("dve", lambda e: e.tensor_reduce(out=out, in_=in_, axis=AX.X, op=op), r, w)


TB = 512
NTT = TB // 128
NJ = TB // 16


def ada_compute(es0, nc, P, h, ps, cfm_d, adaw_d, adabfm_d, adab_d, fm_secs, bc_secs, tag):
    from contextlib import ExitStack
    outs = {}
    for s in fm_secs:
        outs[s] = T(es0, nc, f"ada_fm{tag}_{s}", [128, 16], F32)
    for s in bc_secs:
        outs[s] = T(es0, nc, f"ada_bc{tag}_{s}", [128, 2048], F32)
    with ExitStack() as es:
        condT = T(es, nc, f"condT{tag}", [128, 16], F32)
        condB = T(es, nc, f"condB{tag}", [128, 16, 128], F32)
        abfm = T(es, nc, f"abfm{tag}", [128, 96], F32)
        wblk = [T(es, nc, f"adaw{tag}_{i}", [128, 16, 512], F32) for i in range(2)]
        abb = [T(es, nc, f"adabb{tag}_{i}", [128, 512], F32) for i in range(2)]
        P.dma("sp", condT[:, :], cfm_d, w=[condT.r])
        P.dma("sp", abfm[:, :], adabfm_d, w=[abfm.r])
        h.act(condT[:, :], condT[:, :], AF.Silu, [condT.r], [condT.r])
        h.cp("dve", condB[:, :, :], condT[:, :].unsqueeze(2).to_broadcast([128, 16, 128]), [condT.r], [condB.r])
        it = 0
        for s in sorted(set(fm_secs) | set(bc_secs)):
            for cb in range(4):
                wb = wblk[it % 2]
                ab = abb[it % 2]
                it += 1
                c0 = s * 2048 + cb * 512
                P.dma("sp", wb[:, :, :], adaw_d[:, c0:c0 + 512].rearrange("(k p) n -> p k n", p=128), w=[wb.r])
                if s in bc_secs:
                    P.dma("sp", ab[:, :], adab_d[0:1, c0:c0 + 512].partition_broadcast(128), w=[ab.r])
                    pb = ps[it % 2]
                    for k in range(16):
                        h.mm(pb[:, :], condB[:, k, :], wb[:, k, :], k == 0, k == 15, [condB.r, wb.r], [pb.r])
                    h.tt("dve", outs[s][:, cb * 512:(cb + 1) * 512], pb[:, :], ab[:, :], ALU.add, [pb.r, ab.r], [outs[s].r])
                if s in fm_secs:
                    pf = ps[2 + (it % 2)]
                    for jj in range(4):
                        for k in range(16):
                            h.mm(pf[:, jj:jj + 1], wb[:, k, jj * 128:(jj + 1) * 128], condT[:, k:k + 1], k == 0, k == 15,
                                 [condT.r, wb.r], [pf.r])
                    j0 = cb * 4
                    h.tt("dve", outs[s][:, j0:j0 + 4], pf[:, 0:4], abfm[:, s * 16 + j0:s * 16 + j0 + 4], ALU.add,
                         [pf.r, abfm.r], [outs[s].r])
    P.barrier()
    return outs


def rms_stats(P, h, x_ap, sq_scr, ssq_ap, rstd_ap, r, scr_res, st_res, n):
    h.act(sq_scr, x_ap, AF.Square, r, [scr_res, st_res], accum=ssq_ap)
    h.ts("dve", rstd_ap, ssq_ap, 1.0 / n, EPS, ALU.mult, ALU.add, [st_res], [st_res])
    h.act(rstd_ap, rstd_ap, AF.Sqrt, [st_res], [st_res])
    h.recip(rstd_ap, rstd_ap, [st_res], [st_res])


def to_fm(P, h, ps, ident, src, src_res, ncol, dst, tt, evac):
    for q in range(ncol // 4):
        pb = ps[q % 2]
        for j in range(4):
            kc = q * 4 + j
            h.tr(pb[:, j * 128:(j + 1) * 128], src[:, kc * 128:(kc + 1) * 128], ident[:, :], [src_res, ident.r], [pb.r])
        evac(q, pb)


def build_tail(NT, KMIX, final, ya_norm, n_i1=128, ctx=None, env=None, pfx="", blend=False):
    from contextlib import ExitStack
    nc = ctx.nc if ctx else bass.Bass("TRN2", target_bir_lowering=False)
    NB = NT // TB
    KC = KMIX // 128
    GV = 4
    din, dout = _io(nc, env, pfx)
    NSRC = 2 * NT if blend else NT
    sel_d = din("sel", [128, 2]) if blend else None
    x_d = din("x", [NSRC, D])
    mix_d = din("mix", [NSRC, KMIX])
    wo_d = din("wo", [KMIX, D])
    cfm_d = din("c_fm", [128, 16])
    adaw_d = din("ada_w", [D, 6 * D])
    adabfm_d = din("ada_b_fm", [128, 96])
    adab_d = din("ada_b", [1, 6 * D])
    gffn_d = din("g_ffn_fm", [128, 16])
    wq_d = din("wq", [D, D])
    keys_d = din("keys", [16, 128, 128])
    U_d = din("U", [16384, D])
    V_d = din("V", [16384, D])
    fing_d = din("final_g", [1, D])
    gya_d = din("g_ya_fm", [128, 16])
    ident_d = din("ident", [128, 128])
    summ_d = din("summ", [128, 16])
    out_d = dout("out", [NT, D])
    sc_d = nc.dram_tensor(pfx + "sc_scr", [TB, 2048], F32, kind="Internal").ap()
    bc_d = nc.dram_tensor(pfx + "bc_scr", [2, 128, 2048], F32, kind="Internal").ap()
    scd_res = Res("sc_d")
    bcd_res = Res("bc_d")
    UT_d = nc.dram_tensor(pfx + "UT_scr", [n_i1, 128, 2048], BF16, kind="Internal").ap()
    Vb_d = nc.dram_tensor(pfx + "Vb_scr", [n_i1 * 128, D], BF16, kind="Internal").ap()

    with ExitStack() as es:
        if ctx is None:
            P = Prog(nc)
            h = H(P)
            ps = [T(es, nc, f"ps{i}", [128, 512], F32, psum=True) for i in range(8)]
        else:
            P, h, ps = ctx.P, ctx.h, ctx.ps
        ident = T(es, nc, "ident", [128, 128], F32)
        summ = T(es, nc, "summ", [128, 16], F32)
        sel = T(es, nc, "sel", [128, 2], F32)
        if blend:
            P.dma("sp", sel[:, :], sel_d, w=[sel.r])
        gffn = T(es, nc, "gffn", [128, 16], F32)
        gya = T(es, nc, "gya", [128, 16], F32)
        gs2 = T(es, nc, "gs2", [128, 16], F32)
        keysT = T(es, nc, "keysT", [128, 16, 128], F32)
        P.dma("sp", ident[:, :], ident_d, w=[ident.r])
        P.dma("sp", summ[:, :], summ_d, w=[summ.r])
        P.dma("sp", gffn[:, :], gffn_d, w=[gffn.r])
        P.dma("sp", gya[:, :], gya_d, w=[gya.r])

        sh2 = T(es, nc, "sh2", [128, 16], F32)
        with ExitStack() as es1:
            ada = ada_compute(es1, nc, P, h, ps, cfm_d, adaw_d, adabfm_d, adab_d, fm_secs=[3, 4], bc_secs=[2, 5], tag="t")
            h.cp("dve", sh2[:, :], ada[3][:, :], [ada[3].r], [sh2.r])
            h.stt(gs2[:, :], ada[4][:, :], 1.0, gffn[:, :], ALU.add, ALU.mult, [ada[4].r, gffn.r], [gs2.r])
            P.dma("sp", bc_d[0], ada[2][:, :], r=[ada[2].r], w=[bcd_res])
            P.dma("sp", bc_d[1], ada[5][:, :], r=[ada[5].r], w=[bcd_res])
            kraw = T(es1, nc, "kraw", [128, 16, 128], F32)
            P.dma("sp", kraw[:, :, :], keys_d.rearrange("a k c -> k a c"), w=[kraw.r])
            for q in range(4):
                pb = ps[4 + q % 2]
                for j in range(4):
                    h.tr(pb[:, j * 128:(j + 1) * 128], kraw[:, q * 4 + j, :], ident[:, :], [kraw.r, ident.r], [pb.r])
                h.cp("dve", keysT[:, q * 4:(q + 1) * 4, :], pb[:, :].rearrange("p (a b) -> p a b", a=4), [pb.r], [keysT.r])
            P.barrier()

        with ExitStack() as esp:
            Uraw = [T(esp, nc, f"Uraw{i}", [128, D], F32) for i in range(2)]
            utb = [T(esp, nc, f"utb{i}", [128, 16, 128], BF16) for i in range(2)]
            VR = 1024
            for r0 in range(0, n_i1 * 128, VR):
                P.dma("pool", Vb_d[r0:r0 + VR, :], V_d[r0:r0 + VR, :])
            for i1 in range(n_i1):
                u = Uraw[i1 % 2]
                ut = utb[i1 % 2]
                P.dma("sp", u[:, :], U_d[i1 * 128:(i1 + 1) * 128, :], w=[u.r])
                for q in range(4):
                    pb = ps[(i1 * 4 + q) % 4]
                    for j in range(4):
                        h.tr(pb[:, j * 128:(j + 1) * 128], u[:, (q * 4 + j) * 128:(q * 4 + j + 1) * 128], ident[:, :],
                             [u.r, ident.r], [pb.r])
                    h.cp("act" if q % 2 else "dve", ut[:, q * 4:(q + 1) * 4, :], pb[:, :].rearrange("p (a b) -> p a b", a=4),
                         [pb.r], [ut.r])
                P.dma("sp", UT_d[i1].rearrange("p (k e) -> p k e", k=16), ut[:, :, :], r=[ut.r])
            P.barrier()

        for b in range(NB):
            t0 = b * TB
            ores = Res(f"out_blk{b}")
            oblk = out_d[t0:t0 + TB, :].rearrange("(t p) d -> p t d", p=128)
            with ExitStack() as esb:
                hT = T(esb, nc, f"hT", [128, 16, TB], BF16)
                with ExitStack() as esa:
                    xb = T(esa, nc, "xb", [128, NTT, D], F32)
                    P.dma("sp", xb[:, :, :], x_d[t0:t0 + TB, :].rearrange("(t p) d -> p t d", p=128), w=[xb.r])
                    if blend:
                        with ExitStack() as esx:
                            xb2 = T(esx, nc, "xb2", [128, NTT, D], F32)
                            P.dma("sp", xb2[:, :, :], x_d[NT + t0:NT + t0 + TB, :].rearrange("(t p) d -> p t d", p=128), w=[xb2.r])
                            for tt in range(NTT):
                                h.ts("dve", xb[:, tt, :], xb[:, tt, :], sel[:, 0:1], None, ALU.mult, None, [xb.r, sel.r], [xb.r])
                                h.stt(xb[:, tt, :], xb2[:, tt, :], sel[:, 1:2], xb[:, tt, :], ALU.mult, ALU.add, [xb2.r, sel.r, xb.r], [xb.r])
                            P.barrier()
                    g1 = T(esa, nc, "g1", [128, D], F32)
                    P.dma("sp", g1[:, :], bc_d[0], r=[bcd_res], w=[g1.r])
                    with ExitStack() as esa2:
                        mixT = T(esa2, nc, "mixT", [128, KC, TB], BF16)
                        mraw = [T(esa2, nc, f"mraw{i}", [128, KMIX], F32) for i in range(2)]
                        sta = T(esa2, nc, "sta", [128, 8], F32)
                        sqa = T(esa2, nc, "sqa", [128, 2048], F32)
                        tmp = T(esa2, nc, "tmpa", [128, 512], F32)
                        wob = [T(esa2, nc, f"wob{i}", [128, KC, 512], BF16) for i in range(2)]
                        mrB = T(esa2, nc, "mrB", [128, KMIX], F32) if blend else None
                        for tt in range(NTT):
                            mr = mraw[tt % 2]
                            P.dma("sp", mr[:, :], mix_d[t0 + tt * 128:t0 + (tt + 1) * 128, :], w=[mr.r])
                            if blend:
                                P.dma("sp", mrB[:, :], mix_d[NT + t0 + tt * 128:NT + t0 + (tt + 1) * 128, :], w=[mrB.r])
                                h.ts("dve", mr[:, :], mr[:, :], sel[:, 0:1], None, ALU.mult, None, [mr.r, sel.r], [mr.r])
                                h.stt(mr[:, :], mrB[:, :], sel[:, 1:2], mr[:, :], ALU.mult, ALU.add, [mrB.r, sel.r, mr.r], [mr.r])
                            if ya_norm:
                                rms_stats(P, h, mr[:, 0:2048], sqa[:, :], sta[:, 0:1], sta[:, 1:2], [mr.r], sqa.r, sta.r, 2048)
                                h.ts("dve", mr[:, 0:2048], mr[:, 0:2048], sta[:, 1:2], None, ALU.mult, None, [mr.r, sta.r], [mr.r])

                            def evac(q, pb, tt=tt):
                                dst = mixT[:, q * 4:(q + 1) * 4, tt * 128:(tt + 1) * 128]
                                src = pb[:, :].rearrange("p (a b) -> p a b", a=4)
                                if ya_norm and q < 4:
                                    h.tt("dve", dst, src, gya[:, q * 4:(q + 1) * 4].unsqueeze(2).to_broadcast([128, 4, 128]),
                                         ALU.mult, [pb.r, gya.r], [mixT.r])
                                elif q % 2 == 0:
                                    h.cp("dve", dst, src, [pb.r], [mixT.r])
                                else:
                                    h.cp("act", dst, src, [pb.r], [mixT.r])
                            to_fm(P, h, ps, ident, mr, mr.r, KC, mixT, tt, evac)
                        for db in range(4):
                            wb = wob[db % 2]
                            P.dma("pool", wb[:, :, :], wo_d[:, db * 512:(db + 1) * 512].rearrange("(k p) n -> p k n", p=128), w=[wb.r])
                            for tt in range(NTT):
                                pb = ps[4 + (tt % 2)]
                                for kc in range(KC):
                                    h.mm(pb[:, :], mixT[:, kc, tt * 128:(tt + 1) * 128], wb[:, kc, :], kc == 0, kc == KC - 1,
                                         [mixT.r, wb.r], [pb.r])
                                h.tt("dve", tmp[:, :], pb[:, :], g1[:, db * 512:(db + 1) * 512], ALU.mult, [pb.r, g1.r], [tmp.r])
                                h.tt("dve", xb[:, tt, db * 512:(db + 1) * 512], xb[:, tt, db * 512:(db + 1) * 512], tmp[:, :], ALU.add,
                                     [tmp.r, xb.r], [xb.r])
                    P.barrier()
                    P.dma("sp", oblk, xb[:, :, :], r=[xb.r], w=[ores])
                    with ExitStack() as esn:
                        sq = T(esn, nc, "sq", [128, D], F32)
                        st = T(esn, nc, "st", [128, 8], F32)
                        for tt in range(NTT):
                            rms_stats(P, h, xb[:, tt, :], sq[:, :], st[:, 0:1], st[:, 1:2], [xb.r], sq.r, st.r, D)
                            h.ts("dve", sq[:, :], xb[:, tt, :], st[:, 1:2], None, ALU.mult, None, [xb.r, st.r], [sq.r])

                            def evac(q, pb, tt=tt):
                                for j in range(4):
                                    kc = q * 4 + j
                                    h.act(hT[:, kc, tt * 128:(tt + 1) * 128], pb[:, j * 128:(j + 1) * 128], AF.Identity,
                                          [pb.r, gs2.r, sh2.r], [hT.r], bias=sh2[:, kc:kc + 1], scale=gs2[:, kc:kc + 1])
                            to_fm(P, h, ps, ident, sq, sq.r, 16, hT, tt, evac)
                    P.barrier()
                s2 = T(esb, nc, "s2", [128, NJ, 128], F32)
                theta = T(esb, nc, "theta", [128, NJ, 128], F32)
                e1 = T(esb, nc, "e1", [128, NJ, 128], BF16)
                e2 = T(esb, nc, "e2", [128, NJ, 128], BF16)
                with ExitStack() as esc:
                    qT = T(esc, nc, "qT", [128, 16, TB], F32)
                    wqb = [T(esc, nc, f"wqb{i}", [128, 16, 128], BF16) for i in range(2)]
                    sc = [T(esc, nc, f"sc{i}", [128, 2048], F32) for i in range(2)]
                    for oc in range(16):
                        wb = wqb[oc % 2]
                        P.dma("pool", wb[:, :, :], wq_d[:, oc * 128:(oc + 1) * 128].rearrange("(k p) n -> p k n", p=128), w=[wb.r])
                        pb = ps[oc % 2]
                        for k in range(16):
                            h.mm(pb[:, :], wb[:, k, :], hT[:, k, :], k == 0, k == 15, [wb.r, hT.r], [pb.r])
                        h.cp("act" if oc % 2 else "dve", qT[:, oc, :], pb[:, :], [pb.r], [qT.r])
                    for tt in range(NTT):
                        s_ = sc[tt % 2]
                        for q4 in range(4):
                            pb = ps[2 + q4 % 2]
                            for j in range(4):
                                hi = q4 * 4 + j
                                h.mm(pb[:, j * 128:(j + 1) * 128], qT[:, hi, tt * 128:(tt + 1) * 128], keysT[:, hi, :], True, True,
                                     [qT.r, keysT.r], [pb.r])
                            h.cp("act" if q4 % 2 else "dve", s_[:, q4 * 512:(q4 + 1) * 512], pb[:, :], [pb.r], [s_.r])
                        P.dma("sp", sc_d[tt * 128:(tt + 1) * 128, :], s_[:, :], r=[s_.r], w=[scd_res])
                P.barrier()
                with ExitStack() as esk:
                    S = T(esk, nc, "S", [128, NJ, 256], F32)
                    P.dma("sp", S[:, :, :], sc_d.rearrange("t (h x) -> (t h) x", h=8).rearrange("(j p) x -> p j x", p=128),
                          r=[scd_res], w=[S.r])
                    A16 = T(esk, nc, "A16", [128, NJ, 16], F32)
                    B16 = T(esk, nc, "B16", [128, NJ, 16], F32)
                    C24 = T(esk, nc, "C24", [128, NJ, 24], F32)
                    wk = T(esk, nc, "wk", [128, 128], F32)
                    cand = T(esk, nc, "cand", [128, 256], F32)
                    cw = T(esk, nc, "cw", [128, 256], F32)
                    cw2 = T(esk, nc, "cw2", [128, 256], F32)
                    sm_ = T(esk, nc, "smalls", [128, 4, NJ], F32)
                    ce = T(esk, nc, "ce", [128, NJ, 16], F32)
                    tmpf = T(esk, nc, "tmpf", [128, NJ, 128], F32)
                    wkA = [T(esk, nc, f"wkA{i}", [128, 128], F32) for i in range(2)]
                    wkB = [T(esk, nc, f"wkB{i}", [128, 128], F32) for i in range(2)]
                    cands = [cand, T(esk, nc, "cand1", [128, 256], F32)]
                    cws = [cw, T(esk, nc, "cw1", [128, 256], F32)]
                    cw2s = [cw2, T(esk, nc, "cw21", [128, 256], F32)]
                    Ar = [Res(f"A16_{j}") for j in range(NJ)]
                    Br = [Res(f"B16_{j}") for j in range(NJ)]
                    Cr = [Res(f"C24_{j}") for j in range(NJ)]

                    def ab_ops(j):
                        sa = S[:, j, 0:128]
                        sb_ = S[:, j, 128:256]
                        wa, wb_ = wkA[j % 2], wkB[j % 2]
                        return [
                            lambda: h.max8(A16[:, j, 0:8], sa, [S.r], [Ar[j]]),
                            lambda: h.max8(B16[:, j, 0:8], sb_, [S.r], [Br[j]]),
                            lambda: h.mrep(wa[:, :], A16[:, j, 0:8], sa, [S.r, Ar[j]], [wa.r]),
                            lambda: h.mrep(wb_[:, :], B16[:, j, 0:8], sb_, [S.r, Br[j]], [wb_.r]),
                            lambda: h.max8(A16[:, j, 8:16], wa[:, :], [wa.r], [Ar[j]]),
                            lambda: h.max8(B16[:, j, 8:16], wb_[:, :], [wb_.r], [Br[j]]),
                        ]

                    def cc_ops(j):
                        cd_, c1, c2 = cands[j % 2], cws[j % 2], cw2s[j % 2]
                        return [
                            lambda: h.tt("dve", cd_[:, :].rearrange("p (a b) -> p a b", a=16),
                                         A16[:, j, :].unsqueeze(2).to_broadcast([128, 16, 16]),
                                         B16[:, j, :].unsqueeze(1).to_broadcast([128, 16, 16]), ALU.add, [Ar[j], Br[j]], [cd_.r]),
                            lambda: h.max8(C24[:, j, 0:8], cd_[:, :], [cd_.r], [Cr[j]]),
                            lambda: h.mrep(c1[:, :], C24[:, j, 0:8], cd_[:, :], [cd_.r, Cr[j]], [c1.r]),
                            lambda: h.max8(C24[:, j, 8:16], c1[:, :], [c1.r], [Cr[j]]),
                            lambda: h.mrep(c2[:, :], C24[:, j, 8:16], c1[:, :], [c1.r, Cr[j]], [c2.r]),
                            lambda: h.max8(C24[:, j, 16:24], c2[:, :], [c2.r], [Cr[j]]),
                        ]

                    for op in ab_ops(0):
                        op()
                    for j in range(NJ):
                        cc = cc_ops(j)
                        ab = ab_ops(j + 1) if j + 1 < NJ else []
                        for k in range(6):
                            cc[k]()
                            if k < len(ab):
                                ab[k]()
                    P.barrier()
                    mt = sm_[:, 0, :]
                    thr = sm_[:, 1, :]
                    zz = sm_[:, 2, :]
                    h.tt("dve", mt, A16[:, :, 0], B16[:, :, 0], ALU.add, [A16.r, B16.r], [sm_.r])
                    h.tt("dve", thr, C24[:, :, 15], C24[:, :, 16], ALU.add, [C24.r], [sm_.r])
                    h.ts("dve", thr, thr, 0.5, None, ALU.mult, None, [sm_.r], [sm_.r])
                    h.tt("dve", ce[:, :, :], C24[:, :, 0:16], mt.unsqueeze(2).to_broadcast([128, NJ, 16]), ALU.subtract,
                         [C24.r, sm_.r], [ce.r])
                    h.act(ce[:, :, :], ce[:, :, :], AF.Exp, [ce.r], [ce.r])
                    h.red(zz, ce[:, :, :], ALU.add, [ce.r], [sm_.r])
                    h.recip(zz, zz, [sm_.r], [sm_.r])
                    h.tt("dve", theta[:, :, :], thr.unsqueeze(2).to_broadcast([128, NJ, 128]), S[:, :, 0:128], ALU.subtract,
                         [sm_.r, S.r], [theta.r])
                    h.tt("dve", tmpf[:, :, :], S[:, :, 0:128], A16[:, :, 0:1].to_broadcast([128, NJ, 128]), ALU.subtract,
                         [S.r, A16.r], [tmpf.r])
                    h.act(e1[:, :, :], tmpf[:, :, :], AF.Exp, [tmpf.r], [e1.r])
                    h.tt("dve", tmpf[:, :, :], S[:, :, 128:256], B16[:, :, 0:1].to_broadcast([128, NJ, 128]), ALU.subtract,
                         [S.r, B16.r], [tmpf.r])
                    h.act(tmpf[:, :, :], tmpf[:, :, :], AF.Exp, [tmpf.r], [tmpf.r])
                    h.tt("dve", e2[:, :, :], tmpf[:, :, :], zz.unsqueeze(2).to_broadcast([128, NJ, 128]), ALU.mult,
                         [tmpf.r, sm_.r], [e2.r])
                    h.cp("dve", s2[:, :, :], S[:, :, 128:256], [S.r], [s2.r])
                P.barrier()
                acc = T(esb, nc, "acc", [128, NTT, D], F32)
                h.memset("pool", acc[:, :, :], 0.0, [acc.r])
                with ExitStack() as esl:
                    UT = [T(esl, nc, f"UT{i}", [128, 16, 128], BF16) for i in range(3)]
                    Vg = [T(esl, nc, f"Vg{i}", [128, GV, D], BF16) for i in range(2)]
                    GA = [T(esl, nc, f"GA{i}", [128, TB], BF16) for i in range(2)]
                    AG = [T(esl, nc, f"AG{i}", [128, GV, TB], BF16) for i in range(2)]
                    mks = [T(esl, nc, f"mk{i}", [128, NJ, 128], BF16) for i in range(2)]
                    mkr = [[Res(f"mkr{i}_{q}") for q in range(8)] for i in range(2)]
                    ghr = [[Res(f"ghr{i}_{q}") for q in range(8)] for i in range(2)]
                    Gh = [T(esl, nc, f"Gh{i}", [128, NJ, 128], BF16) for i in range(2)]
                    sums = [T(esl, nc, f"sums{i}", [128, NJ, 16], BF16) for i in range(2)]
                    def load_v(g):
                        vg = Vg[g % 2]
                        P.dma("sp", vg[:, :, :], Vb_d[g * GV * 128:(g + 1) * GV * 128, :].rearrange("(c p) d -> p c d", p=128), w=[vg.r])

                    NPC = 8
                    JP = NJ // NPC

                    def pre1(i1):
                        ut = UT[i1 % 3]
                        P.dma("sp", ut[:, :, :], UT_d[i1].rearrange("p (k e) -> p k e", k=16), w=[ut.r])
                        pa = ps[2 + i1 % 2]
                        for k in range(16):
                            h.mm(pa[:, :], ut[:, k, :], hT[:, k, :], k == 0, k == 15, [ut.r, hT.r], [pa.r])
                        ga = GA[i1 % 2]
                        h.act(ga[:, :], pa[:, :], AF.Gelu, [pa.r], [ga.r])

                    def piece1(i1, q):
                        gh = Gh[i1 % 2]
                        mk = mks[i1 % 2]
                        js = slice(q * JP, (q + 1) * JP)
                        h.tt("dve", mk[:, js, :], s2[:, js, :], theta[:, js, i1:i1 + 1].to_broadcast([128, JP, 128]), ALU.is_ge,
                             [s2.r, theta.r], [mkr[i1 % 2][q]])
                        h.tt("dve" if q == NPC - 1 else "pool", gh[:, js, :], mk[:, js, :], e2[:, js, :], ALU.mult,
                             [mkr[i1 % 2][q], e2.r], [ghr[i1 % 2][q]])

                    def post1(i1):
                        sm = sums[i1 % 2]
                        h.tt("pool", sm[:, :, :], summ[:, :].unsqueeze(1).to_broadcast([128, NJ, 16]),
                             e1[:, :, i1:i1 + 1].to_broadcast([128, NJ, 16]), ALU.mult, [summ.r, e1.r], [sm.r])

                    def stage2(i1):
                        g, c = divmod(i1, GV)
                        ag = AG[g % 2]
                        ga = GA[i1 % 2]
                        gh = Gh[i1 % 2]
                        sm = sums[i1 % 2]
                        pg = ps[4 + i1 % 2]
                        for j in range(NJ):
                            h.mm(pg[:, j * 16:(j + 1) * 16], gh[:, j, :], sm[:, j, :], True, True, [ghr[i1 % 2][j // JP], sm.r], [pg.r])
                        h.tt("dve", ag[:, c, :], ga[:, :], pg[:, :], ALU.mult, [ga.r, pg.r], [ag.r])

                    def unit3(g, u):
                        vg = Vg[g % 2]
                        ag = AG[g % 2]
                        tt, db = divmod(u, 4)
                        po = [ps[6], ps[7], ps[0], ps[1]][u % 4]
                        for c in range(GV):
                            h.mm(po[:, :], ag[:, c, tt * 128:(tt + 1) * 128], vg[:, c, db * 512:(db + 1) * 512], c == 0, c == GV - 1,
                                 [ag.r, vg.r], [po.r])
                        h.tt("dve", acc[:, tt, db * 512:(db + 1) * 512], acc[:, tt, db * 512:(db + 1) * 512], po[:, :], ALU.add,
                             [po.r, acc.r], [acc.r])

                    NU = NTT * 4
                    UPC = NU // GV
                    load_v(0)
                    pre1(0)
                    for q in range(NPC):
                        piece1(0, q)
                    post1(0)
                    for i1 in range(n_i1):
                        g, c = divmod(i1, GV)
                        nxt = i1 + 1 < n_i1
                        if nxt:
                            pre1(i1 + 1)
                        for q in range(NPC):
                            if nxt:
                                piece1(i1 + 1, q)
                            if g > 0:
                                for u in range(c * UPC + q * UPC // NPC, c * UPC + (q + 1) * UPC // NPC):
                                    unit3(g - 1, u)
                        if nxt:
                            post1(i1 + 1)
                        stage2(i1)
                        if c == GV - 1 and (g + 1) * GV < n_i1:
                            load_v(g + 1)
                    for u in range(NU):
                        unit3(n_i1 // GV - 1, u)
                P.barrier()
                with ExitStack() as esf:
                    xb = T(esf, nc, "xbf", [128, NTT, D], F32)
                    g2 = T(esf, nc, "g2", [128, D], F32)
                    fg = T(esf, nc, "fg", [128, D], F32)
                    sq = T(esf, nc, "sqf", [128, D], F32)
                    st = T(esf, nc, "stf", [128, 8], F32)
                    P.dma("sp", xb[:, :, :], oblk, r=[ores], w=[xb.r])
                    P.dma("sp", g2[:, :], bc_d[1], r=[bcd_res], w=[g2.r])
                    if final:
                        P.dma("sp", fg[:, :], fing_d[0:1, :].partition_broadcast(128), w=[fg.r])
                    for tt in range(NTT):
                        h.tt("dve", acc[:, tt, :], acc[:, tt, :], g2[:, :], ALU.mult, [acc.r, g2.r], [acc.r])
                        h.tt("pool", xb[:, tt, :], xb[:, tt, :], acc[:, tt, :], ALU.add, [acc.r, xb.r], [xb.r])
                        if final:
                            rms_stats(P, h, xb[:, tt, :], sq[:, :], st[:, 0:1], st[:, 1:2], [xb.r], sq.r, st.r, D)
                            h.ts("dve", xb[:, tt, :], xb[:, tt, :], st[:, 1:2], None, ALU.mult, None, [xb.r, st.r], [xb.r])
                            h.tt("pool", xb[:, tt, :], xb[:, tt, :], fg[:, :], ALU.mult, [xb.r, fg.r], [xb.r])
                    tk = P.dma("sp", oblk, xb[:, :, :], r=[xb.r], w=[ores])
                    P.wait("sp", [tk])
                P.barrier()
        if ctx is None:
            P.emit()
        print("tail instrs", P.ninstr, {e: len(P.q[e]) for e in P.ENGS})
    return nc


def build_attn(S_LEN=4096, NH=8, HG=4, ctx=None, env=None, pfx="", tokmajor=False):
    from contextlib import ExitStack
    nc = ctx.nc if ctx else bass.Bass("TRN2", target_bir_lowering=False)
    NBK = S_LEN // TB
    NKT = S_LEN // 128
    din, dout = _io(nc, env, pfx)

    x_d = din("x", [S_LEN, D])
    cfm_d = din("c_fm", [128, 16])
    adaw_d = din("ada_w", [D, 6 * D])
    adabfm_d = din("ada_b_fm", [128, 96])
    adab_d = din("ada_b", [1, 6 * D])
    gmix_d = din("g_mix_fm", [128, 16])
    wqkv_d = din("wqkv", [D, 3 * NH * 128])
    ident_d = din("ident", [128, 128])
    masks_d = din("masks", [128, 4, 512])
    ntri_d = din("ntri", [128, 128])
    if tokmajor:
        o_d = dout("o", [S_LEN, NH * 128])
    else:
        oT_d = dout("oT", [NH * 128, S_LEN])
    scale = 128.0 ** -0.5

    with ExitStack() as es:
        if ctx is None:
            P = Prog(nc)
            h = H(P)
            ps = [T(es, nc, f"ps{i}", [128, 512], F32, psum=True) for i in range(8)]
        else:
            P, h, ps = ctx.P, ctx.h, ctx.ps
        ident = T(es, nc, "ident", [128, 128], F32)
        masks = T(es, nc, "masks", [128, 4, 512], F32)
        ntri = T(es, nc, "ntri", [128, 128], F32)
        nones = T(es, nc, "nones", [128, 128], F32)
        gmix = T(es, nc, "gmix", [128, 16], F32)
        gs1 = T(es, nc, "gs1", [128, 16], F32)
        sh1 = T(es, nc, "sh1", [128, 16], F32)
        P.dma("sp", ident[:, :], ident_d, w=[ident.r])
        P.dma("sp", masks[:, :, :], masks_d, w=[masks.r])
        P.dma("sp", ntri[:, :], ntri_d, w=[ntri.r])
        P.dma("sp", gmix[:, :], gmix_d, w=[gmix.r])
        h.memset("pool", nones[:, :], -1.0, [nones.r])
        ntrib = T(es, nc, "ntrib", [128, 128], BF16)
        nonesb = T(es, nc, "nonesb", [128, 128], BF16)
        h.cp("dve", ntrib[:, :], ntri[:, :], [ntri.r], [ntrib.r])
        h.memset("pool", nonesb[:, :], -1.0, [nonesb.r])
        with ExitStack() as es1:
            ada = ada_compute(es1, nc, P, h, ps, cfm_d, adaw_d, adabfm_d, adab_d, fm_secs=[0, 1], bc_secs=[], tag="a")
            h.cp("dve", sh1[:, :], ada[0][:, :], [ada[0].r], [sh1.r])
            h.stt(gs1[:, :], ada[1][:, :], 1.0, gmix[:, :], ALU.add, ALU.mult, [ada[1].r, gmix.r], [gs1.r])
            P.barrier()
        out_toks = []
        for hg in range(NH // HG):
            with ExitStack() as esg:
                QT = T(esg, nc, "QT", [128, HG, S_LEN], BF16)
                KT = T(esg, nc, "KT", [128, HG, S_LEN], BF16)
                Vt = T(esg, nc, "Vt", [128, NKT, HG * 128], BF16)
                with ExitStack() as e1:
                    xb = T(e1, nc, "xb", [128, NTT, D], F32)
                    hT = T(e1, nc, "hT", [128, 16, TB], BF16)
                    sq = T(e1, nc, "sq", [128, D], F32)
                    st = T(e1, nc, "st", [128, 8], F32)
                    wp = [T(e1, nc, f"wp{i}", [128, 16, 128], BF16) for i in range(2)]
                    wv = T(e1, nc, "wv", [128, 16, HG * 128], BF16)
                    c0v = 2 * NH * 128 + hg * HG * 128
                    P.dma("pool", wv[:, :, :], wqkv_d[:, c0v:c0v + HG * 128].rearrange("(k p) n -> p k n", p=128), w=[wv.r])
                    wi = 0
                    for b in range(NBK):
                        t0 = b * TB
                        P.dma("sp", xb[:, :, :], x_d[t0:t0 + TB, :].rearrange("(t p) d -> p t d", p=128), w=[xb.r])
                        for tt in range(NTT):
                            rms_stats(P, h, xb[:, tt, :], sq[:, :], st[:, 0:1], st[:, 1:2], [xb.r], sq.r, st.r, D)
                            h.ts("dve", sq[:, :], xb[:, tt, :], st[:, 1:2], None, ALU.mult, None, [xb.r, st.r], [sq.r])

                            def evac(q, pb, tt=tt):
                                for j in range(4):
                                    kc = q * 4 + j
                                    h.act(hT[:, kc, tt * 128:(tt + 1) * 128], pb[:, j * 128:(j + 1) * 128], AF.Identity,
                                          [pb.r, gs1.r, sh1.r], [hT.r], bias=sh1[:, kc:kc + 1], scale=gs1[:, kc:kc + 1])
                            to_fm(P, h, ps, ident, sq, sq.r, 16, hT, tt, evac)
                        for hl in range(HG):
                            for which, dst in ((0, QT), (1, KT)):
                                wb = wp[wi % 2]
                                wi += 1
                                c0 = which * NH * 128 + (hg * HG + hl) * 128
                                P.dma("pool", wb[:, :, :], wqkv_d[:, c0:c0 + 128].rearrange("(k p) n -> p k n", p=128), w=[wb.r])
                                pb = ps[2 + wi % 2]
                                for k in range(16):
                                    h.mm(pb[:, :], wb[:, k, :], hT[:, k, :], k == 0, k == 15, [wb.r, hT.r], [pb.r])
                                if which == 0:
                                    h.act(dst[:, hl, t0:t0 + TB], pb[:, :], AF.Copy, [pb.r], [dst.r], scale=scale)
                                else:
                                    h.cp("dve", dst[:, hl, t0:t0 + TB], pb[:, :], [pb.r], [dst.r])
                        for tt in range(NTT):
                            pb = ps[4 + tt % 2]
                            for k in range(16):
                                h.mm(pb[:, 0:HG * 128], hT[:, k, tt * 128:(tt + 1) * 128], wv[:, k, :], k == 0, k == 15, [hT.r, wv.r], [pb.r])
                            h.cp("dve" if tt % 2 else "act", Vt[:, b * NTT + tt, :], pb[:, 0:HG * 128], [pb.r], [Vt.r])
                P.barrier()
                with ExitStack() as e2:
                    ex = [T(e2, nc, f"ex{i}", [128, 512], F32) for i in range(2)]
                    spb = [T(e2, nc, f"sp{i}", [128, 512], F32) for i in range(3)]
                    shi = [T(e2, nc, f"shi{i}", [128, 512], BF16) for i in range(4)]
                    slo = [T(e2, nc, f"slo{i}", [128, 512], BF16) for i in range(4)]
                    Rsb = [T(e2, nc, f"Rsb{i}", [128, 512], F32) for i in range(2)]
                    tmpx = [T(e2, nc, f"tmpx{i}", [128, 512], F32) for i in range(2)]
                    Wt = [T(e2, nc, f"Wt{i}", [128, 512], BF16) for i in range(3)]
                    osb = [T(e2, nc, f"osb{i}", [128, 512], F32) for i in range(2)]
                    osT = [T(e2, nc, f"osT{i}", [128, 512], F32) for i in range(2)]
                    tiles = [(hl, qb, idx) for hl in range(HG) for qb in range(NBK) for idx in range(4 * qb + 4)]

                    def geom(tile):
                        hl, qb, idx = tile
                        nk = 4 * qb + 4
                        kt = nk - 1 - idx
                        return hl, qb, idx, nk, kt, kt - 4 * qb

                    def S0(tile, it):
                        hl, qb, idx, nk, kt, jd = geom(tile)
                        pL = ps[it % 2]
                        h.mm(pL[:, :], KT[:, hl, kt * 128:(kt + 1) * 128], QT[:, hl, qb * 512:(qb + 1) * 512], True, True, [KT.r, QT.r], [pL.r])

                    def S1(tile, it):
                        pL = ps[it % 2]
                        e_ = ex[it % 2]
                        s_ = spb[it % 3]
                        h.act(e_[:, :], pL[:, :], AF.Exp, [pL.r], [e_.r])
                        h.act(s_[:, :], e_[:, :], AF.Ln, [e_.r], [s_.r], bias=1.0)

                    def S2(tile, it):
                        hl, qb, idx, nk, kt, jd = geom(tile)
                        s_ = spb[it % 3]
                        if jd >= 0:
                            h.tt("dve", s_[:, :], s_[:, :], masks[:, jd, :], ALU.mult, [s_.r, masks.r], [s_.r])
                        h.cp("dve", shi[it % 4][:, :], s_[:, :], [s_.r], [shi[it % 4].r])
                        h.tt("pool", slo[it % 4][:, :], s_[:, :], shi[it % 4][:, :], ALU.subtract, [s_.r, shi[it % 4].r], [slo[it % 4].r])

                    def S3(tile, it):
                        hl, qb, idx, nk, kt, jd = geom(tile)
                        pE = ps[2 + it % 2]
                        pR = ps[4 + qb % 2]
                        hi_, lo_ = shi[it % 4], slo[it % 4]
                        h.mm(pE[:, :], KT[:, hl, kt * 128:(kt + 1) * 128], QT[:, hl, qb * 512:(qb + 1) * 512], True, False, [KT.r, QT.r], [pE.r])
                        h.mm(pE[:, :], ntrib[:, :], hi_[:, :], False, False, [ntrib.r, hi_.r], [pE.r])
                        h.mm(pE[:, :], ntrib[:, :], lo_[:, :], False, True, [ntrib.r, lo_.r], [pE.r])
                        if idx > 0:
                            h.cp("act", Rsb[it % 2][:, :], pR[:, :], [pR.r], [Rsb[it % 2].r])

                    def S4r(tile, it):
                        hl, qb, idx, nk, kt, jd = geom(tile)
                        pR = ps[4 + qb % 2]
                        hi_, lo_ = shi[it % 4], slo[it % 4]
                        if idx < nk - 1:
                            h.mm(pR[:, :], nonesb[:, :], hi_[:, :], idx == 0, False, [nonesb.r, hi_.r], [pR.r])
                            h.mm(pR[:, :], nonesb[:, :], lo_[:, :], False, idx == nk - 2, [nonesb.r, lo_.r], [pR.r])

                    def S4(tile, it):
                        hl, qb, idx, nk, kt, jd = geom(tile)
                        pE = ps[2 + it % 2]
                        if idx > 0:
                            h.tt("dve", tmpx[it % 2][:, :], pE[:, :], Rsb[it % 2][:, :], ALU.add, [pE.r, Rsb[it % 2].r], [tmpx[it % 2].r])
                        else:
                            h.cp("dve", tmpx[it % 2][:, :], pE[:, :], [pE.r], [tmpx[it % 2].r])

                    def S5(tile, it):
                        w_ = Wt[it % 3]
                        h.act(w_[:, :], tmpx[it % 2][:, :], AF.Exp, [tmpx[it % 2].r], [w_.r])

                    def S6(tile, it):
                        hl, qb, idx, nk, kt, jd = geom(tile)
                        po = ps[6 + qb % 2]
                        w_ = Wt[it % 3]
                        if jd >= 0:
                            h.tt("dve", w_[:, :], w_[:, :], masks[:, jd, :], ALU.mult, [w_.r, masks.r], [w_.r])
                        h.mm(po[:, :], Vt[:, kt, hl * 128:(hl + 1) * 128], w_[:, :], idx == 0, idx == nk - 1, [Vt.r, w_.r], [po.r])
                        if idx == nk - 1:
                            ob = osb[qb % 2]
                            h.cp("dve", ob[:, :], po[:, :], [po.r], [ob.r])
                            hh = hg * HG + hl
                            if tokmajor:
                                pt = po
                                for tt in range(4):
                                    h.tr(pt[:, tt * 128:(tt + 1) * 128], ob[:, tt * 128:(tt + 1) * 128], ident[:, :], [ob.r, ident.r], [pt.r])
                                ot = osT[qb % 2]
                                h.cp("act", ot[:, :], pt[:, :], [pt.r], [ot.r])
                                out_toks.append(P.dma("sp", o_d[qb * 512:(qb + 1) * 512, hh * 128:(hh + 1) * 128].rearrange("(t p) d -> p t d", p=128),
                                                      ot[:, :].rearrange("p (t d) -> p t d", t=4), r=[ot.r]))
                            else:
                                out_toks.append(P.dma("sp", oT_d[hh * 128:(hh + 1) * 128, qb * 512:(qb + 1) * 512], ob[:, :], r=[ob.r]))

                    sched = [(S0, 0), (S1, 1), (S2, 2), (S4r, 4), (S3, 3), (S4, 4), (S5, 5), (S6, 6)]
                    n_t = len(tiles)
                    for n in range(n_t + 6):
                        for fn, off in sched:
                            t = n - off
                            if 0 <= t < n_t:
                                fn(tiles[t], t)
                P.barrier()
        P.wait("sp", out_toks[-48:])
        if ctx is None:
            P.emit()
        print("attn instrs", P.ninstr, {e: len(P.q[e]) for e in P.ENGS})
    return nc


def build_mix0(S_LEN=4096, NHD=16, gm_nblk=4, ctx=None, env=None, pfx="", ygcol=0, ygw=None):
    from contextlib import ExitStack
    nc = ctx.nc if ctx else bass.Bass("TRN2", target_bir_lowering=False)
    din, dout = _io(nc, env, pfx)
    NBK = S_LEN // TB
    NG = NHD // 4
    NX = NHD * 64
    NXC = NX // 128
    NCH = NXC + 2 * NG
    WSSD = 2 * NX + 2 * NG * 128 + NHD

    x_d = din("x", [S_LEN, D])
    xgm_d = din("x_gm", [gm_nblk * TB, D]) if gm_nblk else None
    cfm_d = din("c_fm", [128, 16])
    adaw_d = din("ada_w", [D, 6 * D])
    adabfm_d = din("ada_b_fm", [128, 96])
    adab_d = din("ada_b", [1, 6 * D])
    gmix_d = din("g_mix_fm", [128, 16])
    wssd_d = din("w_ssd", [D, WSSD])
    convw_d = din("conv_w_fm", [128, NCH, 4])
    convb_d = din("conv_b_fm", [128, NCH])
    dtb_d = din("dt_bias", [1, NHD])
    alog_d = din("a_log", [1, NHD])
    dsk_d = din("d_skip", [1, NHD])
    wuv_d = din("w_uv", [D, 4096])
    lng_d = din("ln_g", [1, 2048])
    lnb_d = din("ln_b", [1, 2048])
    wsT_d = din("wsT", [128, 16, 128])
    bsT_d = din("bsT", [128, 16])
    ident_d = din("ident", [128, 128])
    ut_d = din("ut", [128, 128])
    slt_d = din("slt", [128, 128])
    yg_d = dout("yg", [S_LEN, NX])
    yb_d = dout("yb", [gm_nblk * TB, 2048]) if gm_nblk else None

    with ExitStack() as es:
        if ctx is None:
            P = Prog(nc)
            h = H(P)
            ps = [T(es, nc, f"ps{i}", [128, 512], F32, psum=True) for i in range(8)]
        else:
            P, h, ps = ctx.P, ctx.h, ctx.ps

        def cst(name, shape, src, dt=F32):
            t = T(es, nc, name, shape, dt)
            P.dma("sp", t[tuple(slice(None) for _ in shape)], src, w=[t.r])
            return t
        ident = cst("ident", [128, 128], ident_d)
        ut = cst("ut", [128, 128], ut_d)
        slt = cst("slt", [128, 128], slt_d)
        gmix = cst("gmix", [128, 16], gmix_d)
        convw = cst("convw", [128, NCH, 4], convw_d)
        convb = cst("convb", [128, NCH], convb_d)
        dtb = cst("dtb", [128, NHD], dtb_d[0:1, :].partition_broadcast(128))
        aneg = cst("aneg", [128, NHD], alog_d[0:1, :].partition_broadcast(128))
        dsk = cst("dsk", [128, NHD], dsk_d[0:1, :].partition_broadcast(128))
        bsT = cst("bsT", [128, 16], bsT_d)
        wsT = T(es, nc, "wsT", [128, 16, 128], BF16)
        ones = T(es, nc, "ones", [128, 128], F32)
        gs1 = T(es, nc, "gs1", [128, 16], F32)
        sh1 = T(es, nc, "sh1", [128, 16], F32)
        halo = T(es, nc, "halo", [128, NCH, 3], F32)
        Hs = T(es, nc, "Hs", [128, NX], F32)
        Hb = T(es, nc, "Hb", [128, NX], BF16)
        h.memset("pool", ones[:, :], 1.0, [ones.r])
        h.memset("pool", halo[:, :, :], 0.0, [halo.r])
        h.memset("pool", Hs[:, :], 0.0, [Hs.r])
        h.memset("pool", Hb[:, :], 0.0, [Hb.r])
        h.act(aneg[:, :], aneg[:, :], AF.Exp, [aneg.r], [aneg.r])
        h.ts("dve", aneg[:, :], aneg[:, :], -1.0, None, ALU.mult, None, [aneg.r], [aneg.r])
        with ExitStack() as es1:
            wraw = T(es1, nc, "wsraw", [128, 16, 128], F32)
            P.dma("sp", wraw[:, :, :], wsT_d, w=[wraw.r])
            h.tt("dve", wsT[:, :, :], wraw[:, :, :], ut[:, :].unsqueeze(1).to_broadcast([128, 16, 128]), ALU.mult,
                 [wraw.r, ut.r], [wsT.r])
            ada = ada_compute(es1, nc, P, h, ps, cfm_d, adaw_d, adabfm_d, adab_d, fm_secs=[0, 1], bc_secs=[], tag="m")
            h.cp("dve", sh1[:, :], ada[0][:, :], [ada[0].r], [sh1.r])
            h.stt(gs1[:, :], ada[1][:, :], 1.0, gmix[:, :], ALU.add, ALU.mult, [ada[1].r, gmix.r], [gs1.r])
            P.barrier()
        out_toks = []
        if gm_nblk == NBK and env is not None and env.get("x_gm") is x_d:
            blocks = [("both", i) for i in range(NBK)]
        else:
            blocks = [("ssd", i) for i in range(NBK)] + [("gm", i) for i in range(gm_nblk)]
        for kind, b in blocks:
            t0 = b * TB
            xsrc = xgm_d if kind == "gm" else x_d
            with ExitStack() as esb:
                hT = T(esb, nc, "hT", [128, 16, TB], BF16)
                with ExitStack() as e1:
                    xb = T(e1, nc, "xb", [128, NTT, D], F32)
                    sq = T(e1, nc, "sq", [128, D], F32)
                    st = T(e1, nc, "st", [128, 8], F32)
                    P.dma("sp", xb[:, :, :], xsrc[t0:t0 + TB, :].rearrange("(t p) d -> p t d", p=128), w=[xb.r])
                    for tt in range(NTT):
                        rms_stats(P, h, xb[:, tt, :], sq[:, :], st[:, 0:1], st[:, 1:2], [xb.r], sq.r, st.r, D)
                        h.ts("dve", sq[:, :], xb[:, tt, :], st[:, 1:2], None, ALU.mult, None, [xb.r, st.r], [sq.r])

                        def evac(q, pb, tt=tt):
                            for j in range(4):
                                kc = q * 4 + j
                                h.act(hT[:, kc, tt * 128:(tt + 1) * 128], pb[:, j * 128:(j + 1) * 128], AF.Identity,
                                      [pb.r, gs1.r, sh1.r], [hT.r], bias=sh1[:, kc:kc + 1], scale=gs1[:, kc:kc + 1])
                        to_fm(P, h, ps, ident, sq, sq.r, 16, hT, tt, evac)
                    P.barrier()
                if kind in ("ssd", "both"):
                    with ExitStack() as e2:
                        zs = T(e2, nc, "zs", [128, NTT, NX], F32)
                        xcf = T(e2, nc, "xcf", [128, NXC, TB], F32)
                        BCb = T(e2, nc, "BCb", [128, 2 * NG, TB], BF16)
                        Bf = T(e2, nc, "Bf", [128, NG, TB], F32)
                        xtok = T(e2, nc, "xtok", [128, NTT, NX], F32)
                        Btok = T(e2, nc, "Btok", [128, NTT, NG * 128], BF16)
                        dt = T(e2, nc, "dt", [128, NTT, NHD], F32)
                        with ExitStack() as e3:
                            wst = [T(e3, nc, f"wst{i}", [128, 16, 512], BF16) for i in range(2)]
                            wdt = T(e3, nc, "wdt", [128, 16, NHD], BF16)
                            raw = [T(e3, nc, f"raw{i}", [128, 3 + TB], F32) for i in range(2)]
                            cacc = [T(e3, nc, f"cacc{i}", [128, TB], F32) for i in range(2)]
                            wi = 0
                            for gz in range(NX // 512):
                                wb = wst[wi % 2]
                                wi += 1
                                P.dma("pool", wb[:, :, :], wssd_d[:, gz * 512:(gz + 1) * 512].rearrange("(k p) n -> p k n", p=128), w=[wb.r])
                                for tt in range(NTT):
                                    pb = ps[2 + tt % 2]
                                    for k in range(16):
                                        h.mm(pb[:, :], hT[:, k, tt * 128:(tt + 1) * 128], wb[:, k, :], k == 0, k == 15, [hT.r, wb.r], [pb.r])
                                    h.act(zs[:, tt, gz * 512:(gz + 1) * 512], pb[:, :], AF.Silu, [pb.r], [zs.r])
                            for gx in range(NCH // 4):
                                wb = wst[wi % 2]
                                wi += 1
                                c0 = NX + gx * 512
                                P.dma("pool", wb[:, :, :], wssd_d[:, c0:c0 + 512].rearrange("(k p) n -> p k n", p=128), w=[wb.r])
                                for half in range(4):
                                    cc = gx * 4 + half
                                    pb = ps[4 + cc % 2]
                                    rw = raw[cc % 2]
                                    ca = cacc[cc % 2]
                                    for k in range(16):
                                        h.mm(pb[:, :], wb[:, k, half * 128:(half + 1) * 128], hT[:, k, :], k == 0, k == 15, [wb.r, hT.r], [pb.r])
                                    h.cp("pool", rw[:, 0:3], halo[:, cc, :], [halo.r], [rw.r])
                                    h.cp("act", rw[:, 3:3 + TB], pb[:, :], [pb.r], [rw.r])
                                    h.cp("pool", halo[:, cc, :], rw[:, TB:TB + 3], [rw.r], [halo.r])
                                    h.ts("dve", ca[:, :], rw[:, 0:TB], convw[:, cc, 0:1], None, ALU.mult, None, [rw.r, convw.r], [ca.r])
                                    for k in range(1, 4):
                                        h.stt(ca[:, :], rw[:, k:k + TB], convw[:, cc, k:k + 1], ca[:, :], ALU.mult, ALU.add,
                                              [rw.r, convw.r, ca.r], [ca.r])
                                    if cc < NXC:
                                        h.act(xcf[:, cc, :], ca[:, :], AF.Silu, [ca.r, convb.r], [xcf.r], bias=convb[:, cc:cc + 1])
                                    else:
                                        h.act(BCb[:, cc - NXC, :], ca[:, :], AF.Silu, [ca.r, convb.r], [BCb.r], bias=convb[:, cc:cc + 1])
                                        if cc - NXC < NG:
                                            h.act(Bf[:, cc - NXC, :], ca[:, :], AF.Silu, [ca.r, convb.r], [Bf.r], bias=convb[:, cc:cc + 1])
                            P.dma("pool", wdt[:, :, :], wssd_d[:, WSSD - NHD:WSSD].rearrange("(k p) n -> p k n", p=128), w=[wdt.r])
                            for tt in range(NTT):
                                pb = ps[6 + tt % 2]
                                for k in range(16):
                                    h.mm(pb[:, 0:NHD], hT[:, k, tt * 128:(tt + 1) * 128], wdt[:, k, :], k == 0, k == 15, [hT.r, wdt.r], [pb.r])
                                h.tt("dve", dt[:, tt, :], pb[:, 0:NHD], dtb[:, :], ALU.add, [pb.r, dtb.r], [dt.r])
                            h.act(dt[:, :, :], dt[:, :, :], AF.Exp, [dt.r], [dt.r])
                            h.act(dt[:, :, :], dt[:, :, :], AF.Ln, [dt.r], [dt.r], bias=1.0)
                            for tt in range(NTT):
                                for q in range(NXC // 4):
                                    pb = ps[q % 2]
                                    for j in range(4):
                                        h.tr(pb[:, j * 128:(j + 1) * 128], xcf[:, q * 4 + j, tt * 128:(tt + 1) * 128], ident[:, :],
                                             [xcf.r, ident.r], [pb.r])
                                    h.cp("dve" if q % 2 else "act", xtok[:, tt, q * 512:(q + 1) * 512], pb[:, :], [pb.r], [xtok.r])
                                pb = ps[2 + tt % 2]
                                for g in range(NG):
                                    h.tr(pb[:, g * 128:(g + 1) * 128], Bf[:, g, tt * 128:(tt + 1) * 128], ident[:, :], [Bf.r, ident.r], [pb.r])
                                h.cp("dve", Btok[:, tt, :], pb[:, 0:NG * 128], [pb.r], [Btok.r])
                        P.barrier()
                        with ExitStack() as e4:
                            a_sb = T(e4, nc, "a_sb", [128, NHD], F32)
                            acs = T(e4, nc, "acs", [128, 4, NHD], F32)
                            CBm = T(e4, nc, "CBm", [128, NG, 128], F32)
                            aU = [T(e4, nc, f"aU{i}", [128, 128], F32) for i in range(4)]
                            Eq = [T(e4, nc, f"Eq{i}", [128, 4, 128], F32) for i in range(2)]
                            Mq = [T(e4, nc, f"Mq{i}", [128, 4, 128], BF16) for i in range(2)]
                            xdt = T(e4, nc, "xdt", [128, NX], BF16)
                            xs = T(e4, nc, "xs", [128, NX], BF16)
                            t1 = T(e4, nc, "t1", [128, NX], F32)
                            t3 = T(e4, nc, "t3", [128, NX], F32)
                            yo = [T(e4, nc, f"yo{i}", [128, NX], F32) for i in range(2)]
                            pA, pCB = ps[2], ps[3]
                            pYd = [ps[0], ps[1]]
                            pYo = [ps[6], ps[7]]
                            for tt in range(NTT):
                                cols = slice(tt * 128, (tt + 1) * 128)
                                h.tt("dve", a_sb[:, :], dt[:, tt, :], aneg[:, :], ALU.mult, [dt.r, aneg.r], [a_sb.r])
                                h.mm(pA[:, 0:NHD], ut[:, :], a_sb[:, :], True, True, [ut.r, a_sb.r], [pA.r])
                                h.mm(pA[:, 32:32 + NHD], ones[:, :], a_sb[:, :], True, True, [ones.r, a_sb.r], [pA.r])
                                h.cp("dve", acs[:, 0, :], pA[:, 0:NHD], [pA.r], [acs.r])
                                h.act(acs[:, 1, :], pA[:, 0:NHD], AF.Exp, [pA.r], [acs.r])
                                h.act(acs[:, 2, :], pA[:, 32:32 + NHD], AF.Exp, [pA.r], [acs.r])
                                h.tt("dve", acs[:, 3, :], pA[:, 32:32 + NHD], acs[:, 0, :], ALU.subtract, [pA.r, acs.r], [acs.r])
                                h.act(acs[:, 3, :], acs[:, 3, :], AF.Exp, [acs.r], [acs.r])
                                h.tt("dve", acs[:, 3, :], acs[:, 3, :], dt[:, tt, :], ALU.mult, [acs.r, dt.r], [acs.r])
                                for g in range(NG):
                                    h.mm(pCB[:, g * 128:(g + 1) * 128], BCb[:, g, cols], BCb[:, NG + g, cols], True, True, [BCb.r], [pCB.r])
                                h.tt("dve", CBm[:, :, :], pCB[:, 0:NG * 128].rearrange("p (g l) -> p g l", g=NG),
                                     ut[:, :].unsqueeze(1).to_broadcast([128, NG, 128]), ALU.mult, [pCB.r, ut.r], [CBm.r])
                                h.tt("dve", xdt[:, :].rearrange("p (a b) -> p a b", a=NHD), xtok[:, tt, :].rearrange("p (a b) -> p a b", a=NHD),
                                     dt[:, tt, :].unsqueeze(2).to_broadcast([128, NHD, 64]), ALU.mult, [xtok.r, dt.r], [xdt.r])
                                h.tt("pool", xs[:, :].rearrange("p (a b) -> p a b", a=NHD), xtok[:, tt, :].rearrange("p (a b) -> p a b", a=NHD),
                                     acs[:, 3, :].unsqueeze(2).to_broadcast([128, NHD, 64]), ALU.mult, [xtok.r, acs.r], [xs.r])
                                for g in range(NG):
                                    pS = ps[4 + g % 2]
                                    E_ = Eq[g % 2]
                                    M_ = Mq[g % 2]
                                    for r in range(4):
                                        hd = g * 4 + r
                                        au = aU[r]
                                        h.ts("dve", au[:, :], ut[:, :], a_sb[:, hd:hd + 1], None, ALU.mult, None, [ut.r, a_sb.r], [au.r])
                                        h.mm(pS[:, r * 128:(r + 1) * 128], slt[:, :], au[:, :], True, True, [slt.r, au.r], [pS.r])
                                    h.act(E_[:, :, :], pS[:, :].rearrange("p (a b) -> p a b", a=4), AF.Exp, [pS.r], [E_.r])
                                    h.tt("dve", M_[:, :, :], E_[:, :, :], CBm[:, g, :].unsqueeze(1).to_broadcast([128, 4, 128]), ALU.mult,
                                         [E_.r, CBm.r], [M_.r])
                                    for r in range(4):
                                        hd = g * 4 + r
                                        bank, c_ = hd // 8, (hd % 8) * 64
                                        h.mm(pYd[bank][:, c_:c_ + 64], M_[:, r, :], xdt[:, hd * 64:(hd + 1) * 64], True, True,
                                             [M_.r, xdt.r], [pYd[bank].r])
                                        h.mm(pYo[bank][:, c_:c_ + 64], BCb[:, NG + g, cols], Hb[:, hd * 64:(hd + 1) * 64], True, True,
                                             [BCb.r, Hb.r], [pYo[bank].r])
                                y_ = yo[tt % 2]
                                for bank in range(NHD // 8):
                                    cs = slice(bank * 512, (bank + 1) * 512)
                                    hs = slice(bank * 8, (bank + 1) * 8)
                                    h.tt("dve", t1[:, cs].rearrange("p (a b) -> p a b", a=8), pYo[bank][:, :].rearrange("p (a b) -> p a b", a=8),
                                         acs[:, 1, hs].unsqueeze(2).to_broadcast([128, 8, 64]), ALU.mult, [pYo[bank].r, acs.r], [t1.r])
                                    h.tt("dve", t1[:, cs], t1[:, cs], pYd[bank][:, :], ALU.add, [t1.r, pYd[bank].r], [t1.r])
                                    h.tt("pool", t3[:, cs].rearrange("p (a b) -> p a b", a=8), xtok[:, tt, cs].rearrange("p (a b) -> p a b", a=8),
                                         dsk[:, hs].unsqueeze(2).to_broadcast([128, 8, 64]), ALU.mult, [xtok.r, dsk.r], [t3.r])
                                    h.tt("pool", t3[:, cs], t3[:, cs], t1[:, cs], ALU.add, [t1.r, t3.r], [t3.r])
                                    h.tt("pool", y_[:, cs], t3[:, cs], zs[:, tt, cs], ALU.mult, [t3.r, zs.r], [y_.r])
                                out_toks.append(P.dma("sp", yg_d[t0 + tt * 128:t0 + (tt + 1) * 128, :], y_[:, :], r=[y_.r]))
                                for hd in range(NHD):
                                    g = hd // 4
                                    bank, c_ = hd // 8, (hd % 8) * 64
                                    h.mm(pYd[bank][:, c_:c_ + 64], Btok[:, tt, g * 128:(g + 1) * 128], xs[:, hd * 64:(hd + 1) * 64], True, True,
                                         [Btok.r, xs.r], [pYd[bank].r])
                                h.tt("dve", Hs[:, :].rearrange("p (a b) -> p a b", a=NHD), Hs[:, :].rearrange("p (a b) -> p a b", a=NHD),
                                     acs[:, 2, :].unsqueeze(2).to_broadcast([128, NHD, 64]), ALU.mult, [Hs.r, acs.r], [Hs.r])
                                for bank in range(NHD // 8):
                                    cs = slice(bank * 512, (bank + 1) * 512)
                                    h.tt("dve", Hs[:, cs], Hs[:, cs], pYd[bank][:, :], ALU.add, [Hs.r, pYd[bank].r], [Hs.r])
                                h.cp("act", Hb[:, :], Hs[:, :], [Hs.r], [Hb.r])
                        P.barrier()
                if kind in ("gm", "both"):
                    r0 = b * TB
                    with ExitStack() as e5:
                        wst = [T(e5, nc, f"wuv{i}", [128, 16, 512], BF16) for i in range(2)]
                        ug = T(e5, nc, "ug", [128, NTT, 2048], F32)
                        vg = T(e5, nc, "vg", [128, NTT, 2048], F32)
                        lng = T(e5, nc, "lng", [128, 2048], F32)
                        lnb = T(e5, nc, "lnb", [128, 2048], F32)
                        vn = T(e5, nc, "vn", [128, 2048], BF16)
                        bst = T(e5, nc, "bst", [128, 8, 6], F32)
                        mv = T(e5, nc, "mv", [128, 4], F32)
                        P.dma("sp", lng[:, :], lng_d[0:1, :].partition_broadcast(128), w=[lng.r])
                        P.dma("sp", lnb[:, :], lnb_d[0:1, :].partition_broadcast(128), w=[lnb.r])
                        wi = 0
                        for gu in range(8):
                            wb = wst[wi % 2]
                            wi += 1
                            P.dma("pool", wb[:, :, :], wuv_d[:, gu * 512:(gu + 1) * 512].rearrange("(k p) n -> p k n", p=128), w=[wb.r])
                            dst = ug if gu < 4 else vg
                            cg = (gu % 4) * 512
                            for tt in range(NTT):
                                pb = ps[2 + tt % 2]
                                for k in range(16):
                                    h.mm(pb[:, :], hT[:, k, tt * 128:(tt + 1) * 128], wb[:, k, :], k == 0, k == 15, [hT.r, wb.r], [pb.r])
                                h.act(dst[:, tt, cg:cg + 512], pb[:, :], AF.Gelu, [pb.r], [dst.r])
                        for tt in range(NTT):
                            for q in range(4):
                                h.P.op("dve", lambda e, q=q, tt=tt: e.bn_stats(out=bst[:, q, :], in_=vg[:, tt, q * 512:(q + 1) * 512]),
                                       [vg.r], [bst.r])
                            h.P.op("dve", lambda e: e.bn_aggr(out=mv[:, 0:2], in_=bst[:, 0:4, :].rearrange("p a b -> p (a b)")), [bst.r], [mv.r])
                            h.ts("dve", mv[:, 2:3], mv[:, 1:2], EPS, None, ALU.add, None, [mv.r], [mv.r])
                            h.act(mv[:, 2:3], mv[:, 2:3], AF.Sqrt, [mv.r], [mv.r])
                            h.recip(mv[:, 2:3], mv[:, 2:3], [mv.r], [mv.r])
                            h.ts("dve", vg[:, tt, :], vg[:, tt, :], mv[:, 0:1], mv[:, 2:3], ALU.subtract, ALU.mult, [vg.r, mv.r], [vg.r])
                            h.tt("pool", vg[:, tt, :], vg[:, tt, :], lng[:, :], ALU.mult, [vg.r, lng.r], [vg.r])
                            h.tt("pool", vn[:, :], vg[:, tt, :], lnb[:, :], ALU.add, [vg.r, lnb.r], [vn.r])
                            pv = [ps[0], ps[1], ps[6], ps[7]]
                            for g in range(16):
                                pb = pv[g // 4]
                                h.mm(pb[:, (g % 4) * 128:(g % 4 + 1) * 128], wsT[:, g, :], vn[:, g * 128:(g + 1) * 128], True, True,
                                     [wsT.r, vn.r], [pb.r])
                            for q in range(4):
                                cs = slice(q * 512, (q + 1) * 512)
                                h.tt("dve", vg[:, tt, cs].rearrange("p (a b) -> p a b", a=4), pv[q][:, :].rearrange("p (a b) -> p a b", a=4),
                                     bsT[:, q * 4:(q + 1) * 4].unsqueeze(2).to_broadcast([128, 4, 128]), ALU.add, [pv[q].r, bsT.r], [vg.r])
                            h.tt("pool", vg[:, tt, :], vg[:, tt, :], ug[:, tt, :], ALU.mult, [vg.r, ug.r], [vg.r])
                            out_toks.append(P.dma("sp", yb_d[r0 + tt * 128:r0 + (tt + 1) * 128, :], vg[:, tt, :], r=[vg.r]))
                    P.barrier()
        P.wait("sp", out_toks[-48:])
        if ctx is None:
            P.emit()
        print("mix0 instrs", P.ninstr, {e: len(P.q[e]) for e in P.ENGS})
    return nc


def _fm(v):
    return np.ascontiguousarray(np.asarray(v, dtype=np.float32).reshape(-1, 128).T)


def _c(a):
    return np.ascontiguousarray(np.asarray(a, dtype=np.float32))


def _consts():
    f = np.float32
    jj = np.arange(128)
    s_ = jj[:, None, None]
    j_ = np.arange(4)[None, :, None]
    t_ = np.arange(512)[None, None, :]
    return dict(
        ident=np.eye(128, dtype=f),
        ut=(jj[:, None] <= jj[None, :]).astype(f),
        slt=(jj[:, None] > jj[None, :]).astype(f),
        masks=((j_ * 128 + s_) < t_).astype(f),
        ntri=-(jj[:, None] >= jj[None, :]).astype(f),
        summ=(jj[:, None] // 8 == np.arange(16)[None, :]).astype(f),
    )


def _mix0_inputs(p, in0_w, conv_w, conv_b, dt_bias, a_log, d_skip, ln_g, ln_b, ws, bs):
    zc = slice(p * 1024, (p + 1) * 1024)
    xc = slice(2048 + p * 1024, 2048 + (p + 1) * 1024)
    Bc = slice(4096 + p * 512, 4096 + (p + 1) * 512)
    Cc = slice(5120 + p * 512, 5120 + (p + 1) * 512)
    dc = slice(6144 + p * 16, 6144 + (p + 1) * 16)
    w_ssd = np.concatenate([in0_w[:, zc], in0_w[:, xc], in0_w[:, Bc], in0_w[:, Cc], in0_w[:, dc]], axis=1)
    cch = np.concatenate([np.arange(p * 1024, (p + 1) * 1024), 2048 + np.arange(p * 512, (p + 1) * 512),
                          3072 + np.arange(p * 512, (p + 1) * 512)])
    cw = conv_w[:, cch]
    cb = conv_b[cch]
    hs = slice(p * 16, (p + 1) * 16)
    return dict(w_ssd=_c(w_ssd), conv_w_fm=_c(cw.T.reshape(16, 128, 4).transpose(1, 0, 2)), conv_b_fm=_fm(cb),
                dt_bias=_c(dt_bias[None, hs]), a_log=_c(a_log[None, hs]), d_skip=_c(d_skip[None, hs]),
                w_uv=_c(in0_w[:, 6176:]), ln_g=_c(ln_g[None]), ln_b=_c(ln_b[None]),
                wsT=_c(ws.transpose(2, 0, 1)), bsT=_c(bs.T))


def kernel_unfused(x, c, ada_w, ada_b, norm_mix_g, norm_ffn_g, in0_w, conv_w, conv_b, dt_bias, a_log, d_skip, ssd_norm_g,
           gmlp_ln_g, gmlp_ln_b, gmlp_ws, gmlp_bs, out0_w, sb_qkv_w, sb_out_w, peer_wq, peer_keys, peer_u, peer_v, final_g):
    g = {k: np.asarray(v) for k, v in locals().items()}
    x = g["x"]
    c = g["c"]
    K = _consts()
    cores = list(range(8))
    HALF = 2048

    def ada_in(layer, b):
        return dict(c_fm=_fm(c[b]), ada_w=_c(g["ada_w"][layer]), ada_b_fm=_fm(g["ada_b"][layer]), ada_b=_c(g["ada_b"][layer][None]))

    nc1 = build_mix0(4096, 16, 4)
    mi = [_mix0_inputs(p, g["in0_w"][0], g["conv_w"][0], g["conv_b"][0], g["dt_bias"][0], g["a_log"][0], g["d_skip"][0],
                       g["gmlp_ln_g"][0], g["gmlp_ln_b"][0], g["gmlp_ws"][0], g["gmlp_bs"][0]) for p in range(2)]
    maps = []
    for core in cores:
        b, p = divmod(core, 2)
        d = dict(x=_c(x[b]), x_gm=_c(x[b, p * HALF:(p + 1) * HALF]), g_mix_fm=_fm(g["norm_mix_g"][0]),
                 ident=K["ident"], ut=K["ut"], slt=K["slt"])
        d.update(ada_in(0, b))
        d.update(mi[p])
        maps.append(d)
    r1 = run_bass_kernel_spmd(nc1, maps, core_ids=cores).results
    del maps
    nc2 = build_tail(HALF, 4096, final=False, ya_norm=True)
    maps = []
    for core in cores:
        b, p = divmod(core, 2)
        rows = slice(p * HALF, (p + 1) * HALF)
        mix = np.concatenate([r1[2 * b]["yg"][rows], r1[2 * b + 1]["yg"][rows], r1[core]["yb"]], axis=1)
        d = dict(x=_c(x[b, rows]), mix=_c(mix), wo=_c(g["out0_w"][0]), g_ffn_fm=_fm(g["norm_ffn_g"][0]), wq=_c(g["peer_wq"][0]),
                 keys=_c(g["peer_keys"][0].reshape(16, 128, 128)), U=_c(g["peer_u"][0]), V=_c(g["peer_v"][0]),
                 final_g=_c(g["final_g"][None]), g_ya_fm=_fm(g["ssd_norm_g"][0]), ident=K["ident"], summ=K["summ"])
        d.update(ada_in(0, b))
        maps.append(d)
    r2 = run_bass_kernel_spmd(nc2, maps, core_ids=cores).results
    del maps, r1
    nc3 = build_attn(4096, 8, 4)
    qkv = g["sb_qkv_w"][0]
    maps = []
    for core in cores:
        b, p = divmod(core, 2)
        hs = slice(p * 1024, (p + 1) * 1024)
        wqkv = np.concatenate([qkv[:, 0:2048][:, hs], qkv[:, 2048:4096][:, hs], qkv[:, 4096:6144][:, hs]], axis=1)
        xf = np.concatenate([r2[2 * b]["out"], r2[2 * b + 1]["out"]], axis=0)
        d = dict(x=_c(xf), g_mix_fm=_fm(g["norm_mix_g"][1]), wqkv=_c(wqkv), ident=K["ident"], masks=K["masks"], ntri=K["ntri"])
        d.update(ada_in(1, b))
        maps.append(d)
    r3 = run_bass_kernel_spmd(nc3, maps, core_ids=cores).results
    del maps
    nc4 = build_tail(HALF, 2048, final=True, ya_norm=False)
    maps = []
    for core in cores:
        b, p = divmod(core, 2)
        rows = slice(p * HALF, (p + 1) * HALF)
        mix = np.concatenate([r3[2 * b]["oT"][:, rows].T, r3[2 * b + 1]["oT"][:, rows].T], axis=1)
        d = dict(x=_c(r2[core]["out"]), mix=_c(mix), wo=_c(g["sb_out_w"][0]), g_ffn_fm=_fm(g["norm_ffn_g"][1]), wq=_c(g["peer_wq"][1]),
                 keys=_c(g["peer_keys"][1].reshape(16, 128, 128)), U=_c(g["peer_u"][1]), V=_c(g["peer_v"][1]),
                 final_g=_c(g["final_g"][None]), g_ya_fm=_fm(g["ssd_norm_g"][0]), ident=K["ident"], summ=K["summ"])
        d.update(ada_in(1, b))
        maps.append(d)
    r4 = run_bass_kernel_spmd(nc4, maps, core_ids=cores).results
    out = np.empty((4, 4096, 2048), dtype=np.float32)
    for core in cores:
        b, p = divmod(core, 2)
        out[b, p * HALF:(p + 1) * HALF] = r4[core]["out"]
    return out


def build_fused(S_LEN=4096):
    from contextlib import ExitStack
    nc = bass.Bass("TRN2", target_bir_lowering=False)
    HALF = S_LEN // 2

    def ein(name, shape):
        return nc.dram_tensor(name, shape, F32, kind="ExternalInput").ap()

    x_d = ein("x", [S_LEN, D])
    cfm = ein("c_fm", [128, 16])
    adaw = ein("ada_w", [2, D, 6 * D])
    adabfm = ein("ada_b_fm", [2, 128, 96])
    adab = ein("ada_b", [2, 1, 6 * D])
    gmix = ein("g_mix_fm", [2, 128, 16])
    gffn = ein("g_ffn_fm", [2, 128, 16])
    wssd = ein("w_ssd", [2, D, 3088])
    convw = ein("conv_w_fm", [2, 128, 16, 4])
    convb = ein("conv_b_fm", [2, 128, 16])
    dtb = ein("dt_bias", [2, 1, 16])
    alog = ein("a_log", [2, 1, 16])
    dsk = ein("d_skip", [2, 1, 16])
    wuv = ein("w_uv", [D, 4096])
    lng = ein("ln_g", [1, 2048])
    lnb = ein("ln_b", [1, 2048])
    wsT = ein("wsT", [128, 16, 128])
    bsT = ein("bsT", [128, 16])
    wo0 = ein("out0_w", [4096, D])
    gya = ein("g_ya_fm", [128, 16])
    wq = ein("wq", [2, D, D])
    keys = ein("keys", [2, 16, 128, 128])
    U = ein("U", [2, 16384, D])
    V = ein("V", [2, 16384, D])
    fing = ein("final_g", [1, D])
    wqkv = ein("wqkv", [D, 3 * D])
    wo1 = ein("sb_out_w", [D, D])
    sel = ein("sel", [128, 2])
    ident = ein("ident", [128, 128])
    ut = ein("ut", [128, 128])
    slt = ein("slt", [128, 128])
    masks = ein("masks", [128, 4, 512])
    ntri = ein("ntri", [128, 128])
    summ = ein("summ", [128, 16])
    mixA = nc.dram_tensor("mixA", [S_LEN, 4096], F32, kind="Internal").ap()
    x2 = nc.dram_tensor("x2", [S_LEN, D], F32, kind="Internal").ap()
    o_d = nc.dram_tensor("o_int", [S_LEN, D], F32, kind="Internal").ap()
    out_d = nc.dram_tensor("out", [HALF, D], F32, kind="ExternalOutput").ap()

    def ada_env(layer):
        return dict(c_fm=cfm, ada_w=adaw[layer], ada_b_fm=adabfm[layer], ada_b=adab[layer])

    with ExitStack() as es:
        ctx = Ctx(nc, es)
        for p in range(2):
            env = dict(x=x_d, x_gm=x_d, g_mix_fm=gmix[0], w_ssd=wssd[p], conv_w_fm=convw[p], conv_b_fm=convb[p],
                       dt_bias=dtb[p], a_log=alog[p], d_skip=dsk[p], w_uv=wuv, ln_g=lng, ln_b=lnb, wsT=wsT, bsT=bsT,
                       ident=ident, ut=ut, slt=slt, yg=mixA[:, p * 1024:(p + 1) * 1024], yb=mixA[:, 2048:4096])
            env.update(ada_env(0))
            build_mix0(S_LEN, 16, gm_nblk=(S_LEN // TB if p == 0 else 0), ctx=ctx, env=env, pfx=f"m{p}_")
        env = dict(x=x_d, mix=mixA, wo=wo0, g_ffn_fm=gffn[0], wq=wq[0], keys=keys[0], U=U[0], V=V[0], final_g=fing,
                   g_ya_fm=gya, ident=ident, summ=summ, out=x2)
        env.update(ada_env(0))
        build_tail(S_LEN, 4096, final=False, ya_norm=True, ctx=ctx, env=env, pfx="t0_")
        env = dict(x=x2, g_mix_fm=gmix[1], wqkv=wqkv, ident=ident, masks=masks, ntri=ntri, o=o_d)
        env.update(ada_env(1))
        build_attn(S_LEN, 16, 4, ctx=ctx, env=env, pfx="a_", tokmajor=True)
        env = dict(x=x2, mix=o_d, wo=wo1, g_ffn_fm=gffn[1], wq=wq[1], keys=keys[1], U=U[1], V=V[1], final_g=fing,
                   g_ya_fm=gya, ident=ident, summ=summ, out=out_d, sel=sel)
        env.update(ada_env(1))
        build_tail(HALF, 2048, final=True, ya_norm=False, ctx=ctx, env=env, pfx="t1_", blend=True)
        ctx.P.emit()
        print("fused instrs", ctx.P.ninstr, {e: len(ctx.P.q[e]) for e in ctx.P.ENGS})
    return nc


def kernel(x, c, ada_w, ada_b, norm_mix_g, norm_ffn_g, in0_w, conv_w, conv_b, dt_bias, a_log, d_skip, ssd_norm_g,
           gmlp_ln_g, gmlp_ln_b, gmlp_ws, gmlp_bs, out0_w, sb_qkv_w, sb_out_w, peer_wq, peer_keys, peer_u, peer_v, final_g):
    g = {k: np.asarray(v) for k, v in locals().items()}
    K = _consts()
    cores = list(range(8))
    mi = [_mix0_inputs(p, g["in0_w"][0], g["conv_w"][0], g["conv_b"][0], g["dt_bias"][0], g["a_log"][0], g["d_skip"][0],
                       g["gmlp_ln_g"][0], g["gmlp_ln_b"][0], g["gmlp_ws"][0], g["gmlp_bs"][0]) for p in range(2)]
    shared = dict(
        ada_w=_c(g["ada_w"]), ada_b_fm=_c(np.stack([_fm(g["ada_b"][l]) for l in range(2)])), ada_b=_c(g["ada_b"][:, None, :]),
        g_mix_fm=_c(np.stack([_fm(g["norm_mix_g"][l]) for l in range(2)])),
        g_ffn_fm=_c(np.stack([_fm(g["norm_ffn_g"][l]) for l in range(2)])),
        w_uv=mi[0]["w_uv"], ln_g=mi[0]["ln_g"], ln_b=mi[0]["ln_b"], wsT=mi[0]["wsT"], bsT=mi[0]["bsT"],
        out0_w=_c(g["out0_w"][0]), g_ya_fm=_fm(g["ssd_norm_g"][0]), wq=_c(g["peer_wq"]),
        keys=_c(g["peer_keys"].reshape(2, 16, 128, 128)), U=_c(g["peer_u"]), V=_c(g["peer_v"]),
        final_g=_c(g["final_g"][None]), wqkv=_c(g["sb_qkv_w"][0]), sb_out_w=_c(g["sb_out_w"][0]),
        ident=K["ident"], ut=K["ut"], slt=K["slt"], masks=K["masks"], ntri=K["ntri"], summ=K["summ"])
    for k in ("w_ssd", "conv_w_fm", "conv_b_fm", "dt_bias", "a_log", "d_skip"):
        shared[k] = _c(np.stack([mi[0][k], mi[1][k]]))
    nc = build_fused(4096)
    maps = []
    for core in cores:
        b, p = divmod(core, 2)
        d = dict(shared)
        d["x"] = _c(g["x"][b])
        d["c_fm"] = _fm(g["c"][b])
        selv = np.zeros((128, 2), dtype=np.float32)
        selv[:, p] = 1.0
        d["sel"] = selv
        maps.append(d)
    res = run_bass_kernel_spmd(nc, maps, core_ids=cores).results
    out = np.empty((4, 4096, 2048), dtype=np.float32)
    for core in cores:
        b, p = divmod(core, 2)
        out[b, p * 2048:(p + 1) * 2048] = res[core]["out"]
    return out
```
